# Optimizing a Trainium2 kernel written in Bass

```python
import math
import jax, jax.numpy as jnp
from jax import lax
import numpy as np

D_MODEL = 2048
BATCH = 4
SEQ = 4096
DEPTH = 1

NORM_EPS = 1e-6
RWKV_HEADS = 16
RWKV_HEAD_DIM = 64
RWKV_WIDTH = RWKV_HEADS * RWKV_HEAD_DIM
DECAY_LORA = max(32, int(round(1.8 * D_MODEL ** 0.5 / 32)) * 32)
ICLR_LORA = max(32, int(round(1.8 * D_MODEL ** 0.5 / 32)) * 32)
GATE_LORA = max(32, int(round(0.6 * D_MODEL ** 0.8 / 32)) * 32)
GN_EPS = 64e-5
RWKV_COLS = 3 * RWKV_WIDTH + DECAY_LORA + ICLR_LORA + GATE_LORA
RWKV_SPLITS = [RWKV_WIDTH, 2 * RWKV_WIDTH, 3 * RWKV_WIDTH,
               3 * RWKV_WIDTH + DECAY_LORA, 3 * RWKV_WIDTH + DECAY_LORA + ICLR_LORA]
ATT_Q_HEADS = 16
ATT_KV_HEADS = 4
ATT_GROUP = ATT_Q_HEADS // ATT_KV_HEADS
ATT_HEAD_DIM = 64
ATT_Q_WIDTH = ATT_Q_HEADS * ATT_HEAD_DIM
ATT_KV_WIDTH = ATT_KV_HEADS * ATT_HEAD_DIM
WINDOW = 128
ATT_BLOCK = 128
IN_COLS = RWKV_COLS + ATT_Q_WIDTH + 2 * ATT_KV_WIDTH + 2 * D_MODEL
IN_SPLITS = [RWKV_COLS, RWKV_COLS + ATT_Q_WIDTH, RWKV_COLS + ATT_Q_WIDTH + ATT_KV_WIDTH,
             RWKV_COLS + ATT_Q_WIDTH + 2 * ATT_KV_WIDTH,
             RWKV_COLS + ATT_Q_WIDTH + 2 * ATT_KV_WIDTH + D_MODEL]
N_GROUPS = 8
EXPERTS_PER_GROUP = 8
N_EXPERTS = N_GROUPS * EXPERTS_PER_GROUP
TOP_K_FINE = 2
EXPERT_FF = 1024
MOE_BLOCK = 128

kernel_name = "hybrid_rwkv7_swa_sink_hmoe"


def rms_norm(t, gain, eps=NORM_EPS):
    t32 = t.astype(jnp.float32)
    y = t32 * lax.rsqrt(jnp.mean(t32 * t32, axis=-1, keepdims=True) + eps)
    return (y * gain.astype(jnp.float32)).astype(t.dtype)


def rwkv7_time_mix(p, mu, w0, w2, a0, a2, g2, k_k, k_a, r_k, gn_w, gn_b):
    B, S, _ = p.shape
    H, N = RWKV_HEADS, RWKV_HEAD_DIM
    f32 = jnp.float32
    p_prev = jnp.pad(p[:, :-1], ((0, 0), (1, 0), (0, 0)))
    p = p + (p_prev - p) * mu
    r, k, v, xw, xa, xg = jnp.split(p, RWKV_SPLITS, axis=-1)
    w = -jax.nn.softplus(-(w0 + jnp.tanh(xw) @ w2).astype(f32)) - 0.5
    decay = jnp.exp(-jnp.exp(w))
    a = jax.nn.sigmoid((a0 + xa @ a2).astype(f32))
    g = jax.nn.sigmoid(xg) @ g2
    heads = lambda t: t.astype(f32).reshape(B, S, H, N)
    kk = heads(k * k_k)
    kk = kk / jnp.maximum(jnp.sqrt(jnp.sum(kk * kk, axis=-1, keepdims=True)), 1e-12)
    k = heads(k) * (1.0 + (heads(a) - 1.0) * k_a.astype(f32).reshape(H, N))
    r_h, v_h, a_h, w_h = heads(r), heads(v), heads(a), heads(decay)
    to_time = lambda t: jnp.moveaxis(t, 1, 0)

    def step(state, inp):
        rt, wt, kt, vt, an, bt = inp
        sa = jnp.einsum('bhij,bhj->bhi', state, an)
        state = state * wt[:, :, None, :] + sa[..., None] * bt[:, :, None, :] + vt[..., None] * kt[:, :, None, :]
        return state, jnp.einsum('bhij,bhj->bhi', state, rt)

    state0 = jnp.zeros((B, H, N, N), f32)
    xs = (to_time(r_h), to_time(w_h), to_time(k), to_time(v_h), to_time(-kk), to_time(kk * a_h))
    _, y = lax.scan(step, state0, xs)
    y = jnp.moveaxis(y, 0, 1)
    mean = jnp.mean(y, axis=-1, keepdims=True)
    var = jnp.mean(jnp.square(y - mean), axis=-1, keepdims=True)
    y = (y - mean) * lax.rsqrt(var + GN_EPS) * gn_w.astype(f32).reshape(H, N) + gn_b.astype(f32).reshape(H, N)
    bonus = jnp.sum(r_h * k * r_k.astype(f32), axis=-1, keepdims=True) * v_h
    out = (y + bonus).reshape(B, S, H * N).astype(p.dtype)
    return out * g


def sliding_window_sink_attention(q, k, v, q_gain, k_gain, sinks):
    B, S, _ = q.shape
    nb = S // ATT_BLOCK
    q = rms_norm(q.reshape(B, S, ATT_Q_HEADS, ATT_HEAD_DIM), q_gain)
    k = rms_norm(k.reshape(B, S, ATT_KV_HEADS, ATT_HEAD_DIM), k_gain)
    v = v.reshape(B, S, ATT_KV_HEADS, ATT_HEAD_DIM)
    qb = q.reshape(B, nb, ATT_BLOCK, ATT_KV_HEADS, ATT_GROUP, ATT_HEAD_DIM)

    def with_prev(t):
        tb = t.reshape(B, nb, ATT_BLOCK, ATT_KV_HEADS, ATT_HEAD_DIM)
        prev = jnp.pad(tb[:, :-1], ((0, 0), (1, 0), (0, 0), (0, 0), (0, 0)))
        return jnp.concatenate([prev, tb], axis=2)

    kw, vw = with_prev(k), with_prev(v)
    scale = ATT_HEAD_DIM ** -0.5
    s = jnp.einsum('bnqhgd,bnkhd->bnhgqk', qb, kw, preferred_element_type=jnp.float32) * scale
    i = jnp.arange(ATT_BLOCK)[:, None]
    j = jnp.arange(2 * ATT_BLOCK)[None, :]
    rel = ATT_BLOCK + i - j
    blk = jnp.arange(nb)[:, None, None]
    valid = (rel >= 0) & (rel < WINDOW) & ((blk > 0) | (j >= ATT_BLOCK))
    s = jnp.where(valid[None, :, None, None], s, -jnp.inf)
    sink = sinks.astype(jnp.float32).reshape(ATT_KV_HEADS, ATT_GROUP)[None, None, :, :, None, None]
    m = jnp.maximum(jnp.max(s, axis=-1, keepdims=True), sink)
    pr = jnp.exp(s - m)
    denom = jnp.sum(pr, axis=-1, keepdims=True) + jnp.exp(sink - m)
    o = jnp.einsum('bnhgqk,bnkhd->bnqhgd', (pr / denom).astype(v.dtype), vw)
    return o.reshape(B, S, ATT_Q_WIDTH)


def hierarchical_moe(h, wc, bc, wf, bf, wg, wu, wd):
    B, S, D = h.shape
    N = B * S
    M = N * TOP_K_FINE
    hf = h.reshape(N, D)
    coarse = jax.nn.softmax((hf @ wc).astype(jnp.float32) + bc.astype(jnp.float32), axis=-1)
    g_prob, g_idx = lax.top_k(coarse, 1)
    fine_logits = ((hf @ wf).astype(jnp.float32) + bf.astype(jnp.float32)).reshape(N, N_GROUPS, EXPERTS_PER_GROUP)
    fine_in_group = jnp.take_along_axis(fine_logits, g_idx[:, :, None], axis=1)[:, 0]
    f_prob, f_idx = lax.top_k(jax.nn.softmax(fine_in_group, axis=-1), TOP_K_FINE)
    weights = g_prob * f_prob / jnp.sum(f_prob, axis=-1, keepdims=True)
    expert_ids = (g_idx * EXPERTS_PER_GROUP + f_idx).reshape(M)
    order = jnp.argsort(expert_ids)
    sorted_ids = expert_ids[order]
    counts = jnp.bincount(expert_ids, length=N_EXPERTS)
    padded = (counts + MOE_BLOCK - 1) // MOE_BLOCK * MOE_BLOCK
    seg_start = jnp.cumsum(counts) - counts
    pad_end = jnp.cumsum(padded)
    pad_start = pad_end - padded
    dest = pad_start[sorted_ids] + jnp.arange(M) - seg_start[sorted_ids]
    token_of_row = order // TOP_K_FINE
    P = M + N_EXPERTS * MOE_BLOCK
    n_blocks = P // MOE_BLOCK
    buf = jnp.zeros((P, D), h.dtype).at[dest].set(hf[token_of_row])
    block_expert = jnp.minimum(jnp.searchsorted(pad_end, jnp.arange(n_blocks) * MOE_BLOCK, side='right'), N_EXPERTS - 1)

    def expert_block(args):
        xb, e = args
        return (jax.nn.silu(xb @ wg[e]) * (xb @ wu[e])) @ wd[e]

    out = lax.map(expert_block, (buf.reshape(n_blocks, MOE_BLOCK, D), block_expert)).reshape(P, D)
    row_w = weights.reshape(M)[order].astype(out.dtype)
    y = jax.ops.segment_sum(out[dest] * row_w[:, None], token_of_row, num_segments=N)
    return y.reshape(B, S, D)


def setup_inputs(seed: int = 0) -> dict:
    key = jax.random.key(seed)
    ks = jax.random.split(key, 32)
    L = DEPTH
    f32 = jnp.float32
    nrm = lambda k, shape, scale: jax.random.normal(k, shape, f32) * scale
    return {
        "x": nrm(ks[0], (BATCH, SEQ, D_MODEL), 1.0),
        "norm1_w": 1.0 + nrm(ks[1], (L, D_MODEL), 0.02),
        "w_in": nrm(ks[2], (L, D_MODEL, IN_COLS), D_MODEL ** -0.5),
        "rwkv_mu": jax.random.uniform(ks[3], (L, RWKV_COLS), f32),
        "rwkv_w0": jax.random.uniform(ks[4], (L, RWKV_WIDTH), f32, -6.0, 1.0),
        "rwkv_w2": nrm(ks[5], (L, DECAY_LORA, RWKV_WIDTH), 0.1 * DECAY_LORA ** -0.5),
        "rwkv_a0": nrm(ks[6], (L, RWKV_WIDTH), 0.5),
        "rwkv_a2": nrm(ks[7], (L, ICLR_LORA, RWKV_WIDTH), 0.1 * ICLR_LORA ** -0.5),
        "rwkv_g2": nrm(ks[8], (L, GATE_LORA, RWKV_WIDTH), GATE_LORA ** -0.5),
        "rwkv_k_k": 0.85 + nrm(ks[9], (L, RWKV_WIDTH), 0.05),
        "rwkv_k_a": 1.0 + nrm(ks[10], (L, RWKV_WIDTH), 0.05),
        "rwkv_r_k": nrm(ks[11], (L, RWKV_HEADS, RWKV_HEAD_DIM), 0.1),
        "rwkv_gn_w": 1.0 + nrm(ks[12], (L, RWKV_WIDTH), 0.02),
        "rwkv_gn_b": nrm(ks[13], (L, RWKV_WIDTH), 0.02),
        "q_norm_w": 1.0 + nrm(ks[14], (L, ATT_HEAD_DIM), 0.02),
        "k_norm_w": 1.0 + nrm(ks[15], (L, ATT_HEAD_DIM), 0.02),
        "attn_sinks": nrm(ks[16], (L, ATT_Q_HEADS), 1.0),
        "proj_rwkv": nrm(ks[17], (L, RWKV_WIDTH, D_MODEL), RWKV_WIDTH ** -0.5),
        "proj_attn": nrm(ks[18], (L, ATT_Q_WIDTH, D_MODEL), ATT_Q_WIDTH ** -0.5),
        "w_out": nrm(ks[19], (L, D_MODEL, D_MODEL), D_MODEL ** -0.5),
        "norm2_w": 1.0 + nrm(ks[20], (L, D_MODEL), 0.02),
        "router_coarse_w": nrm(ks[21], (L, D_MODEL, N_GROUPS), D_MODEL ** -0.5),
        "router_coarse_b": nrm(ks[22], (L, N_GROUPS), 0.01),
        "router_fine_w": nrm(ks[23], (L, D_MODEL, N_EXPERTS), D_MODEL ** -0.5),
        "router_fine_b": nrm(ks[24], (L, N_EXPERTS), 0.01),
        "expert_w_gate": nrm(ks[25], (L, N_EXPERTS, D_MODEL, EXPERT_FF), D_MODEL ** -0.5),
        "expert_w_up": nrm(ks[26], (L, N_EXPERTS, D_MODEL, EXPERT_FF), D_MODEL ** -0.5),
        "expert_w_down": nrm(ks[27], (L, N_EXPERTS, EXPERT_FF, D_MODEL), EXPERT_FF ** -0.5),
    }


def reference(x, norm1_w, w_in, rwkv_mu, rwkv_w0, rwkv_w2, rwkv_a0, rwkv_a2, rwkv_g2,
              rwkv_k_k, rwkv_k_a, rwkv_r_k, rwkv_gn_w, rwkv_gn_b, q_norm_w, k_norm_w,
              attn_sinks, proj_rwkv, proj_attn, w_out, norm2_w, router_coarse_w,
              router_coarse_b, router_fine_w, router_fine_b, expert_w_gate, expert_w_up,
              expert_w_down):
    for layer in range(DEPTH):
        h = rms_norm(x, norm1_w[layer])
        proj = h @ w_in[layer]
        p_rwkv, q, k_att, v_att, gate_a, gate_b = jnp.split(proj, IN_SPLITS, axis=-1)
        a_out = rwkv7_time_mix(p_rwkv, rwkv_mu[layer], rwkv_w0[layer], rwkv_w2[layer],
                               rwkv_a0[layer], rwkv_a2[layer], rwkv_g2[layer], rwkv_k_k[layer],
                               rwkv_k_a[layer], rwkv_r_k[layer], rwkv_gn_w[layer], rwkv_gn_b[layer])
        b_out = sliding_window_sink_attention(q, k_att, v_att, q_norm_w[layer], k_norm_w[layer],
                                              attn_sinks[layer])
        merged = (jax.nn.sigmoid(gate_a) * (a_out @ proj_rwkv[layer])
                  + jax.nn.sigmoid(gate_b) * (b_out @ proj_attn[layer]))
        x = x + merged @ w_out[layer]
        h2 = rms_norm(x, norm2_w[layer])
        x = x + hierarchical_moe(h2, router_coarse_w[layer], router_coarse_b[layer],
                                 router_fine_w[layer], router_fine_b[layer], expert_w_gate[layer],
                                 expert_w_up[layer], expert_w_down[layer])
    return x
```

```python
import numpy as np
import ml_dtypes
import concourse.bass as bass
import concourse.mybir as mybir
from concourse.bass_utils import run_bass_kernel_spmd

F32 = mybir.dt.float32
BF16 = mybir.dt.bfloat16
I32 = mybir.dt.int32
AF = mybir.ActivationFunctionType
ALU = mybir.AluOpType
AX = mybir.AxisListType

D = 2048
RW = 1024
TT = 128
NS = TT // 128
CH = 64
NCH = TT // CH
IN_COLS = 9152
C_R, C_K, C_V, C_XW, C_XA, C_XG = 0, 1024, 2048, 3072, 3168, 3264
C_Q, C_KA, C_VA, C_GA, C_GB = 3520, 4544, 4800, 5056, 7104
NDS = 40
DECAY_C = 0.6065306597126334


class Sched:
    def __init__(self, nc):
        from contextlib import ExitStack
        self.stack = ExitStack()
        self.nc = nc
        self.E = dict(pe=nc.tensor, dve=nc.vector, act=nc.scalar, pool=nc.gpsimd, sp=nc.sync)
        self.esem = {}
        for e in ("pe", "dve", "act", "pool"):
            self.esem[e] = self.stack.enter_context(nc.semaphore("es_" + e))
        self.ecnt = dict.fromkeys(self.esem, 0)
        self.dsems = [self.stack.enter_context(nc.semaphore("ds%d" % i)) for i in range(NDS)]
        self.dcnt = [0] * NDS
        self.dnext = 0
        self.seen = {e: {} for e in self.E}
        self.W = {}
        self.R = {}

    def _wait(self, eng, deps):
        for sid, (sem, val) in deps.items():
            if val > 0 and self.seen[eng].get(sid, 0) < val:
                self.E[eng].wait_ge(sem, val)
                self.seen[eng][sid] = val

    def _deps(self, reads, writes):
        deps = {}

        def add(d):
            for sid, (sem, val) in d.items():
                if sid not in deps or deps[sid][1] < val:
                    deps[sid] = (sem, val)
        for r in reads:
            add(self.W.get(r, {}))
        for w in writes:
            add(self.W.get(w, {}))
            add(self.R.get(w, {}))
        return deps

    def _record(self, me, reads, writes):
        sid, sem, val = me
        for r in reads:
            self.R.setdefault(r, {})[sid] = (sem, val)
        for w in writes:
            self.W[w] = {sid: (sem, val)}
            self.R[w] = {}

    def op(self, eng, fn, reads=(), writes=()):
        self._wait(eng, self._deps(reads, writes))
        ins = fn(self.E[eng])
        self.ecnt[eng] += 1
        ins.then_inc(self.esem[eng], 1)
        self._record((eng, self.esem[eng], self.ecnt[eng]), reads, writes)

    def dma(self, q, fn, reads=(), writes=()):
        i = self.dnext
        self.dnext = (i + 1) % NDS
        sem = self.dsems[i]
        deps = self._deps(reads, writes)
        sid = "d%d" % i
        if sid not in deps or deps[sid][1] < self.dcnt[i]:
            deps[sid] = (sem, self.dcnt[i])
        self._wait(q, deps)
        ins = fn(self.E[q])
        ins.then_inc(sem, 16)
        self.dcnt[i] += 16
        self._record((sid, sem, self.dcnt[i]), reads, writes)

    def barrier(self):
        keys = list(self.W.keys())
        for eng in self.E:
            self.wait_all(eng, keys)

    def wait_all(self, eng, keys):
        deps = {}
        for k in keys:
            for d in (self.W.get(k, {}), self.R.get(k, {})):
                for sid, (sem, val) in d.items():
                    if sid not in deps or deps[sid][1] < val:
                        deps[sid] = (sem, val)
        self._wait(eng, deps)


def build_program(cfg):
    NPREV = cfg["n_prev"]
    NOWN = cfg["n_own"]
    NG = cfg["n_groups"]
    NE = NG * 8
    CAP = cfg["cap"]
    NT = NPREV + NOWN
    NTOK = NT * TT
    NOWN_TOK = NOWN * TT
    NSUB = NOWN_TOK // 128
    NRL = 8 + NE
    dbg = cfg.get("dbg", False)

    nc = bass.Bass("TRN2", target_bir_lowering=False)
    _ncd = nc.allow_non_contiguous_dma(reason="tiny per-channel parameter loads")
    _ncd.__enter__()
    S = Sched(nc)

    def din(name, shape, dt=F32):
        return nc.dram_tensor(name, list(shape), dt, kind="ExternalInput").ap()

    xs = din("xs", [NTOK, D])
    flag_d = din("flag", [128, 1])
    w_in = din("w_in", [D, IN_COLS])
    norm1_w = din("norm1_w", [1, D])
    norm2_w = din("norm2_w", [1, D])
    mu_d = din("rwkv_mu", [3520, 1])
    w0_d = din("rwkv_w0", [RW, 1])
    a0_d = din("rwkv_a0", [RW, 1])
    kk_d = din("rwkv_k_k", [RW, 1])
    ka_d = din("rwkv_k_a", [RW, 1])
    rk_d = din("rwkv_r_k", [RW, 1])
    w2_d = din("rwkv_w2", [96, RW])
    a2_d = din("rwkv_a2", [96, RW])
    g2_d = din("rwkv_g2", [256, RW])
    gnw_d = din("rwkv_gn_w", [1, RW])
    gnb_d = din("rwkv_gn_b", [1, RW])
    qn_d = din("q_norm_w", [64, 1])
    kn_d = din("k_norm_w", [64, 1])
    sink_d = din("attn_sinks", [16, 1])
    proj_r = din("proj_rwkv", [RW, D])
    proj_a = din("proj_attn", [RW, D])
    w_out = din("w_out", [D, D])
    rcw_d = din("router_coarse_w", [D, NG])
    rcb_d = din("router_coarse_b", [1, NG])
    rfw_d = din("router_fine_w", [D, NE])
    rfb_d = din("router_fine_b", [1, NE])
    ewg = din("expert_w_gate", [NE, D, 1024])
    ewu = din("expert_w_up", [NE, D, 1024])
    ewd = din("expert_w_down", [NE, 1024, D])
    cst_d = din("consts", [128, 1408])
    ecap_d = din("ecap", [128, NE])
    y_out = nc.dram_tensor("y", [NOWN_TOK, D], F32, kind="ExternalOutput").ap()
    Xd = nc.dram_tensor("xdisp", [NE * CAP, D], BF16).ap()
    Yd = nc.dram_tensor("ydisp", [NE * CAP, D], F32).ap()
    dbg_t = {}
    if dbg:
        dbg_t["aout"] = nc.dram_tensor("dbg_aout", [RW, NOWN_TOK], F32, kind="ExternalOutput").ap()
        dbg_t["bout"] = nc.dram_tensor("dbg_bout", [RW, NOWN_TOK], F32, kind="ExternalOutput").ap()
        dbg_t["xin"] = nc.dram_tensor("dbg_x1", [NOWN_TOK, D], F32, kind="ExternalOutput").ap()
        dbg_t["rt"] = nc.dram_tensor("dbg_rt", [NOWN_TOK, 8], F32, kind="ExternalOutput").ap()

    from contextlib import ExitStack
    scope = [None]

    uniq = [0]

    def phase_begin():
        S.barrier()
        st = ExitStack()
        st._prev = scope[0]
        scope[0] = st
        return st

    def phase_end(st):
        S.barrier()
        scope[0] = st._prev
        st.close()

    def sb(name, shape, dt=F32):
        uniq[0] += 1
        name = "%s_%d" % (name, uniq[0])
        if cfg.get("verbose"):
            print("alloc", name, shape, dt, "remaining", nc.sbuf_bytes_remaining)
        if scope[0] is None:
            return nc.alloc_sbuf_tensor("s_" + name, list(shape), dt)
        return scope[0].enter_context(nc.sbuf_tensor("s_" + name, list(shape), dt))

    PS = nc.alloc_psum_tensor("ps", [128, 8, 512], F32)
    bank_ctr = [0]

    bank_lo = [0]

    def nb():
        b = bank_ctr[0]
        bank_ctr[0] = b + 1 if b + 1 < 8 else bank_lo[0]
        return b

    def nb2():
        b = bank_ctr[0]
        if b % 2:
            b = (b + 1) % 8
        bank_ctr[0] = (b + 2) % 8
        return b

    def pk(b):
        return ("ps", b)

    cst = sb("cst", [128, 1408])
    ident = cst[:, 0:128]
    maskA = cst[:, 128:256]
    maskU = cst[0:64, 256:320]
    maskL = cst[0:64, 320:384]
    ident64 = cst[0:64, 0:64]
    bones = cst[:, 384:512]
    headsel = cst[:, 512:514]
    tri_strict = cst[:, 640:768]
    ones_m = cst[:, 768:896]
    mcur_f = cst[:, 896:1024]
    mprev_f = cst[:, 1024:1152]
    onesL_f = cst[:, 1152:1280]
    onesR_f = cst[:, 1280:1408]
    S.dma("sp", lambda e: e.dma_start(out=cst[:, :], in_=cst_d[:, :]), writes=["cst"])
    ecap = sb("ecap", [128, NE])
    S.dma("sp", lambda e: e.dma_start(out=ecap[:, :], in_=ecap_d[:, :]), writes=["cst"])
    flag = sb("flag", [128, 1])
    S.dma("sp", lambda e: e.dma_start(out=flag[:, :], in_=flag_d[:, :]), writes=["cst"])

    if cfg.get("sstop", 99) <= 1:
        S.barrier(); S.stack.close(); return nc
    rt_w = sb("rt_w", [128, NSUB, 2])
    rt_d = sb("rt_d", [128, NSUB, 2], I32)
    scope[0] = ExitStack()
    NPB = 28
    mu_t = sb("mu_t", [128, NPB])
    omm_t = sb("omm_t", [128, NPB])
    S.op("dve", lambda e: e.memset(mu_t[:, :], 0.0), writes=["mu"])
    for j in range(24):
        S.dma("sp", lambda e, j=j: e.dma_start(out=mu_t[:, j:j + 1], in_=mu_d[j * 128:(j + 1) * 128, :]), writes=["mu"])
    S.dma("sp", lambda e: e.dma_start(out=mu_t[0:96, 24:25], in_=mu_d[C_XW:C_XW + 96, :]), writes=["mu"])
    S.dma("sp", lambda e: e.dma_start(out=mu_t[0:96, 25:26], in_=mu_d[C_XA:C_XA + 96, :]), writes=["mu"])
    for j in range(2):
        S.dma("sp", lambda e, j=j: e.dma_start(out=mu_t[:, 26 + j:27 + j], in_=mu_d[C_XG + j * 128:C_XG + (j + 1) * 128, :]), writes=["mu"])
    S.op("dve", lambda e: e.tensor_scalar(omm_t[:, :], mu_t[:, :], -1.0, 1.0, ALU.mult, ALU.add), reads=["mu"], writes=["omm"])
    if cfg.get("sstop", 99) <= 2:
        S.barrier(); S.stack.close(); return nc
    pch = sb("pch", [128, 5, 8])
    for i, dsrc in enumerate((w0_d, a0_d, kk_d, ka_d, rk_d)):
        for b in range(8):
            S.dma("sp", lambda e, i=i, b=b, dsrc=dsrc: e.dma_start(out=pch[:, i, b:b + 1], in_=dsrc[b * 128:(b + 1) * 128, :]), writes=["pch"])
    ka1 = sb("ka1", [128, 8])
    S.op("dve", lambda e: e.tensor_scalar(ka1[:, :], pch[:, 3, :], -1.0, 1.0, ALU.mult, ALU.add), reads=["pch"], writes=["ka1"])
    if cfg.get("sstop", 99) <= 3:
        S.barrier(); S.stack.close(); return nc
    w2s = sb("w2s", [96, RW])
    a2s = sb("a2s", [96, RW])
    g2s = sb("g2s", [128, 2, RW])
    S.dma("sp", lambda e: e.dma_start(out=w2s[:, :], in_=w2_d[:, :]), writes=["lora"])
    S.dma("sp", lambda e: e.dma_start(out=a2s[:, :], in_=a2_d[:, :]), writes=["lora"])
    S.dma("sp", lambda e: e.dma_start(out=g2s[:, :, :], in_=g2_d.rearrange("(k p) c -> p k c", p=128)), writes=["lora"])
    g1col = sb("g1col", [128, 16])
    g2bc = sb("g2bc", [128, D])
    S.dma("sp", lambda e: e.dma_start(out=g1col[:, :], in_=norm1_w.rearrange("o (k p) -> p (o k)", p=128)), writes=["gbc"])
    S.dma("sp", lambda e: e.dma_start(out=g2bc[:, :], in_=norm2_w.partition_broadcast(128)), writes=["gbc"])
    gnw = sb("gnw", [64, RW])
    gnb = sb("gnb", [64, RW])
    S.dma("sp", lambda e: e.dma_start(out=gnw[:, :], in_=gnw_d.partition_broadcast(64)), writes=["gbc"])
    S.dma("sp", lambda e: e.dma_start(out=gnb[:, :], in_=gnb_d.partition_broadcast(64)), writes=["gbc"])
    if cfg.get("sstop", 99) <= 4:
        S.barrier(); S.stack.close(); return nc
    qkg = sb("qkg", [128, 2])
    for hp in range(2):
        S.dma("sp", lambda e, hp=hp: e.dma_start(out=qkg[hp * 64:(hp + 1) * 64, 0:1], in_=qn_d[:, :]), writes=["gbc"])
        S.dma("sp", lambda e, hp=hp: e.dma_start(out=qkg[hp * 64:(hp + 1) * 64, 1:2], in_=kn_d[:, :]), writes=["gbc"])
    esink = sb("esink", [128, 8])
    for b in range(8):
        for hp in range(2):
            h = 2 * b + hp
            S.dma("sp", lambda e, b=b, hp=hp, h=h: e.dma_start(out=esink[hp * 64:(hp + 1) * 64, b:b + 1], in_=sink_d[h:h + 1, :].partition_broadcast(64)), writes=["esink"])
    S.op("act", lambda e: e.activation(out=esink[:, :], in_=esink[:, :], func=AF.Exp), reads=["esink"], writes=["esink"])
    if cfg.get("sstop", 99) <= 5:
        S.barrier(); S.stack.close(); return nc
    wr = sb("wr", [128, 16, NRL])
    S.op("dve", lambda e: e.memset(wr[:, :, :], 0.0), writes=["wr"])
    S.dma("sp", lambda e: e.dma_start(out=wr[:, :, 0:NG], in_=rcw_d.rearrange("(k p) c -> p k c", p=128)), writes=["wr"])
    S.dma("sp", lambda e: e.dma_start(out=wr[:, :, 8:NRL], in_=rfw_d.rearrange("(k p) c -> p k c", p=128)), writes=["wr"])
    rbias = sb("rbias", [128, NRL])
    S.op("dve", lambda e: e.memset(rbias[:, :], 0.0), writes=["wr"])
    S.dma("sp", lambda e: e.dma_start(out=rbias[:, 0:NG], in_=rcb_d.partition_broadcast(128)), writes=["wr"])
    S.dma("sp", lambda e: e.dma_start(out=rbias[:, 8:NRL], in_=rfb_d.partition_broadcast(128)), writes=["wr"])
    if cfg.get("sstop", 99) <= 6:
        S.barrier(); S.stack.close(); return nc
    mcur = sb("mcur", [128, 4, 128], BF16)
    mprev = sb("mprev", [128, 4, 128], BF16)
    onesL = sb("onesL", [128, 128], BF16)
    onesR = sb("onesR", [128, 128], BF16)
    maskA4 = sb("maskA4", [128, 4, 128])
    for i in range(4):
        S.op("dve", lambda e, i=i: e.tensor_copy(mcur[:, i, :], mcur_f), reads=["cst"], writes=["m1"])
        S.op("dve", lambda e, i=i: e.tensor_copy(mprev[:, i, :], mprev_f), reads=["cst"], writes=["m1"])
        S.op("dve", lambda e, i=i: e.tensor_copy(maskA4[:, i, :], maskA), reads=["cst"], writes=["m1"])
    S.op("dve", lambda e: e.tensor_copy(onesL[:, :], onesL_f), reads=["cst"], writes=["m1"])
    S.op("dve", lambda e: e.tensor_copy(onesR[:, :], onesR_f), reads=["cst"], writes=["m1"])
    ones64 = sb("ones64", [128, 64])
    S.op("dve", lambda e: e.memset(ones64[:, :], 1.0), writes=["m1"])

    if cfg.get("sstop", 99) <= 7:
        S.barrier(); S.stack.close(); return nc
    zt = sb("zt", [128, D], BF16)
    S.op("dve", lambda e: e.memset(zt[:, :], 0.0), writes=["zt"])
    for ex in range(NE):
        S.dma("sp", lambda e, ex=ex: e.dma_start(out=Xd[ex * CAP:(ex + 1) * CAP, :], in_=zt[:, :]), reads=["zt"], writes=[("xdz", ex)])
    xdz_keys = [("xdz", ex) for ex in range(NE)]
    bc_reg = nc.gpsimd.alloc_register("bcreg")
    nc.gpsimd.reg_mov(bc_reg, NE * CAP - 1)
    carry = sb("carry", [128, NPB])
    ST = sb("ST", [128, 8, 64])
    S.op("dve", lambda e: e.memset(carry[:, :], 0.0), writes=["carry"])
    S.op("dve", lambda e: e.memset(ST[:, :, :], 0.0), writes=["ST"])
    STb = sb("STb", [128, 8, 64])
    S.op("dve", lambda e: e.memset(STb[:, :, :], 0.0), writes=["STb"])
    base_bc = sb("base_bc", [128, NE])
    S.op("dve", lambda e: e.memset(base_bc[:, :], 0.0), writes=["base"])
    kTs = sb("kTs", [128, 3, 4, 128], BF16)
    Vpad = sb("Vpad", [128, 3, 4, 2, 128], BF16)
    S.op("dve", lambda e: e.memset(kTs[:, :, :, :], 0.0), writes=["kTs0", "kTs1", "kTs2"])
    S.op("dve", lambda e: e.memset(Vpad[:, :, :, :, :], 0.0), writes=["Vp0", "Vp1", "Vp2"])

    st1 = sb("st1", [128, 8])
    tmp_sh = sb("tmp_sh", [128, TT])
    hT = sb("hT", [128, 16, TT], BF16)
    NWB = 3
    wbuf = [sb("wb%d" % i, [128, 16, 128], BF16) for i in range(NWB)]
    wb_ctr = [0]
    a_outT = sb("a_outT", [128, 8, TT], BF16)
    b_outT = sb("b_outT", [128, 8, TT], BF16)

    WC = nc.dram_tensor("wcache", [160, 128, 2048], BF16).ap()
    wcache = {}
    cq = [0]

    def load_wblock(key, i, K16, fill_fn):
        wb = wbuf[i]
        flat = wb[:, :, :].rearrange("p a b -> p (a b)")[:, 0:K16 * 128]
        if key in wcache:
            idx = wcache[key]
            q = ("sp", "pool")[cq[0] % 2]
            cq[0] += 1
            S.dma(q, lambda e: e.dma_start(out=flat, in_=WC[idx, :, 0:K16 * 128]), reads=[("wc", idx)], writes=[("wb", i)])
        else:
            fill_fn()
            idx = len(wcache)
            wcache[key] = idx
            S.dma("sp", lambda e: e.dma_start(out=WC[idx, :, 0:K16 * 128], in_=flat), reads=[("wb", i)], writes=[("wc", idx)])

    def mm_block(dram_w, col_specs, K16, M, rhs_fn, N, rkeys, wname):
        i = wb_ctr[0]
        wb_ctr[0] = (i + 1) % NWB
        wb = wbuf[i]

        def fill():
            for (c0, n, d0) in col_specs:
                S.dma("pool", lambda e, c0=c0, n=n, d0=d0: e.dma_start(
                    out=wb[:, 0:K16, d0:d0 + n], in_=dram_w[:, c0:c0 + n].rearrange("(kc p) c -> p kc c", p=128)),
                    writes=[("wb", i)])
        load_wblock((wname, tuple(col_specs)), i, K16, fill)
        b = nb()

        def f(e):
            ins = None
            for kc in range(K16):
                ins = e.matmul(PS[0:M, b, 0:N], lhsT=wb[:, kc, 0:M], rhs=rhs_fn(kc), start=(kc == 0), stop=(kc == K16 - 1))
            return ins
        S.op("pe", f, reads=[("wb", i)] + rkeys, writes=[pk(b)])
        return b

    def shift_evac(b, M, pblk, out_ap, okey):
        mu = mu_t[0:M, pblk:pblk + 1]
        om = omm_t[0:M, pblk:pblk + 1]
        S.op("dve", lambda e: e.tensor_scalar(tmp_sh[0:M, :], PS[0:M, b, 0:TT], mu, None, ALU.mult), reads=[pk(b)], writes=["tmp_sh"])
        S.op("dve", lambda e: e.scalar_tensor_tensor(out_ap[:, 1:TT], PS[0:M, b, 1:TT], om, tmp_sh[0:M, 0:TT - 1], ALU.mult, ALU.add),
             reads=[pk(b), "tmp_sh"], writes=[okey])
        S.op("dve", lambda e: e.scalar_tensor_tensor(out_ap[:, 0:1], PS[0:M, b, 0:1], om, carry[0:M, pblk:pblk + 1], ALU.mult, ALU.add),
             reads=[pk(b), "carry"], writes=[okey])
        S.op("dve", lambda e: e.tensor_copy(carry[0:M, pblk:pblk + 1], tmp_sh[0:M, TT - 1:TT]), reads=["tmp_sh"], writes=["carry"])

    hT_rhs = lambda kc: hT[:, kc, :]

    def c3(ap, c):
        return ap.rearrange("p (c t) -> p c t", c=c)

    class Stop(Exception):
        pass
    stop = cfg.get("stop", 99)

    def mixer_tile(ti):
        own = ti >= NPREV
        oi = ti - NPREV
        tok0 = ti * TT
        need_kv = own or (ti == NPREV - 1)
        ph = phase_begin()
        xin = sb("xin", [128, D])
        xn = xin
        junk = sb("junk", [128, D])
        for s in range(NS):
            S.dma("sp", lambda e: e.dma_start(out=xin[:, :], in_=xs[tok0 + s * 128:tok0 + (s + 1) * 128, :]), writes=["xin"])
            S.op("act", lambda e: e.activation(out=junk[:, :], in_=xin[:, :], func=AF.Square, accum_out=st1[:, 0:1]), reads=["xin"], writes=["junk", "st1"])
            S.op("dve", lambda e: e.tensor_scalar(st1[:, 1:2], st1[:, 0:1], 1.0 / D, 1e-6, ALU.mult, ALU.add), reads=["st1"], writes=["st1b"])
            S.op("act", lambda e: e.activation(out=st1[:, 2:3], in_=st1[:, 1:2], func=AF.Sqrt), reads=["st1b"], writes=["st1c"])
            S.op("dve", lambda e: e.reciprocal(st1[:, 2:3], st1[:, 2:3]), reads=["st1c"], writes=["st1c"])
            S.op("dve", lambda e: e.tensor_scalar(xn[:, :], xin[:, :], st1[:, 2:3], None, ALU.mult), reads=["xin", "st1c"], writes=["xin"])
            for g in range(4):
                b = nb()

                def f(e, g=g, b=b):
                    ins = None
                    for j in range(4):
                        kc = g * 4 + j
                        ins = e.transpose(PS[:, b, j * 128:(j + 1) * 128], xn[:, kc * 128:(kc + 1) * 128], ident)
                    return ins
                S.op("pe", f, reads=["xin"], writes=[pk(b)])
                for j in range(4):
                    kc = g * 4 + j
                    S.op("act", lambda e, kc=kc, j=j, b=b: e.activation(out=hT[:, kc, s * 128:(s + 1) * 128], in_=PS[:, b, j * 128:(j + 1) * 128],
                                                                    func=AF.Copy, scale=g1col[:, kc:kc + 1]),
                         reads=[pk(b)], writes=["hT"])
        phase_end(ph)
        if stop <= 1:
            raise Stop()
        ph = phase_begin()
        xwT = sb("xwT", [128, TT])
        xaT = sb("xaT", [128, TT])
        sgT = sb("sgT", [128, 2, TT])
        r_b = sb("r_b", [128, TT])
        k_b = sb("k_b", [128, TT])
        v_b = sb("v_b", [128, TT])
        a_b = sb("a_b", [128, TT])
        sw_b = sb("sw_b", [128, TT])
        cs_b = sb("cs_b", [128, TT])
        csx_b = sb("csx_b", [128, TT])
        e1 = sb("e1", [128, TT])
        e2 = sb("e2", [128, TT])
        e3 = sb("e3", [128, TT])
        e4 = sb("e4", [128, TT])
        nbias = sb("nbias", [128, NCH])
        kq = sb("kq", [128, TT])
        t1 = sb("t1", [128, TT])
        kkn = sb("kkn", [128, TT])
        kmod = sb("kmod", [128, TT])
        bb = sb("bb", [128, TT])
        rkk = sb("rkk", [128, TT])
        AR = sb("AR", [128, 8, NCH, 128])
        ARm = [sb("ARm%d" % i_, [128, 8, NCH, 128]) for i_ in range(2)]
        BK = sb("BK", [128, 8, NCH, 128])
        BKp = sb("BKp", [128, NCH, 128])
        gamC = sb("gamC", [128, 8, NCH])
        VU = sb("VU", [128, NCH, 16, 64])
        bsum = sb("bsum", [64, NCH, 16])
        AT = sb("AT", [128, 16, 128])
        ATK = [("AT", g_) for g_ in range(4)]
        PQ = [sb("PQ%d" % i, [64, 16, 128], BF16) for i in range(2)]
        Tt = [sb("Tt%d" % i, [64, 16, 64], BF16) for i in range(2)]
        Wsb = sb("Wsb", [64, 16, 64])
        ysq = sb("ysq", [64, RW])
        yn = sb("yn", [64, RW])
        gst = sb("gst", [64, 4, 16])
        BKtok = sb("BKtok", [128, NCH, 16, 128])
        TtF = sb("TtF", [64, 16, 128])
        S.op("dve", lambda e: e.memset(BKtok[:, :, :, :], 0.0), writes=["BKtok"])
        S.op("dve", lambda e: e.memset(TtF[:, :, :], 0.0), writes=[("TtF", 0), ("TtF", 1)])
        b = mm_block(w_in, [(C_XW, 96, 0)], 16, 96, hT_rhs, TT, ["hT"], "xw")
        shift_evac(b, 96, 24, xwT[0:96, :], "xwT")
        S.op("act", lambda e: e.activation(out=xwT[0:96, :], in_=xwT[0:96, :], func=AF.Tanh), reads=["xwT"], writes=["xwT"])
        b = mm_block(w_in, [(C_XA, 96, 0)], 16, 96, hT_rhs, TT, ["hT"], "xa")
        shift_evac(b, 96, 25, xaT[0:96, :], "xaT")
        for j in range(2):
            b = mm_block(w_in, [(C_XG + j * 128, 128, 0)], 16, 128, hT_rhs, TT, ["hT"], "xg")
            shift_evac(b, 128, 26 + j, sgT[:, j, :], "sgT")
        S.op("act", lambda e: e.activation(out=sgT[:, :, :], in_=sgT[:, :, :], func=AF.Sigmoid), reads=["sgT"], writes=["sgT"])
        if stop <= 1.2:
            raise Stop()
        for blk in range(8):
            b = mm_block(w_in, [(C_R + blk * 128, 128, 0)], 16, 128, hT_rhs, TT, ["hT"], "r")
            shift_evac(b, 128, blk, r_b[:, :], "r_b")
            b = mm_block(w_in, [(C_K + blk * 128, 128, 0)], 16, 128, hT_rhs, TT, ["hT"], "k")
            shift_evac(b, 128, 8 + blk, k_b[:, :], "k_b")
            b = mm_block(w_in, [(C_V + blk * 128, 128, 0)], 16, 128, hT_rhs, TT, ["hT"], "v")
            shift_evac(b, 128, 16 + blk, v_b[:, :], "v_b")
            b = nb()
            S.op("pe", lambda e, b=b: e.matmul(PS[:, b, 0:TT], lhsT=a2s[:, blk * 128:(blk + 1) * 128], rhs=xaT[0:96, :], start=True, stop=True),
                 reads=["xaT"], writes=[pk(b)])
            S.op("act", lambda e, b=b: e.activation(out=a_b[:, :], in_=PS[:, b, 0:TT], func=AF.Sigmoid, bias=pch[:, 1, blk:blk + 1]),
                 reads=[pk(b)], writes=["a_b"])
            b = nb()
            S.op("pe", lambda e, b=b: e.matmul(PS[:, b, 0:TT], lhsT=w2s[:, blk * 128:(blk + 1) * 128], rhs=xwT[0:96, :], start=True, stop=True),
                 reads=["xwT"], writes=[pk(b)])
            S.op("act", lambda e, b=b: e.activation(out=sw_b[:, :], in_=PS[:, b, 0:TT], func=AF.Sigmoid, bias=pch[:, 0, blk:blk + 1]),
                 reads=[pk(b)], writes=["sw_b"])
            for ch in range(NCH):
                S.op("dve", lambda e, ch=ch: e.tensor_tensor_scan(cs_b[:, ch * CH:(ch + 1) * CH], ones64[:, :], sw_b[:, ch * CH:(ch + 1) * CH], 0.0, ALU.mult, ALU.add),
                     reads=["sw_b"], writes=["cs_b"])
            S.op("dve", lambda e: e.tensor_sub(csx_b[:, :], cs_b[:, :], sw_b[:, :]), reads=["cs_b", "sw_b"], writes=["csx_b"])
            S.op("act", lambda e: e.activation(out=e1[:, :], in_=cs_b[:, :], func=AF.Exp, scale=-DECAY_C), reads=["cs_b"], writes=["e1"])
            S.op("act", lambda e: e.activation(out=e2[:, :], in_=csx_b[:, :], func=AF.Exp, scale=-DECAY_C), reads=["csx_b"], writes=["e2"])
            S.op("act", lambda e: e.activation(out=e3[:, :], in_=cs_b[:, :], func=AF.Exp, scale=DECAY_C), reads=["cs_b"], writes=["e3"])
            S.op("dve", lambda e: e.tensor_scalar(nbias[:, :], c3(cs_b[:, :], NCH)[:, :, CH - 1], -DECAY_C, None, ALU.mult), reads=["cs_b"], writes=["nbias"])
            for ch in range(NCH):
                S.op("act", lambda e, ch=ch: e.activation(out=e4[:, ch * CH:(ch + 1) * CH], in_=cs_b[:, ch * CH:(ch + 1) * CH], func=AF.Exp,
                                                          scale=DECAY_C, bias=nbias[:, ch:ch + 1]), reads=["cs_b", "nbias"], writes=["e4"])
            S.op("dve", lambda e: e.tensor_copy(gamC[:, blk, :], c3(e1[:, :], NCH)[:, :, CH - 1]), reads=["e1"], writes=["gamC"])
            if stop <= 1.3:
                raise Stop()
            S.op("dve", lambda e: e.tensor_scalar(kq[:, :], k_b[:, :], pch[:, 2, blk:blk + 1], None, ALU.mult), reads=["k_b"], writes=["kq"])
            S.op("act", lambda e: e.activation(out=t1[:, :], in_=kq[:, :], func=AF.Square), reads=["kq"], writes=["t1"])
            b = nb()
            S.op("pe", lambda e, b=b: e.matmul(PS[:, b, 0:TT], lhsT=bones, rhs=t1[:, :], start=True, stop=True), reads=["t1"], writes=[pk(b)])
            S.op("dve", lambda e, b=b: e.tensor_scalar(t1[:, :], PS[:, b, 0:TT], 1e-24, None, ALU.max), reads=[pk(b)], writes=["t1"])
            S.op("act", lambda e: e.activation(out=t1[:, :], in_=t1[:, :], func=AF.Sqrt), reads=["t1"], writes=["t1"])
            S.op("dve", lambda e: e.reciprocal(t1[:, :], t1[:, :]), reads=["t1"], writes=["t1"])
            S.op("dve", lambda e: e.tensor_mul(kkn[:, :], kq[:, :], t1[:, :]), reads=["kq", "t1"], writes=["kkn"])
            S.op("dve", lambda e: e.tensor_scalar(t1[:, :], a_b[:, :], pch[:, 3, blk:blk + 1], ka1[:, blk:blk + 1], ALU.mult, ALU.add),
                 reads=["a_b", "kkn"], writes=["t1"])
            S.op("dve", lambda e: e.tensor_mul(kmod[:, :], k_b[:, :], t1[:, :]), reads=["k_b", "t1"], writes=["kmod"])
            S.op("dve", lambda e: e.tensor_mul(bb[:, :], kkn[:, :], a_b[:, :]), reads=["kkn", "a_b"], writes=["bb"])
            S.op("dve", lambda e: e.scalar_tensor_tensor(AR[:, blk, :, 0:64], c3(kkn[:, :], NCH), -1.0, c3(e2[:, :], NCH), ALU.mult, ALU.mult),
                 reads=["kkn", "e2"], writes=["AR"])
            S.op("dve", lambda e: e.tensor_mul(AR[:, blk, :, 64:128], c3(r_b[:, :], NCH), c3(e1[:, :], NCH)), reads=["r_b", "e1"], writes=["AR"])
            for par in range(2):
                S.op("dve", lambda e, par=par: e.tensor_scalar(ARm[par][:, blk, :, :], AR[:, blk, :, :], headsel[:, par:par + 1], None, ALU.mult), reads=["AR"], writes=["ARm"])
            S.op("dve", lambda e: e.tensor_mul(BK[:, blk, :, 0:64], c3(kmod[:, :], NCH), c3(e3[:, :], NCH)), reads=["kmod", "e3"], writes=["BK"])
            S.op("dve", lambda e: e.tensor_mul(BK[:, blk, :, 64:128], c3(bb[:, :], NCH), c3(e3[:, :], NCH)), reads=["bb", "e3"], writes=["BK"])
            S.op("dve", lambda e: e.tensor_mul(BKp[:, :, 0:64], c3(kmod[:, :], NCH), c3(e4[:, :], NCH)), reads=["kmod", "e4"], writes=["BKp"])
            S.op("dve", lambda e: e.tensor_mul(BKp[:, :, 64:128], c3(bb[:, :], NCH), c3(e4[:, :], NCH)), reads=["bb", "e4"], writes=["BKp"])
            if stop <= 1.4:
                raise Stop()
            b = nb()

            def f(e, b=b):
                ins = None
                for ch in range(NCH):
                    ins = e.transpose(PS[:, b, ch * 128:(ch + 1) * 128], BKp[:, ch, :], ident)
                return ins
            S.op("pe", f, reads=["BKp"], writes=[pk(b)])
            for hp in range(2):
                S.op("act", lambda e, b=b, hp=hp: e.activation(
                    out=BKtok[:, :, 2 * blk + hp, hp * 64:(hp + 1) * 64],
                    in_=PS[:, b, 0:NCH * 128].rearrange("p (c h j) -> p c h j", c=NCH, h=2)[:, :, hp, :], func=AF.Copy),
                    reads=[pk(b)], writes=["BKtok"])
            b = nb()

            def f(e, b=b):
                ins = None
                for ch in range(NCH):
                    ins = e.transpose(PS[0:64, b, ch * 128:(ch + 1) * 128], v_b[:, ch * CH:(ch + 1) * CH], ident)
                return ins
            S.op("pe", f, reads=["v_b"], writes=[pk(b)])
            S.op("act", lambda e, b=b: e.activation(out=VU[0:64, :, 2 * blk:2 * blk + 2, :],
                                                    in_=PS[0:64, b, 0:NCH * 128].rearrange("p (c h i) -> p c h i", c=NCH, h=2), func=AF.Copy),
                 reads=[pk(b)], writes=["VUv"])
            if own:
                S.op("dve", lambda e: e.scalar_tensor_tensor(rkk[:, :], r_b[:, :], pch[:, 4, blk:blk + 1], kmod[:, :], ALU.mult, ALU.mult),
                     reads=["r_b", "kmod"], writes=["rkk"])

                b = nb()

                def f(e, b=b):
                    ins = None
                    for ch in range(NCH):
                        ins = e.matmul(PS[0:64, b, ch * 128:(ch + 1) * 128], lhsT=rkk[:, ch * CH:(ch + 1) * CH], rhs=bones, start=True, stop=True)
                    return ins
                S.op("pe", f, reads=["rkk"], writes=[pk(b)])
                S.op("dve", lambda e, b=b: e.tensor_copy(bsum[:, :, 2 * blk:2 * blk + 2], PS[0:64, b, 0:NCH * 128].rearrange("p (c h x) -> p c h x", c=NCH, h=2)[:, :, :, 0]),
                     reads=[pk(b)], writes=["bsum"])
        if stop <= 1.5:
            raise Stop()
        for ch in range(NCH):
            for g4 in range(4):
                b = nb()

                def f(e, b=b, g4=g4):
                    ins = None
                    for j in range(4):
                        h = g4 * 4 + j
                        blk, P0 = h // 2, (h % 2) * 64
                        ins = e.matmul(PS[:, b, j * 128:(j + 1) * 128], lhsT=BK[:, blk, ch, :], rhs=ARm[h % 2][:, blk, ch, :], start=True, stop=True)
                    return ins
                S.op("pe", f, reads=["ARm", "BK"], writes=[pk(b)])
                S.op("dve", lambda e, b=b, g4=g4: e.tensor_mul(AT[:, g4 * 4:(g4 + 1) * 4, :], PS[:, b, :].rearrange("p (h t) -> p h t", h=4), maskA4[:, :, :]),
                     reads=[pk(b)], writes=[("AT", g4)])
            if stop <= 1.6:
                raise Stop()
            for g8 in range(2):
                b = nb2()

                def f(e, b=b, g8=g8):
                    ins = None
                    for j in range(8):
                        h = g8 * 8 + j
                        blk, P0 = h // 2, (h % 2) * 64
                        bb_, off = b + j // 4, (j % 4) * 128
                        e.matmul(PS[0:64, bb_, off:off + 64], lhsT=ARm[h % 2][:, blk, ch, 0:64], rhs=BK[:, blk, ch, 64:128], start=True, stop=True)
                        ins = e.matmul(PS[0:64, bb_, off + 64:off + 128], lhsT=BK[:, blk, ch, 64:128], rhs=ARm[h % 2][:, blk, ch, 0:64], start=True, stop=True)
                    return ins
                S.op("pe", f, reads=["ARm", "BK"], writes=[pk(b), pk(b + 1)])
                for q in range(2):
                    hs = g8 * 8 + q * 4
                    S.op("dve", lambda e, b=b, q=q, hs=hs: e.tensor_mul(PQ[0][:, hs:hs + 4, 0:64], PS[0:64, b + q, :].rearrange("p (h x) -> p h x", h=4)[:, :, 0:64],
                                                                        maskL.unsqueeze(1).to_broadcast([64, 4, 64])), reads=[pk(b + q)], writes=[("PQ0", g8)])
                    S.op("dve", lambda e, b=b, q=q, hs=hs: e.tensor_mul(PQ[0][:, hs:hs + 4, 64:128], PS[0:64, b + q, :].rearrange("p (h x) -> p h x", h=4)[:, :, 64:128],
                                                                        maskU.unsqueeze(1).to_broadcast([64, 4, 64])), reads=[pk(b + q)], writes=[("PQ0", g8)])
                S.op("dve", lambda e, g8=g8: e.tensor_add(Tt[1][:, g8 * 8:(g8 + 1) * 8, :], PQ[0][:, g8 * 8:(g8 + 1) * 8, 64:128],
                                                          ident64.unsqueeze(1).to_broadcast([64, 8, 64])), reads=[("PQ0", g8)], writes=[("Tt1", g8)])
            if stop <= 1.7:
                raise Stop()
            tcur = 1
            for k in range(1, 7):
                src = PQ[(k - 1) % 2]
                dst = PQ[k % 2]
                sk, dk = "PQ%d" % ((k - 1) % 2), "PQ%d" % (k % 2)
                for g8 in range(2):
                    do_pq = k <= 5
                    do_t = k >= 2
                    b = nb2()
                    tsrc, tdst = Tt[tcur], Tt[1 - tcur]

                    def f(e, b=b, g8=g8, src=src, do_pq=do_pq, do_t=do_t, tsrc=tsrc):
                        ins = None
                        for j in range(8):
                            h = g8 * 8 + j
                            bb_, off = b + j // 4, (j % 4) * 128
                            if do_pq:
                                e.matmul(PS[0:64, bb_, off:off + 64], lhsT=src[:, h, 64:128], rhs=src[:, h, 0:64], start=True, stop=True)
                                ins = e.matmul(PS[0:64, bb_, off + 64:off + 128], lhsT=src[:, h, 0:64], rhs=src[:, h, 64:128], start=True, stop=True)
                        return ins
                    if do_pq:
                        S.op("pe", f, reads=[(sk, g8)], writes=[pk(b), pk(b + 1)])
                        for q in range(2):
                            hs = g8 * 8 + q * 4
                            S.op("act", lambda e, b=b, q=q, hs=hs, dst=dst: e.activation(out=dst[:, hs:hs + 4, :], in_=PS[0:64, b + q, :].rearrange("p (h x) -> p h x", h=4), func=AF.Copy),
                                 reads=[pk(b + q)], writes=[(dk, g8)])
                    if do_t:
                        b2 = nb()
                        def f2(e, b2=b2, g8=g8, src=src, tsrc=tsrc):
                            ins = None
                            for j in range(8):
                                h = g8 * 8 + j
                                ins = e.matmul(PS[0:64, b2, j * 64:(j + 1) * 64], lhsT=src[:, h, 0:64], rhs=tsrc[:, h, :], start=True, stop=True)
                            return ins
                        S.op("pe", f2, reads=[(sk, g8), ("Tt%d" % tcur, g8)], writes=[pk(b2)])
                        if k < 6:
                            S.op("dve", lambda e, b2=b2, g8=g8, tsrc=tsrc, tdst=tdst: e.tensor_add(
                                tdst[:, g8 * 8:(g8 + 1) * 8, :], tsrc[:, g8 * 8:(g8 + 1) * 8, :], PS[0:64, b2, :].rearrange("p (h x) -> p h x", h=8)),
                                reads=[pk(b2), ("Tt%d" % tcur, g8)], writes=[("Tt%d" % (1 - tcur), g8)])
                        else:
                            S.op("dve", lambda e, b2=b2, g8=g8, tsrc=tsrc: e.tensor_add(
                                TtF[:, g8 * 8:(g8 + 1) * 8, 64:128], tsrc[:, g8 * 8:(g8 + 1) * 8, :], PS[0:64, b2, :].rearrange("p (h x) -> p h x", h=8)),
                                reads=[pk(b2), ("Tt%d" % tcur, g8)], writes=[("TtF", g8)])
                if k >= 2:
                    tcur = 1 - tcur
            if stop <= 1.8:
                raise Stop()
            bw = nb2()

            def f(e, bw=bw):
                ins = None
                for h in range(16):
                    blk, P0 = h // 2, (h % 2) * 64
                    bb_, off = bw + h // 8, (h % 8) * 64
                    e.matmul(PS[0:64, bb_, off:off + 64], lhsT=ARm[h % 2][:, blk, ch, 0:64], rhs=STb[:, blk, :], start=True, stop=False)
                    ins = e.matmul(PS[0:64, bb_, off:off + 64], lhsT=AT[0:64, h, 0:64], rhs=VU[0:64, ch, h, :], start=False, stop=True)
                return ins
            S.op("pe", f, reads=["ARm", "STb", "VUv"] + [("AT", g) for g in range(4)], writes=[pk(bw), pk(bw + 1)])
            for q in range(2):
                S.op("dve", lambda e, q=q, bw=bw: e.tensor_copy(Wsb[:, q * 8:(q + 1) * 8, :], PS[0:64, bw + q, :].rearrange("p (h x) -> p h x", h=8)),
                     reads=[pk(bw + q)], writes=[("Wsb", q)])
            bu = nb2()

            def f(e, bu=bu):
                ins = None
                for h in range(16):
                    bb_, off = bu + h // 8, (h % 8) * 64
                    ins = e.matmul(PS[:, bb_, off:off + 64], lhsT=TtF[:, h, :], rhs=Wsb[:, h, :], start=True, stop=True)
                return ins
            S.op("pe", f, reads=[("Wsb", 0), ("Wsb", 1), ("TtF", 0), ("TtF", 1)], writes=[pk(bu), pk(bu + 1)])
            for q in range(2):
                S.op("dve", lambda e, q=q, bu=bu: e.tensor_copy(VU[64:128, ch, q * 8:(q + 1) * 8, :], PS[64:128, bu + q, :].rearrange("p (h x) -> p h x", h=8)),
                     reads=[pk(bu + q)], writes=[("VUu", q)])
            vu_keys = ["VUv", ("VUu", 0), ("VUu", 1)]
            if own:
                by = nb2()

                def f(e, by=by):
                    ins = None
                    for h in range(16):
                        blk, P0 = h // 2, (h % 2) * 64
                        bb_, off = by + h // 8, (h % 8) * 64
                        e.matmul(PS[0:64, bb_, off:off + 64], lhsT=AT[:, h, 64:128], rhs=VU[:, ch, h, :], start=True, stop=False)
                        ins = e.matmul(PS[0:64, bb_, off:off + 64], lhsT=ARm[h % 2][:, blk, ch, 64:128], rhs=STb[:, blk, :], start=False, stop=True)
                    return ins
                S.op("pe", f, reads=["ARm", "STb"] + vu_keys + [("AT", g) for g in range(4)], writes=[pk(by), pk(by + 1)])
            if stop <= 1.9:
                raise Stop()
            bs = nb()

            def f(e, bs=bs):
                ins = None
                for blk in range(8):
                    e.matmul(PS[:, bs, blk * 64:(blk + 1) * 64], lhsT=BKtok[:, ch, 2 * blk, :], rhs=VU[:, ch, 2 * blk, :], start=True, stop=False)
                    ins = e.matmul(PS[:, bs, blk * 64:(blk + 1) * 64], lhsT=BKtok[:, ch, 2 * blk + 1, :], rhs=VU[:, ch, 2 * blk + 1, :], start=False, stop=True)
                return ins
            S.op("pe", f, reads=["BKtok"] + vu_keys, writes=[pk(bs)])
            for blk in range(8):
                S.op("dve", lambda e, blk=blk, bs=bs: e.scalar_tensor_tensor(ST[:, blk, :], ST[:, blk, :], gamC[:, blk, ch:ch + 1], PS[:, bs, blk * 64:(blk + 1) * 64], ALU.mult, ALU.add),
                     reads=[pk(bs), "gamC", "ST"], writes=["ST"])
            S.op("act", lambda e: e.activation(out=STb[:, :, :], in_=ST[:, :, :], func=AF.Copy), reads=["ST"], writes=["STb"])
            if own:
                for q in range(2):
                    ysrc = PS[0:64, by + q, :].rearrange("p (h x) -> p h x", h=8)
                    hs = slice(q * 8, (q + 1) * 8)
                    S.op("dve", lambda e, ysrc=ysrc, hs=hs: e.tensor_reduce(gst[:, 0, hs], ysrc, AX.X, ALU.add), reads=[pk(by + q)], writes=[("gst0", q)])
                    S.op("act", lambda e, q=q: e.activation(out=ysq[:, q * 512:(q + 1) * 512], in_=PS[0:64, by + q, :], func=AF.Square), reads=[pk(by + q)], writes=[("ysq", q)])
                    S.op("dve", lambda e, q=q, hs=hs: e.tensor_reduce(gst[:, 1, hs], ysq[:, q * 512:(q + 1) * 512].rearrange("p (h x) -> p h x", h=8), AX.X, ALU.add),
                         reads=[("ysq", q)], writes=[("gst1", q)])
                gk = [("gst0", 0), ("gst0", 1), ("gst1", 0), ("gst1", 1)]
                S.op("dve", lambda e: e.tensor_scalar(gst[:, 0, :], gst[:, 0, :], 1.0 / 64, None, ALU.mult), reads=gk, writes=["gstm"])
                S.op("dve", lambda e: e.tensor_mul(gst[:, 2, :], gst[:, 0, :], gst[:, 0, :]), reads=["gstm"], writes=["gst2"])
                S.op("dve", lambda e: e.scalar_tensor_tensor(gst[:, 3, :], gst[:, 1, :], 1.0 / 64, gst[:, 2, :], ALU.mult, ALU.subtract), reads=gk + ["gst2"], writes=["gst3"])
                S.op("dve", lambda e: e.tensor_scalar(gst[:, 3, :], gst[:, 3, :], 64e-5, None, ALU.add), reads=["gst3"], writes=["gst3"])
                S.op("act", lambda e: e.activation(out=gst[:, 3, :], in_=gst[:, 3, :], func=AF.Sqrt), reads=["gst3"], writes=["gst3"])
                S.op("dve", lambda e: e.reciprocal(gst[:, 3, :], gst[:, 3, :]), reads=["gst3"], writes=["gst3"])
                for q in range(2):
                    ysrc = PS[0:64, by + q, :].rearrange("p (h x) -> p h x", h=8)
                    hs = slice(q * 8, (q + 1) * 8)
                    yd = yn[:, q * 512:(q + 1) * 512].rearrange("p (h x) -> p h x", h=8)
                    S.op("dve", lambda e, ysrc=ysrc, hs=hs, yd=yd: e.tensor_sub(yd, ysrc, gst[:, 0, hs].unsqueeze(2).to_broadcast([64, 8, 64])),
                         reads=[pk(by + q), "gstm"], writes=[("yn", q)])
                    S.op("dve", lambda e, hs=hs, yd=yd: e.tensor_mul(yd, yd, gst[:, 3, hs].unsqueeze(2).to_broadcast([64, 8, 64])),
                         reads=["gst3", ("yn", q)], writes=[("yn", q)])
                ynk = [("yn", 0), ("yn", 1)]
                S.op("dve", lambda e: e.tensor_mul(yn[:, :], yn[:, :], gnw[:, :]), reads=ynk, writes=ynk)
                S.op("dve", lambda e: e.tensor_add(yn[:, :], yn[:, :], gnb[:, :]), reads=ynk, writes=ynk)
                S.op("dve", lambda e: e.tensor_mul(ysq[:, :].rearrange("p (h x) -> p h x", h=16), VU[0:64, ch, :, :], bsum[:, ch, :].unsqueeze(2).to_broadcast([64, 16, 64])),
                     reads=["VUv", "bsum", ("ysq", 0), ("ysq", 1)], writes=[("ysq", 0), ("ysq", 1)])
                S.op("dve", lambda e: e.tensor_add(yn[:, :], yn[:, :], ysq[:, :]), reads=ynk + [("ysq", 0), ("ysq", 1)], writes=ynk)
                bg = nb2()

                def f(e, bg=bg):
                    ins = None
                    for half in range(2):
                        for kk_ in range(2):
                            ins = e.matmul(PS[0:64, bg + half, :], lhsT=sgT[:, kk_, ch * CH:(ch + 1) * CH], rhs=g2s[:, kk_, half * 512:(half + 1) * 512], start=(kk_ == 0), stop=(kk_ == 1))
                    return ins
                S.op("pe", f, reads=["sgT"], writes=[pk(bg), pk(bg + 1)])
                for half in range(2):
                    S.op("dve", lambda e, half=half, bg=bg: e.tensor_mul(yn[:, half * 512:(half + 1) * 512], yn[:, half * 512:(half + 1) * 512], PS[0:64, bg + half, :]),
                         reads=[pk(bg + half)] + ynk, writes=ynk)
                bt = nb()

                def f(e, bt=bt):
                    ins = None
                    for blk in range(8):
                        ins = e.transpose(PS[:, bt, blk * 64:(blk + 1) * 64], yn[:, blk * 128:(blk + 1) * 128], ident64)
                    return ins
                S.op("pe", f, reads=ynk, writes=[pk(bt)])
                S.op("act", lambda e, bt=bt: e.activation(out=a_outT[:, :, ch * CH:(ch + 1) * CH], in_=PS[:, bt, :].rearrange("p (b t) -> p b t", b=8), func=AF.Copy),
                     reads=[pk(bt)], writes=["a_outT"])
                if dbg:
                    S.op("dve", lambda e, bt=bt: e.tensor_copy(junk[:, 0:512], PS[:, bt, :]), reads=[pk(bt)], writes=["junk"])
                    for blk in range(8):
                        S.dma("sp", lambda e, blk=blk: e.dma_start(out=dbg_t["aout"][blk * 128:(blk + 1) * 128, oi * TT + ch * CH:oi * TT + (ch + 1) * CH],
                                                                    in_=junk[:, blk * 64:(blk + 1) * 64]), reads=["junk"])
        phase_end(ph)
        if stop <= 2 or (own and stop <= 2.5):
            raise Stop()
        ph = phase_begin()
        qsq = sb("qsq", [128, TT])
        qrs = sb("qrs", [128, TT])
        qT = sb("qT", [128, 8, TT], BF16)
        qTm = [sb("qTm%d" % i_, [128, 8, TT], BF16) for i_ in range(2)]
        Ecur = sb("Ecur", [128, 16, 128], BF16)
        Eprev = sb("Eprev", [128, 16, 128], BF16)
        den = sb("den", [128, 4, 128])
        if need_kv:
            for s in range(NS):
                kb = ti * NS + s
                sl = kb % 3
                for g in range(4):
                    b = mm_block(w_in, [(C_KA + g * 64, 64, 0), (C_KA + g * 64, 64, 64)], 16, 128, lambda kc: hT[:, kc, s * 128:(s + 1) * 128], 128, ["hT"], "ka")
                    S.op("act", lambda e, b=b: e.activation(out=qsq[:, 0:128], in_=PS[:, b, 0:128], func=AF.Square), reads=[pk(b)], writes=["qsq"])
                    b2 = nb()
                    S.op("pe", lambda e, b2=b2: e.matmul(PS[:, b2, 0:128], lhsT=bones, rhs=qsq[:, 0:128], start=True, stop=True), reads=["qsq"], writes=[pk(b2)])
                    S.op("dve", lambda e, b2=b2: e.tensor_scalar(qrs[:, 0:128], PS[:, b2, 0:128], 1.0 / 64, 1e-6, ALU.mult, ALU.add), reads=[pk(b2)], writes=["qrs"])
                    S.op("act", lambda e: e.activation(out=qrs[:, 0:128], in_=qrs[:, 0:128], func=AF.Sqrt), reads=["qrs"], writes=["qrs"])
                    S.op("dve", lambda e: e.reciprocal(qrs[:, 0:128], qrs[:, 0:128]), reads=["qrs"], writes=["qrs"])
                    S.op("dve", lambda e, b=b, g=g, sl=sl: e.scalar_tensor_tensor(kTs[:, sl, g, :], PS[:, b, 0:128], qkg[:, 1:2], qrs[:, 0:128], ALU.mult, ALU.mult),
                         reads=[pk(b), "qrs"], writes=["kTs%d" % sl])
                wi = wb_ctr[0]
                wb_ctr[0] = (wi + 1) % NWB
                wb = wbuf[wi]
                for hf in range(2):
                    load_wblock(("va", hf), wi, 16, lambda hf=hf, wb=wb, wi=wi: S.dma("pool", lambda e: e.dma_start(
                        out=wb[:, :, 0:128], in_=w_in[:, C_VA + hf * 128:C_VA + (hf + 1) * 128].rearrange("(kc p) c -> p kc c", p=128)), writes=[("wb", wi)]))
                    b = nb()

                    def f(e, b=b, wb=wb):
                        ins = None
                        for kc in range(16):
                            ins = e.matmul(PS[:, b, 0:128], lhsT=hT[:, kc, s * 128:(s + 1) * 128], rhs=wb[:, kc, 0:128], start=(kc == 0), stop=(kc == 15))
                        return ins
                    S.op("pe", f, reads=[("wb", wi), "hT"], writes=[pk(b)])
                    for gg in range(2):
                        g = hf * 2 + gg
                        S.op("act", lambda e, b=b, g=g, gg=gg, sl=sl: e.activation(out=Vpad[:, sl, g, 0, 0:64], in_=PS[:, b, gg * 64:(gg + 1) * 64], func=AF.Copy),
                             reads=[pk(b)], writes=["Vp%d" % sl])
                        S.op("act", lambda e, b=b, g=g, gg=gg, sl=sl: e.activation(out=Vpad[:, sl, g, 1, 64:128], in_=PS[:, b, gg * 64:(gg + 1) * 64], func=AF.Copy),
                             reads=[pk(b)], writes=["Vp%d" % sl])
        if own and stop <= 2.7:
            raise Stop()
        if own:
            for qb in range(8):
                b = mm_block(w_in, [(C_Q + qb * 128, 128, 0)], 16, 128, hT_rhs, TT, ["hT"], "q")
                S.op("act", lambda e, b=b: e.activation(out=qsq[:, :], in_=PS[:, b, 0:TT], func=AF.Square), reads=[pk(b)], writes=["qsq"])
                b2 = nb()
                S.op("pe", lambda e, b2=b2: e.matmul(PS[:, b2, 0:TT], lhsT=bones, rhs=qsq[:, :], start=True, stop=True), reads=["qsq"], writes=[pk(b2)])
                S.op("dve", lambda e, b2=b2: e.tensor_scalar(qrs[:, :], PS[:, b2, 0:TT], 1.0 / 64, 1e-6, ALU.mult, ALU.add), reads=[pk(b2)], writes=["qrs"])
                S.op("act", lambda e: e.activation(out=qrs[:, :], in_=qrs[:, :], func=AF.Sqrt), reads=["qrs"], writes=["qrs"])
                S.op("dve", lambda e: e.reciprocal(qrs[:, :], qrs[:, :]), reads=["qrs"], writes=["qrs"])
                S.op("dve", lambda e, b=b, qb=qb: e.scalar_tensor_tensor(qT[:, qb, :], PS[:, b, 0:TT], qkg[:, 0:1], qrs[:, :], ALU.mult, ALU.mult),
                     reads=[pk(b), "qrs"], writes=["qT"])
                for par in range(2):
                    S.op("dve", lambda e, par=par, qb=qb: e.tensor_scalar(qTm[par][:, qb, :], qT[:, qb, :], headsel[:, par:par + 1], None, ALU.mult), reads=["qT"], writes=["qTm"])
            for s in range(NS):
                kb = ti * NS + s
                slc, slp = kb % 3, (kb - 1) % 3
                first = (oi == 0 and s == 0)
                for g4 in range(4):
                    for (sl, Ed, msk, nm) in ((slp, Eprev, mprev, "Ep"), (slc, Ecur, mcur, "Ec")):
                        b = nb()

                        def f(e, b=b, g4=g4, sl=sl):
                            ins = None
                            for j in range(4):
                                h = g4 * 4 + j
                                P0 = (h % 2) * 64
                                ins = e.matmul(PS[:, b, j * 128:(j + 1) * 128], lhsT=kTs[:, sl, h // 4, :], rhs=qTm[h % 2][:, h // 2, s * 128:(s + 1) * 128], start=True, stop=True)
                            return ins
                        S.op("pe", f, reads=["kTs%d" % sl, "qTm"], writes=[pk(b)])
                        S.op("act", lambda e, b=b, g4=g4, Ed=Ed: e.activation(out=Ed[:, g4 * 4:(g4 + 1) * 4, :], in_=PS[:, b, :].rearrange("p (h q) -> p h q", h=4), func=AF.Exp, scale=0.125),
                             reads=[pk(b)], writes=[(nm, g4)])
                        S.op("dve", lambda e, g4=g4, Ed=Ed, msk=msk: e.tensor_mul(Ed[:, g4 * 4:(g4 + 1) * 4, :], Ed[:, g4 * 4:(g4 + 1) * 4, :], msk[:, :, :]),
                             reads=[(nm, g4)], writes=[(nm, g4)])
                        if first and nm == "Ep":
                            S.op("dve", lambda e, g4=g4, Ed=Ed: e.tensor_scalar(Ed[:, g4 * 4:(g4 + 1) * 4, :], Ed[:, g4 * 4:(g4 + 1) * 4, :], flag[:, 0:1], None, ALU.mult),
                                 reads=[(nm, g4)], writes=[(nm, g4)])
                ek = [("Ep", g) for g in range(4)] + [("Ec", g) for g in range(4)]
                for half in range(2):
                    bo = nb()
                    bd = nb()

                    def f(e, bo=bo, bd=bd, half=half):
                        ins = None
                        for jb in range(4):
                            pb = half * 4 + jb
                            g = pb // 2
                            o = PS[:, bo, jb * 128:(jb + 1) * 128]
                            d = PS[:, bd, jb * 128:(jb + 1) * 128]
                            e.matmul(o, lhsT=Vpad[:, slp, g, 0, :], rhs=Eprev[:, 2 * pb, :], start=True, stop=False)
                            e.matmul(o, lhsT=Vpad[:, slp, g, 1, :], rhs=Eprev[:, 2 * pb + 1, :], start=False, stop=False)
                            e.matmul(o, lhsT=Vpad[:, slc, g, 0, :], rhs=Ecur[:, 2 * pb, :], start=False, stop=False)
                            e.matmul(o, lhsT=Vpad[:, slc, g, 1, :], rhs=Ecur[:, 2 * pb + 1, :], start=False, stop=True)
                            e.matmul(d, lhsT=onesL[:, :], rhs=Eprev[:, 2 * pb, :], start=True, stop=False)
                            e.matmul(d, lhsT=onesR[:, :], rhs=Eprev[:, 2 * pb + 1, :], start=False, stop=False)
                            e.matmul(d, lhsT=onesL[:, :], rhs=Ecur[:, 2 * pb, :], start=False, stop=False)
                            ins = e.matmul(d, lhsT=onesR[:, :], rhs=Ecur[:, 2 * pb + 1, :], start=False, stop=True)
                        return ins
                    S.op("pe", f, reads=ek + ["Vp%d" % slp, "Vp%d" % slc], writes=[pk(bo), pk(bd)])
                    S.op("dve", lambda e, bd=bd, half=half: e.tensor_add(den[:, :, :], PS[:, bd, :].rearrange("p (b q) -> p b q", b=4),
                                                                         esink[:, half * 4:(half + 1) * 4].unsqueeze(2).to_broadcast([128, 4, 128])),
                         reads=[pk(bd), "esink"], writes=["den"])
                    S.op("dve", lambda e: e.reciprocal(den[:, :, :], den[:, :, :]), reads=["den"], writes=["den"])
                    S.op("dve", lambda e, bo=bo, half=half: e.tensor_mul(b_outT[:, half * 4:(half + 1) * 4, s * 128:(s + 1) * 128], PS[:, bo, :].rearrange("p (b q) -> p b q", b=4), den[:, :, :]),
                         reads=[pk(bo), "den"], writes=["b_outT"])
            if dbg:
                for blk in range(8):
                    S.op("dve", lambda e, blk=blk: e.tensor_copy(junk[:, 0:TT], b_outT[:, blk, :]), reads=["b_outT"], writes=["junk"])
                    S.dma("sp", lambda e, blk=blk: e.dma_start(out=dbg_t["bout"][blk * 128:(blk + 1) * 128, oi * TT:(oi + 1) * TT], in_=junk[:, 0:TT]), reads=["junk"])
        phase_end(ph)
        if not own:
            return
        if stop <= 3:
            raise Stop()
        ph = phase_begin()
        xin = sb("xin", [128, D])
        xn = xin
        junk = sb("junk", [128, D])
        sga = sb("sga", [128, TT])
        sgb = sb("sgb", [128, TT])
        mt = sb("mt", [128, TT])
        mergedT = sb("mergedT", [128, 16, TT], BF16)
        x1 = xn
        h2 = junk
        h2bf = sb("h2bf", [128, D], BF16)
        h2T = sb("h2T", [128, 16, 128])
        lg = sb("lg", [128, NRL])
        rsm = sb("rsm", [128, 16])
        gmask = sb("gmask", [128, 8])
        fsel3 = sb("fsel3", [128, NG, 8])
        fsel = sb("fsel", [128, 8])
        fm1 = sb("fm1", [128, 8])
        fm2 = sb("fm2", [128, 8])
        fmk = sb("fmk", [128, 8])
        M1 = sb("M1", [128, NG, 8])
        M2 = sb("M2", [128, NG, 8])
        Mt = sb("Mt", [128, NG, 8])
        posT = sb("posT", [128, NE])
        ptmp = sb("ptmp", [128, NE])
        dstf = sb("dstf", [128, 4])
        for c in range(16):
            b = mm_block(w_in, [(C_GA + c * 128, 128, 0)], 16, 128, hT_rhs, TT, ["hT"], "ga")
            S.op("act", lambda e, b=b: e.activation(out=sga[:, :], in_=PS[:, b, 0:TT], func=AF.Sigmoid), reads=[pk(b)], writes=["sga"])
            b = mm_block(w_in, [(C_GB + c * 128, 128, 0)], 16, 128, hT_rhs, TT, ["hT"], "gb")
            S.op("act", lambda e, b=b: e.activation(out=sgb[:, :], in_=PS[:, b, 0:TT], func=AF.Sigmoid), reads=[pk(b)], writes=["sgb"])
            b = mm_block(proj_r, [(c * 128, 128, 0)], 8, 128, lambda kc: a_outT[:, kc, :], TT, ["a_outT"], "pr")
            S.op("dve", lambda e, b=b: e.tensor_mul(mt[:, :], sga[:, :], PS[:, b, 0:TT]), reads=[pk(b), "sga"], writes=["mt"])
            b = mm_block(proj_a, [(c * 128, 128, 0)], 8, 128, lambda kc: b_outT[:, kc, :], TT, ["b_outT"], "pa")
            S.op("dve", lambda e, b=b: e.tensor_mul(sgb[:, :], sgb[:, :], PS[:, b, 0:TT]), reads=[pk(b), "sgb"], writes=["sgb"])
            S.op("dve", lambda e, c=c: e.tensor_add(mergedT[:, c, :], mt[:, :], sgb[:, :]), reads=["mt", "sgb"], writes=["mergedT"])
        for s in range(NS):
            st_i = oi * NS + s
            row0 = oi * TT + s * 128
            S.dma("sp", lambda e: e.dma_start(out=xin[:, :], in_=xs[tok0 + s * 128:tok0 + (s + 1) * 128, :]), writes=["xin"])
            for n in range(4):
                bo = nb()
                for j in range(4):
                    c = n * 4 + j
                    wi = wb_ctr[0]
                    wb_ctr[0] = (wi + 1) % NWB
                    wb = wbuf[wi]
                    load_wblock(("wo", c), wi, 16, lambda c=c, wb=wb, wi=wi: S.dma("pool", lambda e: e.dma_start(
                        out=wb[:, :, :], in_=w_out[:, c * 128:(c + 1) * 128].rearrange("(kc p) c -> p kc c", p=128)), writes=[("wb", wi)]))

                    def f(e, wb=wb, j=j, bo=bo):
                        ins = None
                        for kc in range(16):
                            ins = e.matmul(PS[:, bo, j * 128:(j + 1) * 128], lhsT=mergedT[:, kc, s * 128:(s + 1) * 128], rhs=wb[:, kc, :], start=(kc == 0), stop=(kc == 15))
                        return ins
                    S.op("pe", f, reads=[("wb", wi), "mergedT"], writes=[pk(bo)])
                S.op("dve", lambda e, bo=bo, n=n: e.tensor_add(x1[:, n * 512:(n + 1) * 512], PS[:, bo, :], xin[:, n * 512:(n + 1) * 512]),
                     reads=[pk(bo), "xin"], writes=["xin"])
            S.dma("sp", lambda e: e.dma_start(out=y_out[row0:row0 + 128, :], in_=x1[:, :]), reads=["xin"], writes=[("yrow", st_i)])
            if dbg:
                S.dma("sp", lambda e: e.dma_start(out=dbg_t["xin"][row0:row0 + 128, :], in_=x1[:, :]), reads=["xin"])
            S.op("act", lambda e: e.activation(out=junk[:, :], in_=x1[:, :], func=AF.Square, accum_out=st1[:, 4:5]), reads=["xin"], writes=["junk", "st1d"])
            S.op("dve", lambda e: e.tensor_scalar(st1[:, 5:6], st1[:, 4:5], 1.0 / D, 1e-6, ALU.mult, ALU.add), reads=["st1d"], writes=["st1e"])
            S.op("act", lambda e: e.activation(out=st1[:, 6:7], in_=st1[:, 5:6], func=AF.Sqrt), reads=["st1e"], writes=["st1f"])
            S.op("dve", lambda e: e.reciprocal(st1[:, 6:7], st1[:, 6:7]), reads=["st1f"], writes=["st1f"])
            S.op("dve", lambda e: e.scalar_tensor_tensor(h2[:, :], x1[:, :], st1[:, 6:7], g2bc[:, :], ALU.mult, ALU.mult), reads=["xin", "st1f"], writes=["junk"])
            S.op("act", lambda e: e.activation(out=h2bf[:, :], in_=h2[:, :], func=AF.Copy), reads=["junk"], writes=["h2bf"])
            for g in range(4):
                b = nb()

                def f(e, g=g, b=b):
                    ins = None
                    for j in range(4):
                        kc = g * 4 + j
                        ins = e.transpose(PS[:, b, j * 128:(j + 1) * 128], h2[:, kc * 128:(kc + 1) * 128], ident)
                    return ins
                S.op("pe", f, reads=["junk"], writes=[pk(b)])
                S.op("act", lambda e, g=g, b=b: e.activation(out=h2T[:, g * 4:(g + 1) * 4, :], in_=PS[:, b, :].rearrange("p (j t) -> p j t", j=4), func=AF.Copy),
                     reads=[pk(b)], writes=ATK)
            b = nb()

            def f(e, b=b):
                ins = None
                for kc in range(16):
                    ins = e.matmul(PS[:, b, 0:NRL], lhsT=h2T[:, kc, :], rhs=wr[:, kc, :], start=(kc == 0), stop=(kc == 15))
                return ins
            S.op("pe", f, reads=ATK, writes=[pk(b)])
            S.op("dve", lambda e, b=b: e.tensor_add(lg[:, :], PS[:, b, 0:NRL], rbias[:, :]), reads=[pk(b)], writes=["lg"])
            R = "rt"
            S.op("dve", lambda e: e.tensor_reduce(rsm[:, 0:1], lg[:, 0:NG], AX.X, ALU.max), reads=["lg"], writes=[R])
            S.op("dve", lambda e: e.tensor_scalar(gmask[:, 0:NG], lg[:, 0:NG], rsm[:, 0:1], None, ALU.is_equal), reads=["lg", R], writes=[R])
            S.op("dve", lambda e: e.tensor_scalar(rsm[:, 1:2], rsm[:, 0:1], -1.0, None, ALU.mult), reads=[R], writes=[R])
            S.op("act", lambda e: e.activation(out=fm1[:, 0:NG], in_=lg[:, 0:NG], func=AF.Exp, bias=rsm[:, 1:2], accum_out=rsm[:, 2:3]), reads=["lg", R], writes=[R])
            S.op("dve", lambda e: e.reciprocal(rsm[:, 3:4], rsm[:, 2:3]), reads=[R], writes=[R])
            S.op("dve", lambda e: e.tensor_mul(fsel3[:, :, :], lg[:, 8:NRL].rearrange("p (g x) -> p g x", g=NG), gmask[:, 0:NG].unsqueeze(2).to_broadcast([128, NG, 8])),
                 reads=["lg", R], writes=[R])
            S.op("dve", lambda e: e.tensor_reduce(fsel[:, :], fsel3[:, :, :].rearrange("p g x -> p x g"), AX.X, ALU.add), reads=[R], writes=[R])
            S.op("dve", lambda e: e.tensor_reduce(rsm[:, 4:5], fsel[:, :], AX.X, ALU.max), reads=[R], writes=[R])
            S.op("dve", lambda e: e.tensor_scalar(fm1[:, :], fsel[:, :], rsm[:, 4:5], None, ALU.is_equal), reads=[R], writes=[R])
            S.op("dve", lambda e: e.scalar_tensor_tensor(fmk[:, :], fm1[:, :], -1e30, fsel[:, :], ALU.mult, ALU.add), reads=[R], writes=[R])
            S.op("dve", lambda e: e.tensor_reduce(rsm[:, 5:6], fmk[:, :], AX.X, ALU.max), reads=[R], writes=[R])
            S.op("dve", lambda e: e.tensor_scalar(fm2[:, :], fmk[:, :], rsm[:, 5:6], None, ALU.is_equal), reads=[R], writes=[R])
            S.op("dve", lambda e: e.tensor_sub(rsm[:, 6:7], rsm[:, 5:6], rsm[:, 4:5]), reads=[R], writes=[R])
            S.op("act", lambda e: e.activation(out=rsm[:, 7:8], in_=rsm[:, 6:7], func=AF.Exp), reads=[R], writes=[R])
            S.op("dve", lambda e: e.tensor_scalar(rsm[:, 8:9], rsm[:, 7:8], 1.0, None, ALU.add), reads=[R], writes=[R])
            S.op("dve", lambda e: e.reciprocal(rsm[:, 8:9], rsm[:, 8:9]), reads=[R], writes=[R])
            S.op("dve", lambda e: e.tensor_mul(rsm[:, 9:10], rsm[:, 8:9], rsm[:, 3:4]), reads=[R], writes=[R])
            S.op("dve", lambda e: e.tensor_mul(rsm[:, 10:11], rsm[:, 9:10], rsm[:, 7:8]), reads=[R], writes=[R])
            S.op("dve", lambda e: e.tensor_mul(M1[:, :, :], gmask[:, 0:NG].unsqueeze(2).to_broadcast([128, NG, 8]), fm1[:, :].unsqueeze(1).to_broadcast([128, NG, 8])), reads=[R], writes=[R])
            S.op("dve", lambda e: e.tensor_mul(M2[:, :, :], gmask[:, 0:NG].unsqueeze(2).to_broadcast([128, NG, 8]), fm2[:, :].unsqueeze(1).to_broadcast([128, NG, 8])), reads=[R], writes=[R])
            S.op("dve", lambda e: e.tensor_add(Mt[:, :, :], M1[:, :, :], M2[:, :, :]), reads=[R], writes=["Mt"])
            Mt2 = Mt[:, :, :].rearrange("p g x -> p (g x)")
            b = nb()
            S.op("pe", lambda e, b=b: e.matmul(PS[:, b, 0:NE], lhsT=tri_strict, rhs=Mt2, start=True, stop=True), reads=["Mt"], writes=[pk(b)])
            S.op("dve", lambda e, b=b: e.tensor_add(posT[:, :], PS[:, b, 0:NE], base_bc[:, :]), reads=[pk(b), "base"], writes=["posT"])
            b = nb()
            S.op("pe", lambda e, b=b: e.matmul(PS[:, b, 0:NE], lhsT=ones_m, rhs=Mt2, start=True, stop=True), reads=["Mt"], writes=[pk(b)])
            S.op("dve", lambda e, b=b: e.tensor_add(base_bc[:, :], base_bc[:, :], PS[:, b, 0:NE]), reads=[pk(b), "posT"], writes=["base"])
            S.op("dve", lambda e: e.tensor_scalar(ptmp[:, :], posT[:, :], float(CAP), None, ALU.is_ge), reads=["posT"], writes=["ptmp"])
            S.op("dve", lambda e: e.tensor_add(posT[:, :], posT[:, :], ecap[:, :]), reads=["posT"], writes=["posT"])
            S.op("dve", lambda e: e.scalar_tensor_tensor(posT[:, :], ptmp[:, :], 1e7, posT[:, :], ALU.mult, ALU.add), reads=["posT", "ptmp"], writes=["posT"])
            for kx, Mx in enumerate((M1, M2)):
                S.op("dve", lambda e, Mx=Mx: e.tensor_mul(ptmp[:, :], posT[:, :], Mx[:, :, :].rearrange("p g x -> p (g x)")), reads=["posT", R], writes=["ptmp"])
                S.op("dve", lambda e, kx=kx: e.tensor_reduce(dstf[:, kx:kx + 1], ptmp[:, :], AX.X, ALU.add), reads=["ptmp"], writes=["dstf"])
            S.op("dve", lambda e: e.tensor_scalar(dstf[:, 2:4], dstf[:, 0:2], 1e6, None, ALU.is_lt), reads=["dstf"], writes=["dstf2"])
            S.op("dve", lambda e: e.tensor_mul(rt_w[:, st_i, :], rsm[:, 9:11], dstf[:, 2:4]), reads=[R, "dstf2"], writes=[("rtw", st_i)])
            S.op("dve", lambda e: e.tensor_copy(rt_d[:, st_i, :], dstf[:, 0:2]), reads=["dstf"], writes=[("rtd", st_i)])
            if dbg:
                S.op("dve", lambda e: e.tensor_copy(junk[:, 0:2], dstf[:, 0:2]), reads=["dstf"], writes=["junk"])
                S.op("dve", lambda e: e.tensor_copy(junk[:, 2:4], rt_w[:, st_i, :]), reads=[("rtw", st_i)], writes=["junk"])
                S.op("dve", lambda e: e.tensor_copy(junk[:, 4:8], rsm[:, 0:4]), reads=[R], writes=["junk"])
                S.dma("sp", lambda e: e.dma_start(out=dbg_t["rt"][row0:row0 + 128, :], in_=junk[:, 0:8]), reads=["junk"])
            for kx in range(2):
                S.dma("pool", lambda e, kx=kx: e.indirect_dma_start(
                    out=Xd[:, :], out_offset=bass.IndirectOffsetOnAxis(ap=rt_d[:, st_i, kx:kx + 1], axis=0),
                    in_=h2bf[:, :], in_offset=None, bounds_check=bc_reg, oob_is_err=False),
                    reads=["h2bf", ("rtd", st_i)] + xdz_keys, writes=[("xd", st_i, kx)])
        phase_end(ph)

    S.barrier()
    print("sbuf remaining after mixer alloc:", nc.sbuf_bytes_remaining)
    if stop <= 0:
        S.stack.close()
        return nc
    try:
        for ti in range(NT):
            mixer_tile(ti)
    except Stop:
        S.barrier()
        S.stack.close()
        return nc
    S.barrier()
    if stop <= 4:
        S.stack.close()
        return nc
    scope[0].close()
    scope[0] = ExitStack()

    xd_keys = [("xd", i, k) for i in range(NSUB) for k in range(2)]
    bank_lo[0] = 4
    bank_ctr[0] = 4
    Xe = sb("Xe", [128, D], BF16)
    XeT = sb("XeT", [128, 16, 128], BF16)
    identb = sb("identb", [128, 128], BF16)
    S.op("dve", lambda e: e.tensor_copy(identb[:, :], ident), writes=["identb"])
    NPB_ = 6
    pbuf = [sb("pb%d" % i, [128, 16, 256], BF16) for i in range(NPB_)]
    pb_ctr = [0]
    actT = sb("actT", [128, 8, 128], BF16)
    sil = sb("sil", [128, 256])
    act_tok = sb("act_tok", [128, 1024], BF16)
    Ye = sb("Ye", [128, D])
    x1 = sb("x1m", [128, D])

    def nxt_pb():
        i = pb_ctr[0]
        pb_ctr[0] = (i + 1) % NPB_
        return i

    NSTG = 4
    stg = [sb("stg%d" % i, [128, 16, 256]) for i in range(NSTG)]
    stg_ctr = [0]

    def load_piece(ip, src_ap, view=None):
        k = stg_ctr[0] % NSTG
        eng = ("dve", "act")[stg_ctr[0] % 2]
        q = ("sp", "pool")[stg_ctr[0] % 2]
        stg_ctr[0] += 1
        sv = stg[k][:, :, :] if view is None else view(stg[k])
        S.dma(q, lambda e: e.dma_start(out=sv, in_=src_ap), writes=[("stg", k)])
        if eng == "act":
            S.op("act", lambda e: e.activation(out=pbuf[ip][:, :, :], in_=stg[k][:, :, :], func=AF.Copy), reads=[("stg", k)], writes=[("pb", ip)])
        else:
            S.op(eng, lambda e: e.tensor_copy(pbuf[ip][:, :, :], stg[k][:, :, :]), reads=[("stg", k)], writes=[("pb", ip)])

    for ex in range(NE):
        S.dma("sp", lambda e, ex=ex: e.dma_start(out=Xe[:, :], in_=Xd[ex * CAP:(ex + 1) * CAP, :]), reads=xd_keys, writes=["Xe"])
        for g in range(4):
            b = nb()

            def f(e, g=g, b=b):
                ins = None
                for j in range(4):
                    kc = g * 4 + j
                    ins = e.matmul(PS[:, b, j * 128:(j + 1) * 128], lhsT=Xe[:, kc * 128:(kc + 1) * 128], rhs=identb[:, :], start=True, stop=True)
                return ins
            S.op("pe", f, reads=["Xe", "identb"], writes=[pk(b)])
            S.op("act", lambda e, g=g, b=b: e.activation(out=XeT[:, g * 4:(g + 1) * 4, :], in_=PS[:, b, :].rearrange("p (j t) -> p j t", j=4), func=AF.Copy),
                 reads=[pk(b)], writes=["XeT"])
        for fblk in range(4):
            ig = nxt_pb()
            load_piece(ig, ewg[ex, :, fblk * 256:(fblk + 1) * 256].rearrange("(kc p) c -> p kc c", p=128))
            iu = nxt_pb()
            load_piece(iu, ewu[ex, :, fblk * 256:(fblk + 1) * 256].rearrange("(kc p) c -> p kc c", p=128))
            b = nb()

            def f(e, b=b, ig=ig, iu=iu):
                ins = None
                for kc in range(16):
                    e.matmul(PS[:, b, 0:256], lhsT=XeT[:, kc, :], rhs=pbuf[ig][:, kc, :], start=(kc == 0), stop=(kc == 15))
                for kc in range(16):
                    ins = e.matmul(PS[:, b, 256:512], lhsT=XeT[:, kc, :], rhs=pbuf[iu][:, kc, :], start=(kc == 0), stop=(kc == 15))
                return ins
            S.op("pe", f, reads=[("pb", ig), ("pb", iu), "XeT"], writes=[pk(b)])
            S.op("act", lambda e, b=b: e.activation(out=sil[:, :], in_=PS[:, b, 0:256], func=AF.Silu), reads=[pk(b)], writes=["sil"])
            S.op("dve", lambda e, b=b, fblk=fblk: e.tensor_mul(act_tok[:, fblk * 256:(fblk + 1) * 256], sil[:, :], PS[:, b, 256:512]), reads=[pk(b), "sil"], writes=["act_tok"])
        for g in range(2):
            b = nb()

            def f(e, g=g, b=b):
                ins = None
                for j in range(4):
                    kc = g * 4 + j
                    ins = e.matmul(PS[:, b, j * 128:(j + 1) * 128], lhsT=act_tok[:, kc * 128:(kc + 1) * 128], rhs=identb[:, :], start=True, stop=True)
                return ins
            S.op("pe", f, reads=["act_tok", "identb"], writes=[pk(b)])
            S.op("act", lambda e, g=g, b=b: e.activation(out=actT[:, g * 4:(g + 1) * 4, :], in_=PS[:, b, :].rearrange("p (j t) -> p j t", j=4), func=AF.Copy),
                 reads=[pk(b)], writes=["actT"])
        for pc in range(4):
            ip = nxt_pb()
            load_piece(ip, ewd[ex, pc * 256:(pc + 1) * 256, :].rearrange("(k p) c -> p k c", p=128),
                       view=lambda t: t[:, :, :].rearrange("p a b -> p (a b)").rearrange("p (k c) -> p k c", k=2))
            wdv = pbuf[ip][:, :, :].rearrange("p a b -> p (a b)").rearrange("p (k c) -> p k c", k=2)

            def f(e, pc=pc, wdv=wdv):
                ins = None
                for k2 in range(2):
                    kc = pc * 2 + k2
                    for n in range(4):
                        ins = e.matmul(PS[:, n, :], lhsT=actT[:, kc, :], rhs=wdv[:, k2, n * 512:(n + 1) * 512], start=(kc == 0), stop=(kc == 7))
                return ins
            S.op("pe", f, reads=[("pb", ip), "actT"], writes=[pk(0), pk(1), pk(2), pk(3)])
        for n in range(4):
            eng = "act" if n % 2 else "dve"
            if eng == "act":
                S.op("act", lambda e, n=n: e.activation(out=Ye[:, n * 512:(n + 1) * 512], in_=PS[:, n, :], func=AF.Copy), reads=[pk(n)], writes=[("Ye", n)])
            else:
                S.op("dve", lambda e, n=n: e.tensor_copy(Ye[:, n * 512:(n + 1) * 512], PS[:, n, :]), reads=[pk(n)], writes=[("Ye", n)])
        S.dma("sp", lambda e, ex=ex: e.dma_start(out=Yd[ex * CAP:(ex + 1) * CAP, :], in_=Ye[:, :]), reads=[("Ye", n) for n in range(4)], writes=[("yd", ex)])
    yd_keys = [("yd", ex) for ex in range(NE)]
    r1 = sb("r1", [128, D])
    r2 = sb("r2", [128, D])
    S.op("dve", lambda e: e.memset(r1[:, :], 0.0), writes=["r1"])
    S.op("dve", lambda e: e.memset(r2[:, :], 0.0), writes=["r2"])
    fin = []
    for st_i in range(NSUB):
        row0 = st_i * 128
        S.dma("sp", lambda e, row0=row0: e.dma_start(out=x1[:, :], in_=y_out[row0:row0 + 128, :]), reads=[("yrow", st_i)], writes=["xin"])
        for kx, rr in enumerate((r1, r2)):
            S.dma("pool", lambda e, kx=kx, rr=rr, st_i=st_i: e.indirect_dma_start(
                out=rr[:, :], out_offset=None, in_=Yd[:, :],
                in_offset=bass.IndirectOffsetOnAxis(ap=rt_d[:, st_i, kx:kx + 1], axis=0), bounds_check=bc_reg, oob_is_err=False),
                reads=yd_keys + [("rtd", st_i)], writes=["r%d" % (kx + 1)])
        S.op("dve", lambda e, st_i=st_i: e.scalar_tensor_tensor(x1[:, :], r1[:, :], rt_w[:, st_i, 0:1], x1[:, :], ALU.mult, ALU.add), reads=["r1", "xin", ("rtw", st_i)], writes=["xin"])
        S.op("dve", lambda e, st_i=st_i: e.scalar_tensor_tensor(x1[:, :], r2[:, :], rt_w[:, st_i, 1:2], x1[:, :], ALU.mult, ALU.add), reads=["r2", "xin", ("rtw", st_i)], writes=["xin"])
        S.dma("sp", lambda e, row0=row0: e.dma_start(out=y_out[row0:row0 + 128, :], in_=x1[:, :]), reads=["xin"], writes=[("yfin", st_i)])
        fin.append(("yfin", st_i))
    S.wait_all("sp", fin)
    if dbg:
        S.wait_all("sp", ["junk"])
    S.barrier()
    S.stack.close()
    return nc


def make_consts(n_exp, cap):
    c = np.zeros((128, 1408), np.float32)
    p = np.arange(128)
    c[:, 0:128] = np.eye(128)
    s = (p % 64)[:, None]
    t = np.arange(64)[None, :]
    c[:, 128:192] = (s < t)
    c[:, 192:256] = (s <= t)
    c[0:64, 256:320] = (np.arange(64)[:, None] < t)
    c[0:64, 320:384] = (np.arange(64)[:, None] > t)
    c[:, 384:512] = (p[:, None] // 64 == p[None, :] // 64)
    c[:, 512] = (p < 64)
    c[:, 513] = (p >= 64)
    c[:, 640:768] = (p[:, None] < p[None, :])
    c[:, 768:896] = 1.0
    c[:, 896:1024] = (p[:, None] <= p[None, :])
    c[:, 1024:1152] = (p[:, None] > p[None, :])
    c[:, 1152:1216] = 1.0
    c[:, 1344:1408] = 1.0
    ecap = np.tile((np.arange(n_exp) * cap).astype(np.float32)[None, :], (128, 1))
    return c, ecap


def core_inputs(inp, b, hh, n_prev, n_own, n_groups, cap):
    sq = lambda a: np.ascontiguousarray(np.asarray(a)[0])
    x = np.asarray(inp["x"])
    own = x[b, hh * n_own * TT:(hh + 1) * n_own * TT]
    if hh == 0:
        prev = np.zeros((n_prev * TT, D), np.float32)
    else:
        prev = x[b, hh * n_own * TT - n_prev * TT:hh * n_own * TT]
    ne = n_groups * 8
    c, ecap = make_consts(ne, cap)
    m = {
        "xs": np.ascontiguousarray(np.concatenate([prev, own], 0)),
        "flag": np.full((128, 1), float(hh), np.float32),
        "w_in": sq(inp["w_in"]),
        "norm1_w": sq(inp["norm1_w"]).reshape(1, D),
        "norm2_w": sq(inp["norm2_w"]).reshape(1, D),
        "rwkv_mu": sq(inp["rwkv_mu"]).reshape(-1, 1),
        "rwkv_w0": sq(inp["rwkv_w0"]).reshape(-1, 1),
        "rwkv_a0": sq(inp["rwkv_a0"]).reshape(-1, 1),
        "rwkv_k_k": sq(inp["rwkv_k_k"]).reshape(-1, 1),
        "rwkv_k_a": sq(inp["rwkv_k_a"]).reshape(-1, 1),
        "rwkv_r_k": sq(inp["rwkv_r_k"]).reshape(-1, 1),
        "rwkv_w2": sq(inp["rwkv_w2"]),
        "rwkv_a2": sq(inp["rwkv_a2"]),
        "rwkv_g2": sq(inp["rwkv_g2"]),
        "rwkv_gn_w": sq(inp["rwkv_gn_w"]).reshape(1, -1),
        "rwkv_gn_b": sq(inp["rwkv_gn_b"]).reshape(1, -1),
        "q_norm_w": sq(inp["q_norm_w"]).reshape(-1, 1),
        "k_norm_w": sq(inp["k_norm_w"]).reshape(-1, 1),
        "attn_sinks": sq(inp["attn_sinks"]).reshape(-1, 1),
        "proj_rwkv": sq(inp["proj_rwkv"]),
        "proj_attn": sq(inp["proj_attn"]),
        "w_out": sq(inp["w_out"]),
        "router_coarse_w": sq(inp["router_coarse_w"]),
        "router_coarse_b": sq(inp["router_coarse_b"]).reshape(1, -1),
        "router_fine_w": sq(inp["router_fine_w"]),
        "router_fine_b": sq(inp["router_fine_b"]).reshape(1, -1),
        "expert_w_gate": sq(inp["expert_w_gate"]),
        "expert_w_up": sq(inp["expert_w_up"]),
        "expert_w_down": sq(inp["expert_w_down"]),
        "consts": c,
        "ecap": ecap,
    }
    return m


def kernel(**inputs):
    cfg = dict(n_prev=16, n_own=16, n_groups=8, cap=128)
    nc = build_program(cfg)
    in_maps = []
    for c in range(8):
        in_maps.append(core_inputs(inputs, c // 2, c % 2, 16, 16, 8, 128))
    res = run_bass_kernel_spmd(nc, in_maps, core_ids=list(range(8)))
    out = np.zeros((4, 4096, D), np.float32)
    for c in range(8):
        b, hh = c // 2, c % 2
        out[b, hh * 2048:(hh + 1) * 2048] = res.results[c]["y"]
    return out
```

```python
import numpy as np
import ml_dtypes
import concourse.bass as bass
import concourse.mybir as mybir
from concourse.bass_utils import run_bass_kernel_spmd

F32 = mybir.dt.float32
BF16 = mybir.dt.bfloat16
I32 = mybir.dt.int32
AF = mybir.ActivationFunctionType
ALU = mybir.AluOpType
AX = mybir.AxisListType

D = 2048
RW = 1024
TT = 128
NS = TT // 128
CH = 64
NCH = TT // CH
IN_COLS = 9152
C_R, C_K, C_V, C_XW, C_XA, C_XG = 0, 1024, 2048, 3072, 3168, 3264
C_Q, C_KA, C_VA, C_GA, C_GB = 3520, 4544, 4800, 5056, 7104
NDS = 40
DECAY_C = 0.6065306597126334


class Sched:
    def __init__(self, nc):
        from contextlib import ExitStack
        self.stack = ExitStack()
        self.nc = nc
        self.E = dict(pe=nc.tensor, dve=nc.vector, act=nc.scalar, pool=nc.gpsimd, sp=nc.sync)
        self.esem = {}
        for e in ("pe", "dve", "act", "pool"):
            self.esem[e] = self.stack.enter_context(nc.semaphore("es_" + e))
        self.ecnt = dict.fromkeys(self.esem, 0)
        self.dsems = [self.stack.enter_context(nc.semaphore("ds%d" % i)) for i in range(NDS)]
        self.dcnt = [0] * NDS
        self.dnext = 0
        self.seen = {e: {} for e in self.E}
        self.W = {}
        self.R = {}

    def _wait(self, eng, deps):
        for sid, (sem, val) in deps.items():
            if val > 0 and self.seen[eng].get(sid, 0) < val:
                self.E[eng].wait_ge(sem, val)
                self.seen[eng][sid] = val

    def _deps(self, reads, writes):
        deps = {}

        def add(d):
            for sid, (sem, val) in d.items():
                if sid not in deps or deps[sid][1] < val:
                    deps[sid] = (sem, val)
        for r in reads:
            add(self.W.get(r, {}))
        for w in writes:
            add(self.W.get(w, {}))
            add(self.R.get(w, {}))
        return deps

    def _record(self, me, reads, writes):
        sid, sem, val = me
        for r in reads:
            self.R.setdefault(r, {})[sid] = (sem, val)
        for w in writes:
            self.W[w] = {sid: (sem, val)}
            self.R[w] = {}

    def op(self, eng, fn, reads=(), writes=()):
        self._wait(eng, self._deps(reads, writes))
        ins = fn(self.E[eng])
        self.ecnt[eng] += 1
        ins.then_inc(self.esem[eng], 1)
        self._record((eng, self.esem[eng], self.ecnt[eng]), reads, writes)

    def dma(self, q, fn, reads=(), writes=()):
        i = self.dnext
        self.dnext = (i + 1) % NDS
        sem = self.dsems[i]
        deps = self._deps(reads, writes)
        sid = "d%d" % i
        if sid not in deps or deps[sid][1] < self.dcnt[i]:
            deps[sid] = (sem, self.dcnt[i])
        self._wait(q, deps)
        ins = fn(self.E[q])
        ins.then_inc(sem, 16)
        self.dcnt[i] += 16
        self._record((sid, sem, self.dcnt[i]), reads, writes)

    def barrier(self):
        keys = list(self.W.keys())
        for eng in self.E:
            self.wait_all(eng, keys)

    def wait_all(self, eng, keys):
        deps = {}
        for k in keys:
            for d in (self.W.get(k, {}), self.R.get(k, {})):
                for sid, (sem, val) in d.items():
                    if sid not in deps or deps[sid][1] < val:
                        deps[sid] = (sem, val)
        self._wait(eng, deps)


def build_program(cfg):
    NPREV = cfg["n_prev"]
    NOWN = cfg["n_own"]
    NG = cfg["n_groups"]
    NE = NG * 8
    CAP = cfg["cap"]
    NT = NPREV + NOWN
    NTOK = NT * TT
    NOWN_TOK = NOWN * TT
    NSUB = NOWN_TOK // 128
    NRL = 8 + NE
    dbg = cfg.get("dbg", False)

    nc = bass.Bass("TRN2", target_bir_lowering=False)
    _ncd = nc.allow_non_contiguous_dma(reason="tiny per-channel parameter loads")
    _ncd.__enter__()
    S = Sched(nc)

    def din(name, shape, dt=F32):
        return nc.dram_tensor(name, list(shape), dt, kind="ExternalInput").ap()

    xs = din("xs", [NTOK, D])
    flag_d = din("flag", [128, 1])
    w_in = din("w_in", [D, IN_COLS])
    norm1_w = din("norm1_w", [1, D])
    norm2_w = din("norm2_w", [1, D])
    mu_d = din("rwkv_mu", [3520, 1])
    w0_d = din("rwkv_w0", [RW, 1])
    a0_d = din("rwkv_a0", [RW, 1])
    kk_d = din("rwkv_k_k", [RW, 1])
    ka_d = din("rwkv_k_a", [RW, 1])
    rk_d = din("rwkv_r_k", [RW, 1])
    w2_d = din("rwkv_w2", [96, RW])
    a2_d = din("rwkv_a2", [96, RW])
    g2_d = din("rwkv_g2", [256, RW])
    gnw_d = din("rwkv_gn_w", [1, RW])
    gnb_d = din("rwkv_gn_b", [1, RW])
    qn_d = din("q_norm_w", [64, 1])
    kn_d = din("k_norm_w", [64, 1])
    sink_d = din("attn_sinks", [16, 1])
    proj_r = din("proj_rwkv", [RW, D])
    proj_a = din("proj_attn", [RW, D])
    w_out = din("w_out", [D, D])
    rcw_d = din("router_coarse_w", [D, NG])
    rcb_d = din("router_coarse_b", [1, NG])
    rfw_d = din("router_fine_w", [D, NE])
    rfb_d = din("router_fine_b", [1, NE])
    ewg = din("expert_w_gate", [NE, D, 1024])
    ewu = din("expert_w_up", [NE, D, 1024])
    ewd = din("expert_w_down", [NE, 1024, D])
    cst_d = din("consts", [128, 1408])
    ecap_d = din("ecap", [128, NE])
    y_out = nc.dram_tensor("y", [NOWN_TOK, D], F32, kind="ExternalOutput").ap()
    Xd = nc.dram_tensor("xdisp", [NE * CAP, D], BF16).ap()
    Yd = nc.dram_tensor("ydisp", [NE * CAP, D], F32).ap()
    dbg_t = {}
    if dbg:
        dbg_t["aout"] = nc.dram_tensor("dbg_aout", [RW, NOWN_TOK], F32, kind="ExternalOutput").ap()
        dbg_t["bout"] = nc.dram_tensor("dbg_bout", [RW, NOWN_TOK], F32, kind="ExternalOutput").ap()
        dbg_t["xin"] = nc.dram_tensor("dbg_x1", [NOWN_TOK, D], F32, kind="ExternalOutput").ap()
        dbg_t["rt"] = nc.dram_tensor("dbg_rt", [NOWN_TOK, 8], F32, kind="ExternalOutput").ap()

    from contextlib import ExitStack
    scope = [None]

    uniq = [0]

    def phase_begin():
        S.barrier()
        st = ExitStack()
        st._prev = scope[0]
        scope[0] = st
        return st

    def phase_end(st):
        S.barrier()
        scope[0] = st._prev
        st.close()

    def sb(name, shape, dt=F32):
        uniq[0] += 1
        name = "%s_%d" % (name, uniq[0])
        if cfg.get("verbose"):
            print("alloc", name, shape, dt, "remaining", nc.sbuf_bytes_remaining)
        if scope[0] is None:
            return nc.alloc_sbuf_tensor("s_" + name, list(shape), dt)
        return scope[0].enter_context(nc.sbuf_tensor("s_" + name, list(shape), dt))

    PS = nc.alloc_psum_tensor("ps", [128, 8, 512], F32)
    bank_ctr = [0]

    bank_lo = [0]

    def nb():
        b = bank_ctr[0]
        bank_ctr[0] = b + 1 if b + 1 < 8 else bank_lo[0]
        return b

    def nb2():
        b = bank_ctr[0]
        if b % 2:
            b = (b + 1) % 8
        bank_ctr[0] = (b + 2) % 8
        return b

    def pk(b):
        return ("ps", b)

    cst = sb("cst", [128, 1408])
    ident = cst[:, 0:128]
    maskA = cst[:, 128:256]
    maskU = cst[0:64, 256:320]
    maskL = cst[0:64, 320:384]
    ident64 = cst[0:64, 0:64]
    bones = cst[:, 384:512]
    headsel = cst[:, 512:514]
    tri_strict = cst[:, 640:768]
    ones_m = cst[:, 768:896]
    mcur_f = cst[:, 896:1024]
    mprev_f = cst[:, 1024:1152]
    onesL_f = cst[:, 1152:1280]
    onesR_f = cst[:, 1280:1408]
    S.dma("sp", lambda e: e.dma_start(out=cst[:, :], in_=cst_d[:, :]), writes=["cst"])
    ecap = sb("ecap", [128, NE])
    S.dma("sp", lambda e: e.dma_start(out=ecap[:, :], in_=ecap_d[:, :]), writes=["cst"])
    flag = sb("flag", [128, 1])
    S.dma("sp", lambda e: e.dma_start(out=flag[:, :], in_=flag_d[:, :]), writes=["cst"])

    if cfg.get("sstop", 99) <= 1:
        S.barrier(); S.stack.close(); return nc
    rt_w = sb("rt_w", [128, NSUB, 2])
    rt_d = sb("rt_d", [128, NSUB, 2], I32)
    scope[0] = ExitStack()
    NPB = 28
    mu_t = sb("mu_t", [128, NPB])
    omm_t = sb("omm_t", [128, NPB])
    S.op("dve", lambda e: e.memset(mu_t[:, :], 0.0), writes=["mu"])
    for j in range(24):
        S.dma("sp", lambda e, j=j: e.dma_start(out=mu_t[:, j:j + 1], in_=mu_d[j * 128:(j + 1) * 128, :]), writes=["mu"])
    S.dma("sp", lambda e: e.dma_start(out=mu_t[0:96, 24:25], in_=mu_d[C_XW:C_XW + 96, :]), writes=["mu"])
    S.dma("sp", lambda e: e.dma_start(out=mu_t[0:96, 25:26], in_=mu_d[C_XA:C_XA + 96, :]), writes=["mu"])
    for j in range(2):
        S.dma("sp", lambda e, j=j: e.dma_start(out=mu_t[:, 26 + j:27 + j], in_=mu_d[C_XG + j * 128:C_XG + (j + 1) * 128, :]), writes=["mu"])
    S.op("dve", lambda e: e.tensor_scalar(omm_t[:, :], mu_t[:, :], -1.0, 1.0, ALU.mult, ALU.add), reads=["mu"], writes=["omm"])
    if cfg.get("sstop", 99) <= 2:
        S.barrier(); S.stack.close(); return nc
    pch = sb("pch", [128, 5, 8])
    for i, dsrc in enumerate((w0_d, a0_d, kk_d, ka_d, rk_d)):
        for b in range(8):
            S.dma("sp", lambda e, i=i, b=b, dsrc=dsrc: e.dma_start(out=pch[:, i, b:b + 1], in_=dsrc[b * 128:(b + 1) * 128, :]), writes=["pch"])
    ka1 = sb("ka1", [128, 8])
    S.op("dve", lambda e: e.tensor_scalar(ka1[:, :], pch[:, 3, :], -1.0, 1.0, ALU.mult, ALU.add), reads=["pch"], writes=["ka1"])
    if cfg.get("sstop", 99) <= 3:
        S.barrier(); S.stack.close(); return nc
    w2s = sb("w2s", [96, RW])
    a2s = sb("a2s", [96, RW])
    g2s = sb("g2s", [128, 2, RW])
    S.dma("sp", lambda e: e.dma_start(out=w2s[:, :], in_=w2_d[:, :]), writes=["lora"])
    S.dma("sp", lambda e: e.dma_start(out=a2s[:, :], in_=a2_d[:, :]), writes=["lora"])
    S.dma("sp", lambda e: e.dma_start(out=g2s[:, :, :], in_=g2_d.rearrange("(k p) c -> p k c", p=128)), writes=["lora"])
    g1col = sb("g1col", [128, 16])
    g2bc = sb("g2bc", [128, D])
    S.dma("sp", lambda e: e.dma_start(out=g1col[:, :], in_=norm1_w.rearrange("o (k p) -> p (o k)", p=128)), writes=["gbc"])
    S.dma("sp", lambda e: e.dma_start(out=g2bc[:, :], in_=norm2_w.partition_broadcast(128)), writes=["gbc"])
    gnw = sb("gnw", [64, RW])
    gnb = sb("gnb", [64, RW])
    S.dma("sp", lambda e: e.dma_start(out=gnw[:, :], in_=gnw_d.partition_broadcast(64)), writes=["gbc"])
    S.dma("sp", lambda e: e.dma_start(out=gnb[:, :], in_=gnb_d.partition_broadcast(64)), writes=["gbc"])
    if cfg.get("sstop", 99) <= 4:
        S.barrier(); S.stack.close(); return nc
    qkg = sb("qkg", [128, 2])
    for hp in range(2):
        S.dma("sp", lambda e, hp=hp: e.dma_start(out=qkg[hp * 64:(hp + 1) * 64, 0:1], in_=qn_d[:, :]), writes=["gbc"])
        S.dma("sp", lambda e, hp=hp: e.dma_start(out=qkg[hp * 64:(hp + 1) * 64, 1:2], in_=kn_d[:, :]), writes=["gbc"])
    esink = sb("esink", [128, 8])
    for b in range(8):
        for hp in range(2):
            h = 2 * b + hp
            S.dma("sp", lambda e, b=b, hp=hp, h=h: e.dma_start(out=esink[hp * 64:(hp + 1) * 64, b:b + 1], in_=sink_d[h:h + 1, :].partition_broadcast(64)), writes=["esink"])
    S.op("act", lambda e: e.activation(out=esink[:, :], in_=esink[:, :], func=AF.Exp), reads=["esink"], writes=["esink"])
    if cfg.get("sstop", 99) <= 5:
        S.barrier(); S.stack.close(); return nc
    wr = sb("wr", [128, 16, NRL])
    S.op("dve", lambda e: e.memset(wr[:, :, :], 0.0), writes=["wr"])
    S.dma("sp", lambda e: e.dma_start(out=wr[:, :, 0:NG], in_=rcw_d.rearrange("(k p) c -> p k c", p=128)), writes=["wr"])
    S.dma("sp", lambda e: e.dma_start(out=wr[:, :, 8:NRL], in_=rfw_d.rearrange("(k p) c -> p k c", p=128)), writes=["wr"])
    rbias = sb("rbias", [128, NRL])
    S.op("dve", lambda e: e.memset(rbias[:, :], 0.0), writes=["wr"])
    S.dma("sp", lambda e: e.dma_start(out=rbias[:, 0:NG], in_=rcb_d.partition_broadcast(128)), writes=["wr"])
    S.dma("sp", lambda e: e.dma_start(out=rbias[:, 8:NRL], in_=rfb_d.partition_broadcast(128)), writes=["wr"])
    if cfg.get("sstop", 99) <= 6:
        S.barrier(); S.stack.close(); return nc
    mcur = sb("mcur", [128, 4, 128], BF16)
    mprev = sb("mprev", [128, 4, 128], BF16)
    onesL = sb("onesL", [128, 128], BF16)
    onesR = sb("onesR", [128, 128], BF16)
    maskA4 = sb("maskA4", [128, 4, 128])
    for i in range(4):
        S.op("dve", lambda e, i=i: e.tensor_copy(mcur[:, i, :], mcur_f), reads=["cst"], writes=["m1"])
        S.op("dve", lambda e, i=i: e.tensor_copy(mprev[:, i, :], mprev_f), reads=["cst"], writes=["m1"])
        S.op("dve", lambda e, i=i: e.tensor_copy(maskA4[:, i, :], maskA), reads=["cst"], writes=["m1"])
    S.op("dve", lambda e: e.tensor_copy(onesL[:, :], onesL_f), reads=["cst"], writes=["m1"])
    S.op("dve", lambda e: e.tensor_copy(onesR[:, :], onesR_f), reads=["cst"], writes=["m1"])
    ones64 = sb("ones64", [128, 64])
    S.op("dve", lambda e: e.memset(ones64[:, :], 1.0), writes=["m1"])

    if cfg.get("sstop", 99) <= 7:
        S.barrier(); S.stack.close(); return nc
    zt = sb("zt", [128, D], BF16)
    S.op("dve", lambda e: e.memset(zt[:, :], 0.0), writes=["zt"])
    for ex in range(NE):
        S.dma("sp", lambda e, ex=ex: e.dma_start(out=Xd[ex * CAP:(ex + 1) * CAP, :], in_=zt[:, :]), reads=["zt"], writes=[("xdz", ex)])
    xdz_keys = [("xdz", ex) for ex in range(NE)]
    bc_reg = nc.gpsimd.alloc_register("bcreg")
    nc.gpsimd.reg_mov(bc_reg, NE * CAP - 1)
    carry = sb("carry", [128, NPB])
    ST = sb("ST", [128, 8, 64])
    S.op("dve", lambda e: e.memset(carry[:, :], 0.0), writes=["carry"])
    S.op("dve", lambda e: e.memset(ST[:, :, :], 0.0), writes=["ST"])
    STb = sb("STb", [128, 8, 64])
    S.op("dve", lambda e: e.memset(STb[:, :, :], 0.0), writes=["STb"])
    base_bc = sb("base_bc", [128, NE])
    S.op("dve", lambda e: e.memset(base_bc[:, :], 0.0), writes=["base"])
    kTs = sb("kTs", [128, 3, 4, 128], BF16)
    Vpad = sb("Vpad", [128, 3, 4, 2, 128], BF16)
    S.op("dve", lambda e: e.memset(kTs[:, :, :, :], 0.0), writes=["kTs0", "kTs1", "kTs2"])
    S.op("dve", lambda e: e.memset(Vpad[:, :, :, :, :], 0.0), writes=["Vp0", "Vp1", "Vp2"])

    st1 = sb("st1", [128, 8])
    tmp_sh = sb("tmp_sh", [128, TT])
    hT = sb("hT", [128, 16, TT], BF16)
    NWB = 3
    wbuf = [sb("wb%d" % i, [128, 16, 128], BF16) for i in range(NWB)]
    wb_ctr = [0]
    a_outT = sb("a_outT", [128, 8, TT], BF16)
    b_outT = sb("b_outT", [128, 8, TT], BF16)

    WC = nc.dram_tensor("wcache", [160, 128, 2048], BF16).ap()
    wcache = {}
    cq = [0]

    def load_wblock(key, i, K16, fill_fn):
        wb = wbuf[i]
        flat = wb[:, :, :].rearrange("p a b -> p (a b)")[:, 0:K16 * 128]
        if key in wcache:
            idx = wcache[key]
            q = ("sp", "pool")[cq[0] % 2]
            cq[0] += 1
            S.dma(q, lambda e: e.dma_start(out=flat, in_=WC[idx, :, 0:K16 * 128]), reads=[("wc", idx)], writes=[("wb", i)])
        else:
            fill_fn()
            idx = len(wcache)
            wcache[key] = idx
            S.dma("sp", lambda e: e.dma_start(out=WC[idx, :, 0:K16 * 128], in_=flat), reads=[("wb", i)], writes=[("wc", idx)])

    def mm_block(dram_w, col_specs, K16, M, rhs_fn, N, rkeys, wname):
        i = wb_ctr[0]
        wb_ctr[0] = (i + 1) % NWB
        wb = wbuf[i]

        def fill():
            for (c0, n, d0) in col_specs:
                S.dma("pool", lambda e, c0=c0, n=n, d0=d0: e.dma_start(
                    out=wb[:, 0:K16, d0:d0 + n], in_=dram_w[:, c0:c0 + n].rearrange("(kc p) c -> p kc c", p=128)),
                    writes=[("wb", i)])
        load_wblock((wname, tuple(col_specs)), i, K16, fill)
        b = nb()

        def f(e):
            ins = None
            for kc in range(K16):
                ins = e.matmul(PS[0:M, b, 0:N], lhsT=wb[:, kc, 0:M], rhs=rhs_fn(kc), start=(kc == 0), stop=(kc == K16 - 1))
            return ins
        S.op("pe", f, reads=[("wb", i)] + rkeys, writes=[pk(b)])
        return b

    def shift_evac(b, M, pblk, out_ap, okey):
        mu = mu_t[0:M, pblk:pblk + 1]
        om = omm_t[0:M, pblk:pblk + 1]
        S.op("dve", lambda e: e.tensor_scalar(tmp_sh[0:M, :], PS[0:M, b, 0:TT], mu, None, ALU.mult), reads=[pk(b)], writes=["tmp_sh"])
        S.op("dve", lambda e: e.scalar_tensor_tensor(out_ap[:, 1:TT], PS[0:M, b, 1:TT], om, tmp_sh[0:M, 0:TT - 1], ALU.mult, ALU.add),
             reads=[pk(b), "tmp_sh"], writes=[okey])
        S.op("dve", lambda e: e.scalar_tensor_tensor(out_ap[:, 0:1], PS[0:M, b, 0:1], om, carry[0:M, pblk:pblk + 1], ALU.mult, ALU.add),
             reads=[pk(b), "carry"], writes=[okey])
        S.op("dve", lambda e: e.tensor_copy(carry[0:M, pblk:pblk + 1], tmp_sh[0:M, TT - 1:TT]), reads=["tmp_sh"], writes=["carry"])

    hT_rhs = lambda kc: hT[:, kc, :]

    def c3(ap, c):
        return ap.rearrange("p (c t) -> p c t", c=c)

    class Stop(Exception):
        pass
    stop = cfg.get("stop", 99)

    def mixer_tile(ti):
        own = ti >= NPREV
        oi = ti - NPREV
        tok0 = ti * TT
        need_kv = own or (ti == NPREV - 1)
        ph = phase_begin()
        xin = sb("xin", [128, D])
        xn = xin
        junk = sb("junk", [128, D])
        for s in range(NS):
            S.dma("sp", lambda e: e.dma_start(out=xin[:, :], in_=xs[tok0 + s * 128:tok0 + (s + 1) * 128, :]), writes=["xin"])
            S.op("act", lambda e: e.activation(out=junk[:, :], in_=xin[:, :], func=AF.Square, accum_out=st1[:, 0:1]), reads=["xin"], writes=["junk", "st1"])
            S.op("dve", lambda e: e.tensor_scalar(st1[:, 1:2], st1[:, 0:1], 1.0 / D, 1e-6, ALU.mult, ALU.add), reads=["st1"], writes=["st1b"])
            S.op("act", lambda e: e.activation(out=st1[:, 2:3], in_=st1[:, 1:2], func=AF.Sqrt), reads=["st1b"], writes=["st1c"])
            S.op("dve", lambda e: e.reciprocal(st1[:, 2:3], st1[:, 2:3]), reads=["st1c"], writes=["st1c"])
            S.op("dve", lambda e: e.tensor_scalar(xn[:, :], xin[:, :], st1[:, 2:3], None, ALU.mult), reads=["xin", "st1c"], writes=["xin"])
            for g in range(4):
                b = nb()

                def f(e, g=g, b=b):
                    ins = None
                    for j in range(4):
                        kc = g * 4 + j
                        ins = e.transpose(PS[:, b, j * 128:(j + 1) * 128], xn[:, kc * 128:(kc + 1) * 128], ident)
                    return ins
                S.op("pe", f, reads=["xin"], writes=[pk(b)])
                for j in range(4):
                    kc = g * 4 + j
                    S.op("act", lambda e, kc=kc, j=j, b=b: e.activation(out=hT[:, kc, s * 128:(s + 1) * 128], in_=PS[:, b, j * 128:(j + 1) * 128],
                                                                    func=AF.Copy, scale=g1col[:, kc:kc + 1]),
                         reads=[pk(b)], writes=["hT"])
        phase_end(ph)
        if stop <= 1:
            raise Stop()
        ph = phase_begin()
        xwT = sb("xwT", [128, TT])
        xaT = sb("xaT", [128, TT])
        sgT = sb("sgT", [128, 2, TT])
        r_b = sb("r_b", [128, TT])
        k_b = sb("k_b", [128, TT])
        v_b = sb("v_b", [128, TT])
        a_b = sb("a_b", [128, TT])
        sw_b = sb("sw_b", [128, TT])
        r_bs = [r_b, sb("r_b2", [128, TT])]
        k_bs = [k_b, sb("k_b2", [128, TT])]
        v_bs = [v_b, sb("v_b2", [128, TT])]
        a_bs = [a_b, sb("a_b2", [128, TT])]
        sw_bs = [sw_b, sb("sw_b2", [128, TT])]
        cs_b = sb("cs_b", [128, TT])
        csx_b = sb("csx_b", [128, TT])
        e1 = sb("e1", [128, TT])
        e2 = sb("e2", [128, TT])
        e3 = sb("e3", [128, TT])
        e4 = sb("e4", [128, TT])
        nbias = sb("nbias", [128, NCH])
        kq = sb("kq", [128, TT])
        t1 = sb("t1", [128, TT])
        kkn = sb("kkn", [128, TT])
        kmod = sb("kmod", [128, TT])
        bb = sb("bb", [128, TT])
        rkk = sb("rkk", [128, TT])
        AR = sb("AR", [128, 8, NCH, 128])
        ARm = [sb("ARm%d" % i_, [128, 8, NCH, 128]) for i_ in range(2)]
        BK = sb("BK", [128, 8, NCH, 128])
        BKp = sb("BKp", [128, NCH, 128])
        gamC = sb("gamC", [128, 8, NCH])
        VU = sb("VU", [128, NCH, 16, 64])
        bsum = sb("bsum", [64, NCH, 16])
        AT = sb("AT", [128, 16, 128])
        ATK = [("AT", g_) for g_ in range(4)]
        PQ = [sb("PQ%d" % i, [64, 16, 128], BF16) for i in range(2)]
        Tt = [sb("Tt%d" % i, [64, 16, 64], BF16) for i in range(2)]
        Wsb = sb("Wsb", [64, 16, 64])
        ysq = sb("ysq", [64, RW])
        yn = sb("yn", [64, RW])
        gst = sb("gst", [64, 4, 16])
        BKtok = sb("BKtok", [128, NCH, 16, 128])
        TtF = sb("TtF", [64, 16, 128])
        S.op("dve", lambda e: e.memset(BKtok[:, :, :, :], 0.0), writes=["BKtok"])
        S.op("dve", lambda e: e.memset(TtF[:, :, :], 0.0), writes=[("TtF", 0), ("TtF", 1)])
        b = mm_block(w_in, [(C_XW, 96, 0)], 16, 96, hT_rhs, TT, ["hT"], "xw")
        shift_evac(b, 96, 24, xwT[0:96, :], "xwT")
        S.op("act", lambda e: e.activation(out=xwT[0:96, :], in_=xwT[0:96, :], func=AF.Tanh), reads=["xwT"], writes=["xwT"])
        b = mm_block(w_in, [(C_XA, 96, 0)], 16, 96, hT_rhs, TT, ["hT"], "xa")
        shift_evac(b, 96, 25, xaT[0:96, :], "xaT")
        for j in range(2):
            b = mm_block(w_in, [(C_XG + j * 128, 128, 0)], 16, 128, hT_rhs, TT, ["hT"], "xg")
            shift_evac(b, 128, 26 + j, sgT[:, j, :], "sgT")
        S.op("act", lambda e: e.activation(out=sgT[:, :, :], in_=sgT[:, :, :], func=AF.Sigmoid), reads=["sgT"], writes=["sgT"])
        if stop <= 1.2:
            raise Stop()
        def stageA(blk):
            par = blk % 2
            r_b, k_b, v_b, a_b, sw_b = r_bs[par], k_bs[par], v_bs[par], a_bs[par], sw_bs[par]
            b = mm_block(w_in, [(C_R + blk * 128, 128, 0)], 16, 128, hT_rhs, TT, ["hT"], "r")
            shift_evac(b, 128, blk, r_b[:, :], ("r_b", par))
            b = mm_block(w_in, [(C_K + blk * 128, 128, 0)], 16, 128, hT_rhs, TT, ["hT"], "k")
            shift_evac(b, 128, 8 + blk, k_b[:, :], ("k_b", par))
            b = mm_block(w_in, [(C_V + blk * 128, 128, 0)], 16, 128, hT_rhs, TT, ["hT"], "v")
            shift_evac(b, 128, 16 + blk, v_b[:, :], ("v_b", par))
            b = nb()
            S.op("pe", lambda e, b=b: e.matmul(PS[:, b, 0:TT], lhsT=a2s[:, blk * 128:(blk + 1) * 128], rhs=xaT[0:96, :], start=True, stop=True),
                 reads=["xaT"], writes=[pk(b)])
            S.op("act", lambda e, b=b: e.activation(out=a_b[:, :], in_=PS[:, b, 0:TT], func=AF.Sigmoid, bias=pch[:, 1, blk:blk + 1]),
                 reads=[pk(b)], writes=[("a_b", par)])
            b = nb()
            S.op("pe", lambda e, b=b: e.matmul(PS[:, b, 0:TT], lhsT=w2s[:, blk * 128:(blk + 1) * 128], rhs=xwT[0:96, :], start=True, stop=True),
                 reads=["xwT"], writes=[pk(b)])
            S.op("act", lambda e, b=b: e.activation(out=sw_b[:, :], in_=PS[:, b, 0:TT], func=AF.Sigmoid, bias=pch[:, 0, blk:blk + 1]),
                 reads=[pk(b)], writes=[("sw_b", par)])

        def stageB(blk):
            par = blk % 2
            r_b, k_b, v_b, a_b, sw_b = r_bs[par], k_bs[par], v_bs[par], a_bs[par], sw_bs[par]
            for ch in range(NCH):
                S.op("dve", lambda e, ch=ch: e.tensor_tensor_scan(cs_b[:, ch * CH:(ch + 1) * CH], ones64[:, :], sw_b[:, ch * CH:(ch + 1) * CH], 0.0, ALU.mult, ALU.add),
                     reads=[("sw_b", par)], writes=["cs_b"])
            S.op("dve", lambda e: e.tensor_sub(csx_b[:, :], cs_b[:, :], sw_b[:, :]), reads=["cs_b", ("sw_b", par)], writes=["csx_b"])
            S.op("act", lambda e: e.activation(out=e1[:, :], in_=cs_b[:, :], func=AF.Exp, scale=-DECAY_C), reads=["cs_b"], writes=["e1"])
            S.op("act", lambda e: e.activation(out=e2[:, :], in_=csx_b[:, :], func=AF.Exp, scale=-DECAY_C), reads=["csx_b"], writes=["e2"])
            S.op("act", lambda e: e.activation(out=e3[:, :], in_=cs_b[:, :], func=AF.Exp, scale=DECAY_C), reads=["cs_b"], writes=["e3"])
            S.op("dve", lambda e: e.tensor_scalar(nbias[:, :], c3(cs_b[:, :], NCH)[:, :, CH - 1], -DECAY_C, None, ALU.mult), reads=["cs_b"], writes=["nbias"])
            for ch in range(NCH):
                S.op("act", lambda e, ch=ch: e.activation(out=e4[:, ch * CH:(ch + 1) * CH], in_=cs_b[:, ch * CH:(ch + 1) * CH], func=AF.Exp,
                                                          scale=DECAY_C, bias=nbias[:, ch:ch + 1]), reads=["cs_b", "nbias"], writes=["e4"])
            S.op("dve", lambda e: e.tensor_copy(gamC[:, blk, :], c3(e1[:, :], NCH)[:, :, CH - 1]), reads=["e1"], writes=["gamC"])
            S.op("dve", lambda e: e.tensor_scalar(kq[:, :], k_b[:, :], pch[:, 2, blk:blk + 1], None, ALU.mult), reads=[("k_b", par)], writes=["kq"])
            S.op("act", lambda e: e.activation(out=t1[:, :], in_=kq[:, :], func=AF.Square), reads=["kq"], writes=["t1"])
            b = nb()
            S.op("pe", lambda e, b=b: e.matmul(PS[:, b, 0:TT], lhsT=bones, rhs=t1[:, :], start=True, stop=True), reads=["t1"], writes=[pk(b)])
            S.op("dve", lambda e, b=b: e.tensor_scalar(t1[:, :], PS[:, b, 0:TT], 1e-24, None, ALU.max), reads=[pk(b)], writes=["t1"])
            S.op("act", lambda e: e.activation(out=t1[:, :], in_=t1[:, :], func=AF.Sqrt), reads=["t1"], writes=["t1"])
            S.op("dve", lambda e: e.reciprocal(t1[:, :], t1[:, :]), reads=["t1"], writes=["t1"])
            S.op("dve", lambda e: e.tensor_mul(kkn[:, :], kq[:, :], t1[:, :]), reads=["kq", "t1"], writes=["kkn"])
            S.op("dve", lambda e: e.tensor_scalar(t1[:, :], a_b[:, :], pch[:, 3, blk:blk + 1], ka1[:, blk:blk + 1], ALU.mult, ALU.add),
                 reads=[("a_b", par), "kkn"], writes=["t1"])
            S.op("dve", lambda e: e.tensor_mul(kmod[:, :], k_b[:, :], t1[:, :]), reads=[("k_b", par), "t1"], writes=["kmod"])
            S.op("dve", lambda e: e.tensor_mul(bb[:, :], kkn[:, :], a_b[:, :]), reads=["kkn", ("a_b", par)], writes=["bb"])
            S.op("dve", lambda e: e.scalar_tensor_tensor(AR[:, blk, :, 0:64], c3(kkn[:, :], NCH), -1.0, c3(e2[:, :], NCH), ALU.mult, ALU.mult),
                 reads=["kkn", "e2"], writes=["AR"])
            S.op("dve", lambda e: e.tensor_mul(AR[:, blk, :, 64:128], c3(r_b[:, :], NCH), c3(e1[:, :], NCH)), reads=[("r_b", par), "e1"], writes=["AR"])
            for par in range(2):
                S.op("dve", lambda e, par=par: e.tensor_scalar(ARm[par][:, blk, :, :], AR[:, blk, :, :], headsel[:, par:par + 1], None, ALU.mult), reads=["AR"], writes=["ARm"])
            S.op("dve", lambda e: e.tensor_mul(BK[:, blk, :, 0:64], c3(kmod[:, :], NCH), c3(e3[:, :], NCH)), reads=["kmod", "e3"], writes=["BK"])
            S.op("dve", lambda e: e.tensor_mul(BK[:, blk, :, 64:128], c3(bb[:, :], NCH), c3(e3[:, :], NCH)), reads=["bb", "e3"], writes=["BK"])
            S.op("dve", lambda e: e.tensor_mul(BKp[:, :, 0:64], c3(kmod[:, :], NCH), c3(e4[:, :], NCH)), reads=["kmod", "e4"], writes=["BKp"])
            S.op("dve", lambda e: e.tensor_mul(BKp[:, :, 64:128], c3(bb[:, :], NCH), c3(e4[:, :], NCH)), reads=["bb", "e4"], writes=["BKp"])
            b = nb()

            def f(e, b=b):
                ins = None
                for ch in range(NCH):
                    ins = e.transpose(PS[:, b, ch * 128:(ch + 1) * 128], BKp[:, ch, :], ident)
                return ins
            S.op("pe", f, reads=["BKp"], writes=[pk(b)])
            for hp in range(2):
                S.op("act", lambda e, b=b, hp=hp: e.activation(
                    out=BKtok[:, :, 2 * blk + hp, hp * 64:(hp + 1) * 64],
                    in_=PS[:, b, 0:NCH * 128].rearrange("p (c h j) -> p c h j", c=NCH, h=2)[:, :, hp, :], func=AF.Copy),
                    reads=[pk(b)], writes=["BKtok"])
            b = nb()

            def f(e, b=b):
                ins = None
                for ch in range(NCH):
                    ins = e.transpose(PS[0:64, b, ch * 128:(ch + 1) * 128], v_b[:, ch * CH:(ch + 1) * CH], ident)
                return ins
            S.op("pe", f, reads=[("v_b", par)], writes=[pk(b)])
            S.op("act", lambda e, b=b: e.activation(out=VU[0:64, :, 2 * blk:2 * blk + 2, :],
                                                    in_=PS[0:64, b, 0:NCH * 128].rearrange("p (c h i) -> p c h i", c=NCH, h=2), func=AF.Copy),
                 reads=[pk(b)], writes=["VUv"])
            if own:
                S.op("dve", lambda e: e.scalar_tensor_tensor(rkk[:, :], r_b[:, :], pch[:, 4, blk:blk + 1], kmod[:, :], ALU.mult, ALU.mult),
                     reads=[("r_b", par), "kmod"], writes=["rkk"])

                b = nb()

                def f(e, b=b):
                    ins = None
                    for ch in range(NCH):
                        ins = e.matmul(PS[0:64, b, ch * 128:(ch + 1) * 128], lhsT=rkk[:, ch * CH:(ch + 1) * CH], rhs=bones, start=True, stop=True)
                    return ins
                S.op("pe", f, reads=["rkk"], writes=[pk(b)])
                S.op("dve", lambda e, b=b: e.tensor_copy(bsum[:, :, 2 * blk:2 * blk + 2], PS[0:64, b, 0:NCH * 128].rearrange("p (c h x) -> p c h x", c=NCH, h=2)[:, :, :, 0]),
                     reads=[pk(b)], writes=["bsum"])

        stageA(0)
        for blk in range(8):
            if blk + 1 < 8:
                stageA(blk + 1)
            stageB(blk)
        if stop <= 1.5:
            raise Stop()
        for ch in range(NCH):
            for g4 in range(4):
                b = nb()

                def f(e, b=b, g4=g4):
                    ins = None
                    for j in range(4):
                        h = g4 * 4 + j
                        blk, P0 = h // 2, (h % 2) * 64
                        ins = e.matmul(PS[:, b, j * 128:(j + 1) * 128], lhsT=BK[:, blk, ch, :], rhs=ARm[h % 2][:, blk, ch, :], start=True, stop=True)
                    return ins
                S.op("pe", f, reads=["ARm", "BK"], writes=[pk(b)])
                S.op("dve", lambda e, b=b, g4=g4: e.tensor_mul(AT[:, g4 * 4:(g4 + 1) * 4, :], PS[:, b, :].rearrange("p (h t) -> p h t", h=4), maskA4[:, :, :]),
                     reads=[pk(b)], writes=[("AT", g4)])
            if stop <= 1.6:
                raise Stop()
            for g8 in range(2):
                b = nb2()

                def f(e, b=b, g8=g8):
                    ins = None
                    for j in range(8):
                        h = g8 * 8 + j
                        blk, P0 = h // 2, (h % 2) * 64
                        bb_, off = b + j // 4, (j % 4) * 128
                        e.matmul(PS[0:64, bb_, off:off + 64], lhsT=ARm[h % 2][:, blk, ch, 0:64], rhs=BK[:, blk, ch, 64:128], start=True, stop=True)
                        ins = e.matmul(PS[0:64, bb_, off + 64:off + 128], lhsT=BK[:, blk, ch, 64:128], rhs=ARm[h % 2][:, blk, ch, 0:64], start=True, stop=True)
                    return ins
                S.op("pe", f, reads=["ARm", "BK"], writes=[pk(b), pk(b + 1)])
                for q in range(2):
                    hs = g8 * 8 + q * 4
                    S.op("dve", lambda e, b=b, q=q, hs=hs: e.tensor_mul(PQ[0][:, hs:hs + 4, 0:64], PS[0:64, b + q, :].rearrange("p (h x) -> p h x", h=4)[:, :, 0:64],
                                                                        maskL.unsqueeze(1).to_broadcast([64, 4, 64])), reads=[pk(b + q)], writes=[("PQ0", g8)])
                    S.op("dve", lambda e, b=b, q=q, hs=hs: e.tensor_mul(PQ[0][:, hs:hs + 4, 64:128], PS[0:64, b + q, :].rearrange("p (h x) -> p h x", h=4)[:, :, 64:128],
                                                                        maskU.unsqueeze(1).to_broadcast([64, 4, 64])), reads=[pk(b + q)], writes=[("PQ0", g8)])
                S.op("dve", lambda e, g8=g8: e.tensor_add(Tt[1][:, g8 * 8:(g8 + 1) * 8, :], PQ[0][:, g8 * 8:(g8 + 1) * 8, 64:128],
                                                          ident64.unsqueeze(1).to_broadcast([64, 8, 64])), reads=[("PQ0", g8)], writes=[("Tt1", g8)])
            if stop <= 1.7:
                raise Stop()
            tcur = 1
            for k in range(1, 7):
                src = PQ[(k - 1) % 2]
                dst = PQ[k % 2]
                sk, dk = "PQ%d" % ((k - 1) % 2), "PQ%d" % (k % 2)
                for g8 in range(2):
                    do_pq = k <= 5
                    do_t = k >= 2
                    b = nb2()
                    tsrc, tdst = Tt[tcur], Tt[1 - tcur]

                    def f(e, b=b, g8=g8, src=src, do_pq=do_pq, do_t=do_t, tsrc=tsrc):
                        ins = None
                        for j in range(8):
                            h = g8 * 8 + j
                            bb_, off = b + j // 4, (j % 4) * 128
                            if do_pq:
                                e.matmul(PS[0:64, bb_, off:off + 64], lhsT=src[:, h, 64:128], rhs=src[:, h, 0:64], start=True, stop=True)
                                ins = e.matmul(PS[0:64, bb_, off + 64:off + 128], lhsT=src[:, h, 0:64], rhs=src[:, h, 64:128], start=True, stop=True)
                        return ins
                    if do_pq:
                        S.op("pe", f, reads=[(sk, g8)], writes=[pk(b), pk(b + 1)])
                        for q in range(2):
                            hs = g8 * 8 + q * 4
                            S.op("act", lambda e, b=b, q=q, hs=hs, dst=dst: e.activation(out=dst[:, hs:hs + 4, :], in_=PS[0:64, b + q, :].rearrange("p (h x) -> p h x", h=4), func=AF.Copy),
                                 reads=[pk(b + q)], writes=[(dk, g8)])
                    if do_t:
                        b2 = nb()
                        def f2(e, b2=b2, g8=g8, src=src, tsrc=tsrc):
                            ins = None
                            for j in range(8):
                                h = g8 * 8 + j
                                ins = e.matmul(PS[0:64, b2, j * 64:(j + 1) * 64], lhsT=src[:, h, 0:64], rhs=tsrc[:, h, :], start=True, stop=True)
                            return ins
                        S.op("pe", f2, reads=[(sk, g8), ("Tt%d" % tcur, g8)], writes=[pk(b2)])
                        if k < 6:
                            S.op("dve", lambda e, b2=b2, g8=g8, tsrc=tsrc, tdst=tdst: e.tensor_add(
                                tdst[:, g8 * 8:(g8 + 1) * 8, :], tsrc[:, g8 * 8:(g8 + 1) * 8, :], PS[0:64, b2, :].rearrange("p (h x) -> p h x", h=8)),
                                reads=[pk(b2), ("Tt%d" % tcur, g8)], writes=[("Tt%d" % (1 - tcur), g8)])
                        else:
                            S.op("dve", lambda e, b2=b2, g8=g8, tsrc=tsrc: e.tensor_add(
                                TtF[:, g8 * 8:(g8 + 1) * 8, 64:128], tsrc[:, g8 * 8:(g8 + 1) * 8, :], PS[0:64, b2, :].rearrange("p (h x) -> p h x", h=8)),
                                reads=[pk(b2), ("Tt%d" % tcur, g8)], writes=[("TtF", g8)])
                if k >= 2:
                    tcur = 1 - tcur
            if stop <= 1.8:
                raise Stop()
            bw = nb2()

            def f(e, bw=bw):
                ins = None
                for h in range(16):
                    blk, P0 = h // 2, (h % 2) * 64
                    bb_, off = bw + h // 8, (h % 8) * 64
                    e.matmul(PS[0:64, bb_, off:off + 64], lhsT=ARm[h % 2][:, blk, ch, 0:64], rhs=STb[:, blk, :], start=True, stop=False)
                    ins = e.matmul(PS[0:64, bb_, off:off + 64], lhsT=AT[0:64, h, 0:64], rhs=VU[0:64, ch, h, :], start=False, stop=True)
                return ins
            S.op("pe", f, reads=["ARm", "STb", "VUv"] + [("AT", g) for g in range(4)], writes=[pk(bw), pk(bw + 1)])
            for q in range(2):
                S.op("dve", lambda e, q=q, bw=bw: e.tensor_copy(Wsb[:, q * 8:(q + 1) * 8, :], PS[0:64, bw + q, :].rearrange("p (h x) -> p h x", h=8)),
                     reads=[pk(bw + q)], writes=[("Wsb", q)])
            bu = nb2()

            def f(e, bu=bu):
                ins = None
                for h in range(16):
                    bb_, off = bu + h // 8, (h % 8) * 64
                    ins = e.matmul(PS[:, bb_, off:off + 64], lhsT=TtF[:, h, :], rhs=Wsb[:, h, :], start=True, stop=True)
                return ins
            S.op("pe", f, reads=[("Wsb", 0), ("Wsb", 1), ("TtF", 0), ("TtF", 1)], writes=[pk(bu), pk(bu + 1)])
            for q in range(2):
                S.op("dve", lambda e, q=q, bu=bu: e.tensor_copy(VU[64:128, ch, q * 8:(q + 1) * 8, :], PS[64:128, bu + q, :].rearrange("p (h x) -> p h x", h=8)),
                     reads=[pk(bu + q)], writes=[("VUu", q)])
            vu_keys = ["VUv", ("VUu", 0), ("VUu", 1)]
            if own:
                by = nb2()

                def f(e, by=by):
                    ins = None
                    for h in range(16):
                        blk, P0 = h // 2, (h % 2) * 64
                        bb_, off = by + h // 8, (h % 8) * 64
                        e.matmul(PS[0:64, bb_, off:off + 64], lhsT=AT[:, h, 64:128], rhs=VU[:, ch, h, :], start=True, stop=False)
                        ins = e.matmul(PS[0:64, bb_, off:off + 64], lhsT=ARm[h % 2][:, blk, ch, 64:128], rhs=STb[:, blk, :], start=False, stop=True)
                    return ins
                S.op("pe", f, reads=["ARm", "STb"] + vu_keys + [("AT", g) for g in range(4)], writes=[pk(by), pk(by + 1)])
            if stop <= 1.9:
                raise Stop()
            bs = nb()

            def f(e, bs=bs):
                ins = None
                for blk in range(8):
                    e.matmul(PS[:, bs, blk * 64:(blk + 1) * 64], lhsT=BKtok[:, ch, 2 * blk, :], rhs=VU[:, ch, 2 * blk, :], start=True, stop=False)
                    ins = e.matmul(PS[:, bs, blk * 64:(blk + 1) * 64], lhsT=BKtok[:, ch, 2 * blk + 1, :], rhs=VU[:, ch, 2 * blk + 1, :], start=False, stop=True)
                return ins
            S.op("pe", f, reads=["BKtok"] + vu_keys, writes=[pk(bs)])
            for blk in range(8):
                S.op("dve", lambda e, blk=blk, bs=bs: e.scalar_tensor_tensor(ST[:, blk, :], ST[:, blk, :], gamC[:, blk, ch:ch + 1], PS[:, bs, blk * 64:(blk + 1) * 64], ALU.mult, ALU.add),
                     reads=[pk(bs), "gamC", "ST"], writes=["ST"])
            S.op("act", lambda e: e.activation(out=STb[:, :, :], in_=ST[:, :, :], func=AF.Copy), reads=["ST"], writes=["STb"])
            if own:
                for q in range(2):
                    ysrc = PS[0:64, by + q, :].rearrange("p (h x) -> p h x", h=8)
                    hs = slice(q * 8, (q + 1) * 8)
                    S.op("dve", lambda e, ysrc=ysrc, hs=hs: e.tensor_reduce(gst[:, 0, hs], ysrc, AX.X, ALU.add), reads=[pk(by + q)], writes=[("gst0", q)])
                    S.op("act", lambda e, q=q: e.activation(out=ysq[:, q * 512:(q + 1) * 512], in_=PS[0:64, by + q, :], func=AF.Square), reads=[pk(by + q)], writes=[("ysq", q)])
                    S.op("dve", lambda e, q=q, hs=hs: e.tensor_reduce(gst[:, 1, hs], ysq[:, q * 512:(q + 1) * 512].rearrange("p (h x) -> p h x", h=8), AX.X, ALU.add),
                         reads=[("ysq", q)], writes=[("gst1", q)])
                gk = [("gst0", 0), ("gst0", 1), ("gst1", 0), ("gst1", 1)]
                S.op("dve", lambda e: e.tensor_scalar(gst[:, 0, :], gst[:, 0, :], 1.0 / 64, None, ALU.mult), reads=gk, writes=["gstm"])
                S.op("dve", lambda e: e.tensor_mul(gst[:, 2, :], gst[:, 0, :], gst[:, 0, :]), reads=["gstm"], writes=["gst2"])
                S.op("dve", lambda e: e.scalar_tensor_tensor(gst[:, 3, :], gst[:, 1, :], 1.0 / 64, gst[:, 2, :], ALU.mult, ALU.subtract), reads=gk + ["gst2"], writes=["gst3"])
                S.op("dve", lambda e: e.tensor_scalar(gst[:, 3, :], gst[:, 3, :], 64e-5, None, ALU.add), reads=["gst3"], writes=["gst3"])
                S.op("act", lambda e: e.activation(out=gst[:, 3, :], in_=gst[:, 3, :], func=AF.Sqrt), reads=["gst3"], writes=["gst3"])
                S.op("dve", lambda e: e.reciprocal(gst[:, 3, :], gst[:, 3, :]), reads=["gst3"], writes=["gst3"])
                for q in range(2):
                    ysrc = PS[0:64, by + q, :].rearrange("p (h x) -> p h x", h=8)
                    hs = slice(q * 8, (q + 1) * 8)
                    yd = yn[:, q * 512:(q + 1) * 512].rearrange("p (h x) -> p h x", h=8)
                    S.op("dve", lambda e, ysrc=ysrc, hs=hs, yd=yd: e.tensor_sub(yd, ysrc, gst[:, 0, hs].unsqueeze(2).to_broadcast([64, 8, 64])),
                         reads=[pk(by + q), "gstm"], writes=[("yn", q)])
                    S.op("dve", lambda e, hs=hs, yd=yd: e.tensor_mul(yd, yd, gst[:, 3, hs].unsqueeze(2).to_broadcast([64, 8, 64])),
                         reads=["gst3", ("yn", q)], writes=[("yn", q)])
                ynk = [("yn", 0), ("yn", 1)]
                S.op("dve", lambda e: e.tensor_mul(yn[:, :], yn[:, :], gnw[:, :]), reads=ynk, writes=ynk)
                S.op("dve", lambda e: e.tensor_add(yn[:, :], yn[:, :], gnb[:, :]), reads=ynk, writes=ynk)
                S.op("dve", lambda e: e.tensor_mul(ysq[:, :].rearrange("p (h x) -> p h x", h=16), VU[0:64, ch, :, :], bsum[:, ch, :].unsqueeze(2).to_broadcast([64, 16, 64])),
                     reads=["VUv", "bsum", ("ysq", 0), ("ysq", 1)], writes=[("ysq", 0), ("ysq", 1)])
                S.op("dve", lambda e: e.tensor_add(yn[:, :], yn[:, :], ysq[:, :]), reads=ynk + [("ysq", 0), ("ysq", 1)], writes=ynk)
                bg = nb2()

                def f(e, bg=bg):
                    ins = None
                    for half in range(2):
                        for kk_ in range(2):
                            ins = e.matmul(PS[0:64, bg + half, :], lhsT=sgT[:, kk_, ch * CH:(ch + 1) * CH], rhs=g2s[:, kk_, half * 512:(half + 1) * 512], start=(kk_ == 0), stop=(kk_ == 1))
                    return ins
                S.op("pe", f, reads=["sgT"], writes=[pk(bg), pk(bg + 1)])
                for half in range(2):
                    S.op("dve", lambda e, half=half, bg=bg: e.tensor_mul(yn[:, half * 512:(half + 1) * 512], yn[:, half * 512:(half + 1) * 512], PS[0:64, bg + half, :]),
                         reads=[pk(bg + half)] + ynk, writes=ynk)
                bt = nb()

                def f(e, bt=bt):
                    ins = None
                    for blk in range(8):
                        ins = e.transpose(PS[:, bt, blk * 64:(blk + 1) * 64], yn[:, blk * 128:(blk + 1) * 128], ident64)
                    return ins
                S.op("pe", f, reads=ynk, writes=[pk(bt)])
                S.op("act", lambda e, bt=bt: e.activation(out=a_outT[:, :, ch * CH:(ch + 1) * CH], in_=PS[:, bt, :].rearrange("p (b t) -> p b t", b=8), func=AF.Copy),
                     reads=[pk(bt)], writes=["a_outT"])
                if dbg:
                    S.op("dve", lambda e, bt=bt: e.tensor_copy(junk[:, 0:512], PS[:, bt, :]), reads=[pk(bt)], writes=["junk"])
                    for blk in range(8):
                        S.dma("sp", lambda e, blk=blk: e.dma_start(out=dbg_t["aout"][blk * 128:(blk + 1) * 128, oi * TT + ch * CH:oi * TT + (ch + 1) * CH],
                                                                    in_=junk[:, blk * 64:(blk + 1) * 64]), reads=["junk"])
        phase_end(ph)
        if stop <= 2 or (own and stop <= 2.5):
            raise Stop()
        ph = phase_begin()
        qsq = sb("qsq", [128, TT])
        qrs = sb("qrs", [128, TT])
        qT = sb("qT", [128, 8, TT], BF16)
        qTm = [sb("qTm%d" % i_, [128, 8, TT], BF16) for i_ in range(2)]
        Ecur = sb("Ecur", [128, 16, 128], BF16)
        Eprev = sb("Eprev", [128, 16, 128], BF16)
        den = sb("den", [128, 4, 128])
        if need_kv:
            for s in range(NS):
                kb = ti * NS + s
                sl = kb % 3
                for g in range(4):
                    b = mm_block(w_in, [(C_KA + g * 64, 64, 0), (C_KA + g * 64, 64, 64)], 16, 128, lambda kc: hT[:, kc, s * 128:(s + 1) * 128], 128, ["hT"], "ka")
                    S.op("act", lambda e, b=b: e.activation(out=qsq[:, 0:128], in_=PS[:, b, 0:128], func=AF.Square), reads=[pk(b)], writes=["qsq"])
                    b2 = nb()
                    S.op("pe", lambda e, b2=b2: e.matmul(PS[:, b2, 0:128], lhsT=bones, rhs=qsq[:, 0:128], start=True, stop=True), reads=["qsq"], writes=[pk(b2)])
                    S.op("dve", lambda e, b2=b2: e.tensor_scalar(qrs[:, 0:128], PS[:, b2, 0:128], 1.0 / 64, 1e-6, ALU.mult, ALU.add), reads=[pk(b2)], writes=["qrs"])
                    S.op("act", lambda e: e.activation(out=qrs[:, 0:128], in_=qrs[:, 0:128], func=AF.Sqrt), reads=["qrs"], writes=["qrs"])
                    S.op("dve", lambda e: e.reciprocal(qrs[:, 0:128], qrs[:, 0:128]), reads=["qrs"], writes=["qrs"])
                    S.op("dve", lambda e, b=b, g=g, sl=sl: e.scalar_tensor_tensor(kTs[:, sl, g, :], PS[:, b, 0:128], qkg[:, 1:2], qrs[:, 0:128], ALU.mult, ALU.mult),
                         reads=[pk(b), "qrs"], writes=["kTs%d" % sl])
                wi = wb_ctr[0]
                wb_ctr[0] = (wi + 1) % NWB
                wb = wbuf[wi]
                for hf in range(2):
                    load_wblock(("va", hf), wi, 16, lambda hf=hf, wb=wb, wi=wi: S.dma("pool", lambda e: e.dma_start(
                        out=wb[:, :, 0:128], in_=w_in[:, C_VA + hf * 128:C_VA + (hf + 1) * 128].rearrange("(kc p) c -> p kc c", p=128)), writes=[("wb", wi)]))
                    b = nb()

                    def f(e, b=b, wb=wb):
                        ins = None
                        for kc in range(16):
                            ins = e.matmul(PS[:, b, 0:128], lhsT=hT[:, kc, s * 128:(s + 1) * 128], rhs=wb[:, kc, 0:128], start=(kc == 0), stop=(kc == 15))
                        return ins
                    S.op("pe", f, reads=[("wb", wi), "hT"], writes=[pk(b)])
                    for gg in range(2):
                        g = hf * 2 + gg
                        S.op("act", lambda e, b=b, g=g, gg=gg, sl=sl: e.activation(out=Vpad[:, sl, g, 0, 0:64], in_=PS[:, b, gg * 64:(gg + 1) * 64], func=AF.Copy),
                             reads=[pk(b)], writes=["Vp%d" % sl])
                        S.op("act", lambda e, b=b, g=g, gg=gg, sl=sl: e.activation(out=Vpad[:, sl, g, 1, 64:128], in_=PS[:, b, gg * 64:(gg + 1) * 64], func=AF.Copy),
                             reads=[pk(b)], writes=["Vp%d" % sl])
        if own and stop <= 2.7:
            raise Stop()
        if own:
            for qb in range(8):
                b = mm_block(w_in, [(C_Q + qb * 128, 128, 0)], 16, 128, hT_rhs, TT, ["hT"], "q")
                S.op("act", lambda e, b=b: e.activation(out=qsq[:, :], in_=PS[:, b, 0:TT], func=AF.Square), reads=[pk(b)], writes=["qsq"])
                b2 = nb()
                S.op("pe", lambda e, b2=b2: e.matmul(PS[:, b2, 0:TT], lhsT=bones, rhs=qsq[:, :], start=True, stop=True), reads=["qsq"], writes=[pk(b2)])
                S.op("dve", lambda e, b2=b2: e.tensor_scalar(qrs[:, :], PS[:, b2, 0:TT], 1.0 / 64, 1e-6, ALU.mult, ALU.add), reads=[pk(b2)], writes=["qrs"])
                S.op("act", lambda e: e.activation(out=qrs[:, :], in_=qrs[:, :], func=AF.Sqrt), reads=["qrs"], writes=["qrs"])
                S.op("dve", lambda e: e.reciprocal(qrs[:, :], qrs[:, :]), reads=["qrs"], writes=["qrs"])
                S.op("dve", lambda e, b=b, qb=qb: e.scalar_tensor_tensor(qT[:, qb, :], PS[:, b, 0:TT], qkg[:, 0:1], qrs[:, :], ALU.mult, ALU.mult),
                     reads=[pk(b), "qrs"], writes=["qT"])
                for par in range(2):
                    S.op("dve", lambda e, par=par, qb=qb: e.tensor_scalar(qTm[par][:, qb, :], qT[:, qb, :], headsel[:, par:par + 1], None, ALU.mult), reads=["qT"], writes=["qTm"])
            for s in range(NS):
                kb = ti * NS + s
                slc, slp = kb % 3, (kb - 1) % 3
                first = (oi == 0 and s == 0)
                for g4 in range(4):
                    for (sl, Ed, msk, nm) in ((slp, Eprev, mprev, "Ep"), (slc, Ecur, mcur, "Ec")):
                        b = nb()

                        def f(e, b=b, g4=g4, sl=sl):
                            ins = None
                            for j in range(4):
                                h = g4 * 4 + j
                                P0 = (h % 2) * 64
                                ins = e.matmul(PS[:, b, j * 128:(j + 1) * 128], lhsT=kTs[:, sl, h // 4, :], rhs=qTm[h % 2][:, h // 2, s * 128:(s + 1) * 128], start=True, stop=True)
                            return ins
                        S.op("pe", f, reads=["kTs%d" % sl, "qTm"], writes=[pk(b)])
                        S.op("act", lambda e, b=b, g4=g4, Ed=Ed: e.activation(out=Ed[:, g4 * 4:(g4 + 1) * 4, :], in_=PS[:, b, :].rearrange("p (h q) -> p h q", h=4), func=AF.Exp, scale=0.125),
                             reads=[pk(b)], writes=[(nm, g4)])
                        S.op("dve", lambda e, g4=g4, Ed=Ed, msk=msk: e.tensor_mul(Ed[:, g4 * 4:(g4 + 1) * 4, :], Ed[:, g4 * 4:(g4 + 1) * 4, :], msk[:, :, :]),
                             reads=[(nm, g4)], writes=[(nm, g4)])
                        if first and nm == "Ep":
                            S.op("dve", lambda e, g4=g4, Ed=Ed: e.tensor_scalar(Ed[:, g4 * 4:(g4 + 1) * 4, :], Ed[:, g4 * 4:(g4 + 1) * 4, :], flag[:, 0:1], None, ALU.mult),
                                 reads=[(nm, g4)], writes=[(nm, g4)])
                ek = [("Ep", g) for g in range(4)] + [("Ec", g) for g in range(4)]
                for half in range(2):
                    bo = nb()
                    bd = nb()

                    def f(e, bo=bo, bd=bd, half=half):
                        ins = None
                        for jb in range(4):
                            pb = half * 4 + jb
                            g = pb // 2
                            o = PS[:, bo, jb * 128:(jb + 1) * 128]
                            d = PS[:, bd, jb * 128:(jb + 1) * 128]
                            e.matmul(o, lhsT=Vpad[:, slp, g, 0, :], rhs=Eprev[:, 2 * pb, :], start=True, stop=False)
                            e.matmul(o, lhsT=Vpad[:, slp, g, 1, :], rhs=Eprev[:, 2 * pb + 1, :], start=False, stop=False)
                            e.matmul(o, lhsT=Vpad[:, slc, g, 0, :], rhs=Ecur[:, 2 * pb, :], start=False, stop=False)
                            e.matmul(o, lhsT=Vpad[:, slc, g, 1, :], rhs=Ecur[:, 2 * pb + 1, :], start=False, stop=True)
                            e.matmul(d, lhsT=onesL[:, :], rhs=Eprev[:, 2 * pb, :], start=True, stop=False)
                            e.matmul(d, lhsT=onesR[:, :], rhs=Eprev[:, 2 * pb + 1, :], start=False, stop=False)
                            e.matmul(d, lhsT=onesL[:, :], rhs=Ecur[:, 2 * pb, :], start=False, stop=False)
                            ins = e.matmul(d, lhsT=onesR[:, :], rhs=Ecur[:, 2 * pb + 1, :], start=False, stop=True)
                        return ins
                    S.op("pe", f, reads=ek + ["Vp%d" % slp, "Vp%d" % slc], writes=[pk(bo), pk(bd)])
                    S.op("dve", lambda e, bd=bd, half=half: e.tensor_add(den[:, :, :], PS[:, bd, :].rearrange("p (b q) -> p b q", b=4),
                                                                         esink[:, half * 4:(half + 1) * 4].unsqueeze(2).to_broadcast([128, 4, 128])),
                         reads=[pk(bd), "esink"], writes=["den"])
                    S.op("dve", lambda e: e.reciprocal(den[:, :, :], den[:, :, :]), reads=["den"], writes=["den"])
                    S.op("dve", lambda e, bo=bo, half=half: e.tensor_mul(b_outT[:, half * 4:(half + 1) * 4, s * 128:(s + 1) * 128], PS[:, bo, :].rearrange("p (b q) -> p b q", b=4), den[:, :, :]),
                         reads=[pk(bo), "den"], writes=["b_outT"])
            if dbg:
                for blk in range(8):
                    S.op("dve", lambda e, blk=blk: e.tensor_copy(junk[:, 0:TT], b_outT[:, blk, :]), reads=["b_outT"], writes=["junk"])
                    S.dma("sp", lambda e, blk=blk: e.dma_start(out=dbg_t["bout"][blk * 128:(blk + 1) * 128, oi * TT:(oi + 1) * TT], in_=junk[:, 0:TT]), reads=["junk"])
        phase_end(ph)
        if not own:
            return
        if stop <= 3:
            raise Stop()
        ph = phase_begin()
        xin = sb("xin", [128, D])
        xn = xin
        junk = sb("junk", [128, D])
        sga = sb("sga", [128, TT])
        sgb = sb("sgb", [128, TT])
        mt = sb("mt", [128, TT])
        mergedT = sb("mergedT", [128, 16, TT], BF16)
        x1 = xn
        h2 = junk
        h2bf = sb("h2bf", [128, D], BF16)
        h2T = sb("h2T", [128, 16, 128])
        lg = sb("lg", [128, NRL])
        rsm = sb("rsm", [128, 16])
        gmask = sb("gmask", [128, 8])
        fsel3 = sb("fsel3", [128, NG, 8])
        fsel = sb("fsel", [128, 8])
        fm1 = sb("fm1", [128, 8])
        fm2 = sb("fm2", [128, 8])
        fmk = sb("fmk", [128, 8])
        M1 = sb("M1", [128, NG, 8])
        M2 = sb("M2", [128, NG, 8])
        Mt = sb("Mt", [128, NG, 8])
        posT = sb("posT", [128, NE])
        ptmp = sb("ptmp", [128, NE])
        dstf = sb("dstf", [128, 4])
        for c in range(16):
            b = mm_block(w_in, [(C_GA + c * 128, 128, 0)], 16, 128, hT_rhs, TT, ["hT"], "ga")
            S.op("act", lambda e, b=b: e.activation(out=sga[:, :], in_=PS[:, b, 0:TT], func=AF.Sigmoid), reads=[pk(b)], writes=["sga"])
            b = mm_block(w_in, [(C_GB + c * 128, 128, 0)], 16, 128, hT_rhs, TT, ["hT"], "gb")
            S.op("act", lambda e, b=b: e.activation(out=sgb[:, :], in_=PS[:, b, 0:TT], func=AF.Sigmoid), reads=[pk(b)], writes=["sgb"])
            b = mm_block(proj_r, [(c * 128, 128, 0)], 8, 128, lambda kc: a_outT[:, kc, :], TT, ["a_outT"], "pr")
            S.op("dve", lambda e, b=b: e.tensor_mul(mt[:, :], sga[:, :], PS[:, b, 0:TT]), reads=[pk(b), "sga"], writes=["mt"])
            b = mm_block(proj_a, [(c * 128, 128, 0)], 8, 128, lambda kc: b_outT[:, kc, :], TT, ["b_outT"], "pa")
            S.op("dve", lambda e, b=b: e.tensor_mul(sgb[:, :], sgb[:, :], PS[:, b, 0:TT]), reads=[pk(b), "sgb"], writes=["sgb"])
            S.op("dve", lambda e, c=c: e.tensor_add(mergedT[:, c, :], mt[:, :], sgb[:, :]), reads=["mt", "sgb"], writes=["mergedT"])
        for s in range(NS):
            st_i = oi * NS + s
            row0 = oi * TT + s * 128
            S.dma("sp", lambda e: e.dma_start(out=xin[:, :], in_=xs[tok0 + s * 128:tok0 + (s + 1) * 128, :]), writes=["xin"])
            for n in range(4):
                bo = nb()
                for j in range(4):
                    c = n * 4 + j
                    wi = wb_ctr[0]
                    wb_ctr[0] = (wi + 1) % NWB
                    wb = wbuf[wi]
                    load_wblock(("wo", c), wi, 16, lambda c=c, wb=wb, wi=wi: S.dma("pool", lambda e: e.dma_start(
                        out=wb[:, :, :], in_=w_out[:, c * 128:(c + 1) * 128].rearrange("(kc p) c -> p kc c", p=128)), writes=[("wb", wi)]))

                    def f(e, wb=wb, j=j, bo=bo):
                        ins = None
                        for kc in range(16):
                            ins = e.matmul(PS[:, bo, j * 128:(j + 1) * 128], lhsT=mergedT[:, kc, s * 128:(s + 1) * 128], rhs=wb[:, kc, :], start=(kc == 0), stop=(kc == 15))
                        return ins
                    S.op("pe", f, reads=[("wb", wi), "mergedT"], writes=[pk(bo)])
                S.op("dve", lambda e, bo=bo, n=n: e.tensor_add(x1[:, n * 512:(n + 1) * 512], PS[:, bo, :], xin[:, n * 512:(n + 1) * 512]),
                     reads=[pk(bo), "xin"], writes=["xin"])
            S.dma("sp", lambda e: e.dma_start(out=y_out[row0:row0 + 128, :], in_=x1[:, :]), reads=["xin"], writes=[("yrow", st_i)])
            if dbg:
                S.dma("sp", lambda e: e.dma_start(out=dbg_t["xin"][row0:row0 + 128, :], in_=x1[:, :]), reads=["xin"])
            S.op("act", lambda e: e.activation(out=junk[:, :], in_=x1[:, :], func=AF.Square, accum_out=st1[:, 4:5]), reads=["xin"], writes=["junk", "st1d"])
            S.op("dve", lambda e: e.tensor_scalar(st1[:, 5:6], st1[:, 4:5], 1.0 / D, 1e-6, ALU.mult, ALU.add), reads=["st1d"], writes=["st1e"])
            S.op("act", lambda e: e.activation(out=st1[:, 6:7], in_=st1[:, 5:6], func=AF.Sqrt), reads=["st1e"], writes=["st1f"])
            S.op("dve", lambda e: e.reciprocal(st1[:, 6:7], st1[:, 6:7]), reads=["st1f"], writes=["st1f"])
            S.op("dve", lambda e: e.scalar_tensor_tensor(h2[:, :], x1[:, :], st1[:, 6:7], g2bc[:, :], ALU.mult, ALU.mult), reads=["xin", "st1f"], writes=["junk"])
            S.op("act", lambda e: e.activation(out=h2bf[:, :], in_=h2[:, :], func=AF.Copy), reads=["junk"], writes=["h2bf"])
            for g in range(4):
                b = nb()

                def f(e, g=g, b=b):
                    ins = None
                    for j in range(4):
                        kc = g * 4 + j
                        ins = e.transpose(PS[:, b, j * 128:(j + 1) * 128], h2[:, kc * 128:(kc + 1) * 128], ident)
                    return ins
                S.op("pe", f, reads=["junk"], writes=[pk(b)])
                S.op("act", lambda e, g=g, b=b: e.activation(out=h2T[:, g * 4:(g + 1) * 4, :], in_=PS[:, b, :].rearrange("p (j t) -> p j t", j=4), func=AF.Copy),
                     reads=[pk(b)], writes=ATK)
            b = nb()

            def f(e, b=b):
                ins = None
                for kc in range(16):
                    ins = e.matmul(PS[:, b, 0:NRL], lhsT=h2T[:, kc, :], rhs=wr[:, kc, :], start=(kc == 0), stop=(kc == 15))
                return ins
            S.op("pe", f, reads=ATK, writes=[pk(b)])
            S.op("dve", lambda e, b=b: e.tensor_add(lg[:, :], PS[:, b, 0:NRL], rbias[:, :]), reads=[pk(b)], writes=["lg"])
            R = "rt"
            S.op("dve", lambda e: e.tensor_reduce(rsm[:, 0:1], lg[:, 0:NG], AX.X, ALU.max), reads=["lg"], writes=[R])
            S.op("dve", lambda e: e.tensor_scalar(gmask[:, 0:NG], lg[:, 0:NG], rsm[:, 0:1], None, ALU.is_equal), reads=["lg", R], writes=[R])
            S.op("dve", lambda e: e.tensor_scalar(rsm[:, 1:2], rsm[:, 0:1], -1.0, None, ALU.mult), reads=[R], writes=[R])
            S.op("act", lambda e: e.activation(out=fm1[:, 0:NG], in_=lg[:, 0:NG], func=AF.Exp, bias=rsm[:, 1:2], accum_out=rsm[:, 2:3]), reads=["lg", R], writes=[R])
            S.op("dve", lambda e: e.reciprocal(rsm[:, 3:4], rsm[:, 2:3]), reads=[R], writes=[R])
            S.op("dve", lambda e: e.tensor_mul(fsel3[:, :, :], lg[:, 8:NRL].rearrange("p (g x) -> p g x", g=NG), gmask[:, 0:NG].unsqueeze(2).to_broadcast([128, NG, 8])),
                 reads=["lg", R], writes=[R])
            S.op("dve", lambda e: e.tensor_reduce(fsel[:, :], fsel3[:, :, :].rearrange("p g x -> p x g"), AX.X, ALU.add), reads=[R], writes=[R])
            S.op("dve", lambda e: e.tensor_reduce(rsm[:, 4:5], fsel[:, :], AX.X, ALU.max), reads=[R], writes=[R])
            S.op("dve", lambda e: e.tensor_scalar(fm1[:, :], fsel[:, :], rsm[:, 4:5], None, ALU.is_equal), reads=[R], writes=[R])
            S.op("dve", lambda e: e.scalar_tensor_tensor(fmk[:, :], fm1[:, :], -1e30, fsel[:, :], ALU.mult, ALU.add), reads=[R], writes=[R])
            S.op("dve", lambda e: e.tensor_reduce(rsm[:, 5:6], fmk[:, :], AX.X, ALU.max), reads=[R], writes=[R])
            S.op("dve", lambda e: e.tensor_scalar(fm2[:, :], fmk[:, :], rsm[:, 5:6], None, ALU.is_equal), reads=[R], writes=[R])
            S.op("dve", lambda e: e.tensor_sub(rsm[:, 6:7], rsm[:, 5:6], rsm[:, 4:5]), reads=[R], writes=[R])
            S.op("act", lambda e: e.activation(out=rsm[:, 7:8], in_=rsm[:, 6:7], func=AF.Exp), reads=[R], writes=[R])
            S.op("dve", lambda e: e.tensor_scalar(rsm[:, 8:9], rsm[:, 7:8], 1.0, None, ALU.add), reads=[R], writes=[R])
            S.op("dve", lambda e: e.reciprocal(rsm[:, 8:9], rsm[:, 8:9]), reads=[R], writes=[R])
            S.op("dve", lambda e: e.tensor_mul(rsm[:, 9:10], rsm[:, 8:9], rsm[:, 3:4]), reads=[R], writes=[R])
            S.op("dve", lambda e: e.tensor_mul(rsm[:, 10:11], rsm[:, 9:10], rsm[:, 7:8]), reads=[R], writes=[R])
            S.op("dve", lambda e: e.tensor_mul(M1[:, :, :], gmask[:, 0:NG].unsqueeze(2).to_broadcast([128, NG, 8]), fm1[:, :].unsqueeze(1).to_broadcast([128, NG, 8])), reads=[R], writes=[R])
            S.op("dve", lambda e: e.tensor_mul(M2[:, :, :], gmask[:, 0:NG].unsqueeze(2).to_broadcast([128, NG, 8]), fm2[:, :].unsqueeze(1).to_broadcast([128, NG, 8])), reads=[R], writes=[R])
            S.op("dve", lambda e: e.tensor_add(Mt[:, :, :], M1[:, :, :], M2[:, :, :]), reads=[R], writes=["Mt"])
            Mt2 = Mt[:, :, :].rearrange("p g x -> p (g x)")
            b = nb()
            S.op("pe", lambda e, b=b: e.matmul(PS[:, b, 0:NE], lhsT=tri_strict, rhs=Mt2, start=True, stop=True), reads=["Mt"], writes=[pk(b)])
            S.op("dve", lambda e, b=b: e.tensor_add(posT[:, :], PS[:, b, 0:NE], base_bc[:, :]), reads=[pk(b), "base"], writes=["posT"])
            b = nb()
            S.op("pe", lambda e, b=b: e.matmul(PS[:, b, 0:NE], lhsT=ones_m, rhs=Mt2, start=True, stop=True), reads=["Mt"], writes=[pk(b)])
            S.op("dve", lambda e, b=b: e.tensor_add(base_bc[:, :], base_bc[:, :], PS[:, b, 0:NE]), reads=[pk(b), "posT"], writes=["base"])
            S.op("dve", lambda e: e.tensor_scalar(ptmp[:, :], posT[:, :], float(CAP), None, ALU.is_ge), reads=["posT"], writes=["ptmp"])
            S.op("dve", lambda e: e.tensor_add(posT[:, :], posT[:, :], ecap[:, :]), reads=["posT"], writes=["posT"])
            S.op("dve", lambda e: e.scalar_tensor_tensor(posT[:, :], ptmp[:, :], 1e7, posT[:, :], ALU.mult, ALU.add), reads=["posT", "ptmp"], writes=["posT"])
            for kx, Mx in enumerate((M1, M2)):
                S.op("dve", lambda e, Mx=Mx: e.tensor_mul(ptmp[:, :], posT[:, :], Mx[:, :, :].rearrange("p g x -> p (g x)")), reads=["posT", R], writes=["ptmp"])
                S.op("dve", lambda e, kx=kx: e.tensor_reduce(dstf[:, kx:kx + 1], ptmp[:, :], AX.X, ALU.add), reads=["ptmp"], writes=["dstf"])
            S.op("dve", lambda e: e.tensor_scalar(dstf[:, 2:4], dstf[:, 0:2], 1e6, None, ALU.is_lt), reads=["dstf"], writes=["dstf2"])
            S.op("dve", lambda e: e.tensor_mul(rt_w[:, st_i, :], rsm[:, 9:11], dstf[:, 2:4]), reads=[R, "dstf2"], writes=[("rtw", st_i)])
            S.op("dve", lambda e: e.tensor_copy(rt_d[:, st_i, :], dstf[:, 0:2]), reads=["dstf"], writes=[("rtd", st_i)])
            if dbg:
                S.op("dve", lambda e: e.tensor_copy(junk[:, 0:2], dstf[:, 0:2]), reads=["dstf"], writes=["junk"])
                S.op("dve", lambda e: e.tensor_copy(junk[:, 2:4], rt_w[:, st_i, :]), reads=[("rtw", st_i)], writes=["junk"])
                S.op("dve", lambda e: e.tensor_copy(junk[:, 4:8], rsm[:, 0:4]), reads=[R], writes=["junk"])
                S.dma("sp", lambda e: e.dma_start(out=dbg_t["rt"][row0:row0 + 128, :], in_=junk[:, 0:8]), reads=["junk"])
            for kx in range(2):
                S.dma("pool", lambda e, kx=kx: e.indirect_dma_start(
                    out=Xd[:, :], out_offset=bass.IndirectOffsetOnAxis(ap=rt_d[:, st_i, kx:kx + 1], axis=0),
                    in_=h2bf[:, :], in_offset=None, bounds_check=bc_reg, oob_is_err=False),
                    reads=["h2bf", ("rtd", st_i)] + xdz_keys, writes=[("xd", st_i, kx)])
        phase_end(ph)

    S.barrier()
    print("sbuf remaining after mixer alloc:", nc.sbuf_bytes_remaining)
    if stop <= 0:
        S.stack.close()
        return nc
    try:
        for ti in range(NT):
            mixer_tile(ti)
    except Stop:
        S.barrier()
        S.stack.close()
        return nc
    S.barrier()
    if stop <= 4:
        S.stack.close()
        return nc
    scope[0].close()
    scope[0] = ExitStack()

    xd_keys = [("xd", i, k) for i in range(NSUB) for k in range(2)]
    bank_lo[0] = 4
    bank_ctr[0] = 4
    Xe = sb("Xe", [128, D], BF16)
    XeT = sb("XeT", [128, 16, 128], BF16)
    identb = sb("identb", [128, 128], BF16)
    S.op("dve", lambda e: e.tensor_copy(identb[:, :], ident), writes=["identb"])
    NPB_ = 6
    pbuf = [sb("pb%d" % i, [128, 16, 256], BF16) for i in range(NPB_)]
    pb_ctr = [0]
    actT = sb("actT", [128, 8, 128], BF16)
    sil = sb("sil", [128, 256])
    act_tok = sb("act_tok", [128, 1024], BF16)
    Ye = sb("Ye", [128, D])
    x1 = sb("x1m", [128, D])

    def nxt_pb():
        i = pb_ctr[0]
        pb_ctr[0] = (i + 1) % NPB_
        return i

    NSTG = 4
    stg = [sb("stg%d" % i, [128, 16, 256]) for i in range(NSTG)]
    stg_ctr = [0]

    def load_piece(ip, src_ap, view=None):
        k = stg_ctr[0] % NSTG
        eng = ("dve", "act")[stg_ctr[0] % 2]
        q = ("sp", "pool")[stg_ctr[0] % 2]
        stg_ctr[0] += 1
        sv = stg[k][:, :, :] if view is None else view(stg[k])
        S.dma(q, lambda e: e.dma_start(out=sv, in_=src_ap), writes=[("stg", k)])
        if eng == "act":
            S.op("act", lambda e: e.activation(out=pbuf[ip][:, :, :], in_=stg[k][:, :, :], func=AF.Copy), reads=[("stg", k)], writes=[("pb", ip)])
        else:
            S.op(eng, lambda e: e.tensor_copy(pbuf[ip][:, :, :], stg[k][:, :, :]), reads=[("stg", k)], writes=[("pb", ip)])

    for ex in range(NE):
        S.dma("sp", lambda e, ex=ex: e.dma_start(out=Xe[:, :], in_=Xd[ex * CAP:(ex + 1) * CAP, :]), reads=xd_keys, writes=["Xe"])
        for g in range(4):
            b = nb()

            def f(e, g=g, b=b):
                ins = None
                for j in range(4):
                    kc = g * 4 + j
                    ins = e.matmul(PS[:, b, j * 128:(j + 1) * 128], lhsT=Xe[:, kc * 128:(kc + 1) * 128], rhs=identb[:, :], start=True, stop=True)
                return ins
            S.op("pe", f, reads=["Xe", "identb"], writes=[pk(b)])
            S.op("act", lambda e, g=g, b=b: e.activation(out=XeT[:, g * 4:(g + 1) * 4, :], in_=PS[:, b, :].rearrange("p (j t) -> p j t", j=4), func=AF.Copy),
                 reads=[pk(b)], writes=["XeT"])
        for fblk in range(4):
            ig = nxt_pb()
            load_piece(ig, ewg[ex, :, fblk * 256:(fblk + 1) * 256].rearrange("(kc p) c -> p kc c", p=128))
            iu = nxt_pb()
            load_piece(iu, ewu[ex, :, fblk * 256:(fblk + 1) * 256].rearrange("(kc p) c -> p kc c", p=128))
            b = nb()

            def f(e, b=b, ig=ig, iu=iu):
                ins = None
                for kc in range(16):
                    e.matmul(PS[:, b, 0:256], lhsT=XeT[:, kc, :], rhs=pbuf[ig][:, kc, :], start=(kc == 0), stop=(kc == 15))
                for kc in range(16):
                    ins = e.matmul(PS[:, b, 256:512], lhsT=XeT[:, kc, :], rhs=pbuf[iu][:, kc, :], start=(kc == 0), stop=(kc == 15))
                return ins
            S.op("pe", f, reads=[("pb", ig), ("pb", iu), "XeT"], writes=[pk(b)])
            S.op("act", lambda e, b=b: e.activation(out=sil[:, :], in_=PS[:, b, 0:256], func=AF.Silu), reads=[pk(b)], writes=["sil"])
            S.op("dve", lambda e, b=b, fblk=fblk: e.tensor_mul(act_tok[:, fblk * 256:(fblk + 1) * 256], sil[:, :], PS[:, b, 256:512]), reads=[pk(b), "sil"], writes=["act_tok"])
        for g in range(2):
            b = nb()

            def f(e, g=g, b=b):
                ins = None
                for j in range(4):
                    kc = g * 4 + j
                    ins = e.matmul(PS[:, b, j * 128:(j + 1) * 128], lhsT=act_tok[:, kc * 128:(kc + 1) * 128], rhs=identb[:, :], start=True, stop=True)
                return ins
            S.op("pe", f, reads=["act_tok", "identb"], writes=[pk(b)])
            S.op("act", lambda e, g=g, b=b: e.activation(out=actT[:, g * 4:(g + 1) * 4, :], in_=PS[:, b, :].rearrange("p (j t) -> p j t", j=4), func=AF.Copy),
                 reads=[pk(b)], writes=["actT"])
        for pc in range(4):
            ip = nxt_pb()
            load_piece(ip, ewd[ex, pc * 256:(pc + 1) * 256, :].rearrange("(k p) c -> p k c", p=128),
                       view=lambda t: t[:, :, :].rearrange("p a b -> p (a b)").rearrange("p (k c) -> p k c", k=2))
            wdv = pbuf[ip][:, :, :].rearrange("p a b -> p (a b)").rearrange("p (k c) -> p k c", k=2)

            def f(e, pc=pc, wdv=wdv):
                ins = None
                for k2 in range(2):
                    kc = pc * 2 + k2
                    for n in range(4):
                        ins = e.matmul(PS[:, n, :], lhsT=actT[:, kc, :], rhs=wdv[:, k2, n * 512:(n + 1) * 512], start=(kc == 0), stop=(kc == 7))
                return ins
            S.op("pe", f, reads=[("pb", ip), "actT"], writes=[pk(0), pk(1), pk(2), pk(3)])
        for n in range(4):
            eng = "act" if n % 2 else "dve"
            if eng == "act":
                S.op("act", lambda e, n=n: e.activation(out=Ye[:, n * 512:(n + 1) * 512], in_=PS[:, n, :], func=AF.Copy), reads=[pk(n)], writes=[("Ye", n)])
            else:
                S.op("dve", lambda e, n=n: e.tensor_copy(Ye[:, n * 512:(n + 1) * 512], PS[:, n, :]), reads=[pk(n)], writes=[("Ye", n)])
        S.dma("sp", lambda e, ex=ex: e.dma_start(out=Yd[ex * CAP:(ex + 1) * CAP, :], in_=Ye[:, :]), reads=[("Ye", n) for n in range(4)], writes=[("yd", ex)])
    yd_keys = [("yd", ex) for ex in range(NE)]
    r1 = sb("r1", [128, D])
    r2 = sb("r2", [128, D])
    S.op("dve", lambda e: e.memset(r1[:, :], 0.0), writes=["r1"])
    S.op("dve", lambda e: e.memset(r2[:, :], 0.0), writes=["r2"])
    fin = []
    for st_i in range(NSUB):
        row0 = st_i * 128
        S.dma("sp", lambda e, row0=row0: e.dma_start(out=x1[:, :], in_=y_out[row0:row0 + 128, :]), reads=[("yrow", st_i)], writes=["xin"])
        for kx, rr in enumerate((r1, r2)):
            S.dma("pool", lambda e, kx=kx, rr=rr, st_i=st_i: e.indirect_dma_start(
                out=rr[:, :], out_offset=None, in_=Yd[:, :],
                in_offset=bass.IndirectOffsetOnAxis(ap=rt_d[:, st_i, kx:kx + 1], axis=0), bounds_check=bc_reg, oob_is_err=False),
                reads=yd_keys + [("rtd", st_i)], writes=["r%d" % (kx + 1)])
        S.op("dve", lambda e, st_i=st_i: e.scalar_tensor_tensor(x1[:, :], r1[:, :], rt_w[:, st_i, 0:1], x1[:, :], ALU.mult, ALU.add), reads=["r1", "xin", ("rtw", st_i)], writes=["xin"])
        S.op("dve", lambda e, st_i=st_i: e.scalar_tensor_tensor(x1[:, :], r2[:, :], rt_w[:, st_i, 1:2], x1[:, :], ALU.mult, ALU.add), reads=["r2", "xin", ("rtw", st_i)], writes=["xin"])
        S.dma("sp", lambda e, row0=row0: e.dma_start(out=y_out[row0:row0 + 128, :], in_=x1[:, :]), reads=["xin"], writes=[("yfin", st_i)])
        fin.append(("yfin", st_i))
    S.wait_all("sp", fin)
    if dbg:
        S.wait_all("sp", ["junk"])
    S.barrier()
    S.stack.close()
    return nc


def make_consts(n_exp, cap):
    c = np.zeros((128, 1408), np.float32)
    p = np.arange(128)
    c[:, 0:128] = np.eye(128)
    s = (p % 64)[:, None]
    t = np.arange(64)[None, :]
    c[:, 128:192] = (s < t)
    c[:, 192:256] = (s <= t)
    c[0:64, 256:320] = (np.arange(64)[:, None] < t)
    c[0:64, 320:384] = (np.arange(64)[:, None] > t)
    c[:, 384:512] = (p[:, None] // 64 == p[None, :] // 64)
    c[:, 512] = (p < 64)
    c[:, 513] = (p >= 64)
    c[:, 640:768] = (p[:, None] < p[None, :])
    c[:, 768:896] = 1.0
    c[:, 896:1024] = (p[:, None] <= p[None, :])
    c[:, 1024:1152] = (p[:, None] > p[None, :])
    c[:, 1152:1216] = 1.0
    c[:, 1344:1408] = 1.0
    ecap = np.tile((np.arange(n_exp) * cap).astype(np.float32)[None, :], (128, 1))
    return c, ecap


def core_inputs(inp, b, hh, n_prev, n_own, n_groups, cap):
    sq = lambda a: np.ascontiguousarray(np.asarray(a)[0])
    x = np.asarray(inp["x"])
    own = x[b, hh * n_own * TT:(hh + 1) * n_own * TT]
    if hh == 0:
        prev = np.zeros((n_prev * TT, D), np.float32)
    else:
        prev = x[b, hh * n_own * TT - n_prev * TT:hh * n_own * TT]
    ne = n_groups * 8
    c, ecap = make_consts(ne, cap)
    m = {
        "xs": np.ascontiguousarray(np.concatenate([prev, own], 0)),
        "flag": np.full((128, 1), float(hh), np.float32),
        "w_in": sq(inp["w_in"]),
        "norm1_w": sq(inp["norm1_w"]).reshape(1, D),
        "norm2_w": sq(inp["norm2_w"]).reshape(1, D),
        "rwkv_mu": sq(inp["rwkv_mu"]).reshape(-1, 1),
        "rwkv_w0": sq(inp["rwkv_w0"]).reshape(-1, 1),
        "rwkv_a0": sq(inp["rwkv_a0"]).reshape(-1, 1),
        "rwkv_k_k": sq(inp["rwkv_k_k"]).reshape(-1, 1),
        "rwkv_k_a": sq(inp["rwkv_k_a"]).reshape(-1, 1),
        "rwkv_r_k": sq(inp["rwkv_r_k"]).reshape(-1, 1),
        "rwkv_w2": sq(inp["rwkv_w2"]),
        "rwkv_a2": sq(inp["rwkv_a2"]),
        "rwkv_g2": sq(inp["rwkv_g2"]),
        "rwkv_gn_w": sq(inp["rwkv_gn_w"]).reshape(1, -1),
        "rwkv_gn_b": sq(inp["rwkv_gn_b"]).reshape(1, -1),
        "q_norm_w": sq(inp["q_norm_w"]).reshape(-1, 1),
        "k_norm_w": sq(inp["k_norm_w"]).reshape(-1, 1),
        "attn_sinks": sq(inp["attn_sinks"]).reshape(-1, 1),
        "proj_rwkv": sq(inp["proj_rwkv"]),
        "proj_attn": sq(inp["proj_attn"]),
        "w_out": sq(inp["w_out"]),
        "router_coarse_w": sq(inp["router_coarse_w"]),
        "router_coarse_b": sq(inp["router_coarse_b"]).reshape(1, -1),
        "router_fine_w": sq(inp["router_fine_w"]),
        "router_fine_b": sq(inp["router_fine_b"]).reshape(1, -1),
        "expert_w_gate": sq(inp["expert_w_gate"]),
        "expert_w_up": sq(inp["expert_w_up"]),
        "expert_w_down": sq(inp["expert_w_down"]),
        "consts": c,
        "ecap": ecap,
    }
    return m


def kernel(**inputs):
    cfg = dict(n_prev=16, n_own=16, n_groups=8, cap=128)
    nc = build_program(cfg)
    in_maps = []
    for c in range(8):
        in_maps.append(core_inputs(inputs, c // 2, c % 2, 16, 16, 8, 128))
    res = run_bass_kernel_spmd(nc, in_maps, core_ids=list(range(8)))
    out = np.zeros((4, 4096, D), np.float32)
    for c in range(8):
        b, hh = c // 2, c % 2
        out[b, hh * 2048:(hh + 1) * 2048] = res.results[c]["y"]
    return out
```

```python
import numpy as np
import ml_dtypes
import concourse.bass as bass
import concourse.mybir as mybir
from concourse.bass_utils import run_bass_kernel_spmd

F32 = mybir.dt.float32
BF16 = mybir.dt.bfloat16
I32 = mybir.dt.int32
AF = mybir.ActivationFunctionType
ALU = mybir.AluOpType
AX = mybir.AxisListType

D = 2048
RW = 1024
TT = 128
NS = TT // 128
CH = 64
NCH = TT // CH
IN_COLS = 9152
C_R, C_K, C_V, C_XW, C_XA, C_XG = 0, 1024, 2048, 3072, 3168, 3264
C_Q, C_KA, C_VA, C_GA, C_GB = 3520, 4544, 4800, 5056, 7104
NDS = 40
DECAY_C = 0.6065306597126334


class Sched:
    def __init__(self, nc):
        from contextlib import ExitStack
        self.stack = ExitStack()
        self.nc = nc
        self.E = dict(pe=nc.tensor, dve=nc.vector, act=nc.scalar, pool=nc.gpsimd, sp=nc.sync)
        self.esem = {}
        for e in ("pe", "dve", "act", "pool"):
            self.esem[e] = self.stack.enter_context(nc.semaphore("es_" + e))
        self.ecnt = dict.fromkeys(self.esem, 0)
        self.dsems = [self.stack.enter_context(nc.semaphore("ds%d" % i)) for i in range(NDS)]
        self.dcnt = [0] * NDS
        self.dnext = 0
        self.seen = {e: {} for e in self.E}
        self.W = {}
        self.R = {}

    def _wait(self, eng, deps):
        for sid, (sem, val) in deps.items():
            if val > 0 and self.seen[eng].get(sid, 0) < val:
                self.E[eng].wait_ge(sem, val)
                self.seen[eng][sid] = val

    def _deps(self, reads, writes):
        deps = {}

        def add(d):
            for sid, (sem, val) in d.items():
                if sid not in deps or deps[sid][1] < val:
                    deps[sid] = (sem, val)
        for r in reads:
            add(self.W.get(r, {}))
        for w in writes:
            add(self.W.get(w, {}))
            add(self.R.get(w, {}))
        return deps

    def _record(self, me, reads, writes):
        sid, sem, val = me
        for r in reads:
            self.R.setdefault(r, {})[sid] = (sem, val)
        for w in writes:
            self.W[w] = {sid: (sem, val)}
            self.R[w] = {}

    def op(self, eng, fn, reads=(), writes=()):
        self._wait(eng, self._deps(reads, writes))
        ins = fn(self.E[eng])
        self.ecnt[eng] += 1
        ins.then_inc(self.esem[eng], 1)
        self._record((eng, self.esem[eng], self.ecnt[eng]), reads, writes)

    def dma(self, q, fn, reads=(), writes=()):
        i = self.dnext
        self.dnext = (i + 1) % NDS
        sem = self.dsems[i]
        deps = self._deps(reads, writes)
        sid = "d%d" % i
        if sid not in deps or deps[sid][1] < self.dcnt[i]:
            deps[sid] = (sem, self.dcnt[i])
        self._wait(q, deps)
        ins = fn(self.E[q])
        ins.then_inc(sem, 16)
        self.dcnt[i] += 16
        self._record((sid, sem, self.dcnt[i]), reads, writes)

    def barrier(self):
        keys = list(self.W.keys())
        for eng in self.E:
            self.wait_all(eng, keys)

    def wait_all(self, eng, keys):
        deps = {}
        for k in keys:
            for d in (self.W.get(k, {}), self.R.get(k, {})):
                for sid, (sem, val) in d.items():
                    if sid not in deps or deps[sid][1] < val:
                        deps[sid] = (sem, val)
        self._wait(eng, deps)


def build_program(cfg):
    NPREV = cfg["n_prev"]
    NOWN = cfg["n_own"]
    NG = cfg["n_groups"]
    NE = NG * 8
    CAP = cfg["cap"]
    NT = NPREV + NOWN
    NTOK = NT * TT
    NOWN_TOK = NOWN * TT
    NSUB = NOWN_TOK // 128
    NRL = 8 + NE
    dbg = cfg.get("dbg", False)

    nc = bass.Bass("TRN2", target_bir_lowering=False)
    _ncd = nc.allow_non_contiguous_dma(reason="tiny per-channel parameter loads")
    _ncd.__enter__()
    S = Sched(nc)

    def din(name, shape, dt=F32):
        return nc.dram_tensor(name, list(shape), dt, kind="ExternalInput").ap()

    xs = din("xs", [NTOK, D])
    flag_d = din("flag", [128, 1])
    w_in = din("w_in", [D, IN_COLS])
    norm1_w = din("norm1_w", [1, D])
    norm2_w = din("norm2_w", [1, D])
    mu_d = din("rwkv_mu", [3520, 1])
    w0_d = din("rwkv_w0", [RW, 1])
    a0_d = din("rwkv_a0", [RW, 1])
    kk_d = din("rwkv_k_k", [RW, 1])
    ka_d = din("rwkv_k_a", [RW, 1])
    rk_d = din("rwkv_r_k", [RW, 1])
    w2_d = din("rwkv_w2", [96, RW])
    a2_d = din("rwkv_a2", [96, RW])
    g2_d = din("rwkv_g2", [256, RW])
    gnw_d = din("rwkv_gn_w", [1, RW])
    gnb_d = din("rwkv_gn_b", [1, RW])
    qn_d = din("q_norm_w", [64, 1])
    kn_d = din("k_norm_w", [64, 1])
    sink_d = din("attn_sinks", [16, 1])
    proj_r = din("proj_rwkv", [RW, D])
    proj_a = din("proj_attn", [RW, D])
    w_out = din("w_out", [D, D])
    rcw_d = din("router_coarse_w", [D, NG])
    rcb_d = din("router_coarse_b", [1, NG])
    rfw_d = din("router_fine_w", [D, NE])
    rfb_d = din("router_fine_b", [1, NE])
    ewg = din("expert_w_gate", [NE, D, 1024])
    ewu = din("expert_w_up", [NE, D, 1024])
    ewd = din("expert_w_down", [NE, 1024, D])
    cst_d = din("consts", [128, 1408])
    ecap_d = din("ecap", [128, NE])
    y_out = nc.dram_tensor("y", [NOWN_TOK, D], F32, kind="ExternalOutput").ap()
    Xd = nc.dram_tensor("xdisp", [NE * CAP, D], BF16).ap()
    Yd = nc.dram_tensor("ydisp", [NE * CAP, D], F32).ap()
    dbg_t = {}
    if dbg:
        dbg_t["aout"] = nc.dram_tensor("dbg_aout", [RW, NOWN_TOK], F32, kind="ExternalOutput").ap()
        dbg_t["bout"] = nc.dram_tensor("dbg_bout", [RW, NOWN_TOK], F32, kind="ExternalOutput").ap()
        dbg_t["xin"] = nc.dram_tensor("dbg_x1", [NOWN_TOK, D], F32, kind="ExternalOutput").ap()
        dbg_t["rt"] = nc.dram_tensor("dbg_rt", [NOWN_TOK, 8], F32, kind="ExternalOutput").ap()

    from contextlib import ExitStack
    scope = [None]

    uniq = [0]

    def phase_begin():
        S.barrier()
        st = ExitStack()
        st._prev = scope[0]
        scope[0] = st
        return st

    def phase_end(st):
        S.barrier()
        scope[0] = st._prev
        st.close()

    def sb(name, shape, dt=F32):
        uniq[0] += 1
        name = "%s_%d" % (name, uniq[0])
        if cfg.get("verbose"):
            print("alloc", name, shape, dt, "remaining", nc.sbuf_bytes_remaining)
        if scope[0] is None:
            return nc.alloc_sbuf_tensor("s_" + name, list(shape), dt)
        return scope[0].enter_context(nc.sbuf_tensor("s_" + name, list(shape), dt))

    PS = nc.alloc_psum_tensor("ps", [128, 8, 512], F32)
    bank_ctr = [0]

    bank_lo = [0]

    def nb():
        b = bank_ctr[0]
        bank_ctr[0] = b + 1 if b + 1 < 8 else bank_lo[0]
        return b

    def nb2():
        b = bank_ctr[0]
        if b % 2:
            b = (b + 1) % 8
        bank_ctr[0] = (b + 2) % 8
        return b

    def pk(b):
        return ("ps", b)

    cst = sb("cst", [128, 1408])
    ident = cst[:, 0:128]
    maskA = cst[:, 128:256]
    maskU = cst[0:64, 256:320]
    maskL = cst[0:64, 320:384]
    ident64 = cst[0:64, 0:64]
    bones = cst[:, 384:512]
    headsel = cst[:, 512:514]
    tri_strict = cst[:, 640:768]
    ones_m = cst[:, 768:896]
    mcur_f = cst[:, 896:1024]
    mprev_f = cst[:, 1024:1152]
    onesL_f = cst[:, 1152:1280]
    onesR_f = cst[:, 1280:1408]
    S.dma("sp", lambda e: e.dma_start(out=cst[:, :], in_=cst_d[:, :]), writes=["cst"])
    ecap = sb("ecap", [128, NE])
    S.dma("sp", lambda e: e.dma_start(out=ecap[:, :], in_=ecap_d[:, :]), writes=["cst"])
    flag = sb("flag", [128, 1])
    S.dma("sp", lambda e: e.dma_start(out=flag[:, :], in_=flag_d[:, :]), writes=["cst"])

    if cfg.get("sstop", 99) <= 1:
        S.barrier(); S.stack.close(); return nc
    rt_w = sb("rt_w", [128, NSUB, 2])
    rt_d = sb("rt_d", [128, NSUB, 2], I32)
    scope[0] = ExitStack()
    NPB = 28
    mu_t = sb("mu_t", [128, NPB])
    omm_t = sb("omm_t", [128, NPB])
    S.op("dve", lambda e: e.memset(mu_t[:, :], 0.0), writes=["mu"])
    for j in range(24):
        S.dma("sp", lambda e, j=j: e.dma_start(out=mu_t[:, j:j + 1], in_=mu_d[j * 128:(j + 1) * 128, :]), writes=["mu"])
    S.dma("sp", lambda e: e.dma_start(out=mu_t[0:96, 24:25], in_=mu_d[C_XW:C_XW + 96, :]), writes=["mu"])
    S.dma("sp", lambda e: e.dma_start(out=mu_t[0:96, 25:26], in_=mu_d[C_XA:C_XA + 96, :]), writes=["mu"])
    for j in range(2):
        S.dma("sp", lambda e, j=j: e.dma_start(out=mu_t[:, 26 + j:27 + j], in_=mu_d[C_XG + j * 128:C_XG + (j + 1) * 128, :]), writes=["mu"])
    S.op("dve", lambda e: e.tensor_scalar(omm_t[:, :], mu_t[:, :], -1.0, 1.0, ALU.mult, ALU.add), reads=["mu"], writes=["omm"])
    if cfg.get("sstop", 99) <= 2:
        S.barrier(); S.stack.close(); return nc
    pch = sb("pch", [128, 5, 8])
    for i, dsrc in enumerate((w0_d, a0_d, kk_d, ka_d, rk_d)):
        for b in range(8):
            S.dma("sp", lambda e, i=i, b=b, dsrc=dsrc: e.dma_start(out=pch[:, i, b:b + 1], in_=dsrc[b * 128:(b + 1) * 128, :]), writes=["pch"])
    ka1 = sb("ka1", [128, 8])
    S.op("dve", lambda e: e.tensor_scalar(ka1[:, :], pch[:, 3, :], -1.0, 1.0, ALU.mult, ALU.add), reads=["pch"], writes=["ka1"])
    if cfg.get("sstop", 99) <= 3:
        S.barrier(); S.stack.close(); return nc
    w2s = sb("w2s", [96, RW])
    a2s = sb("a2s", [96, RW])
    g2s = sb("g2s", [128, 2, RW])
    S.dma("sp", lambda e: e.dma_start(out=w2s[:, :], in_=w2_d[:, :]), writes=["lora"])
    S.dma("sp", lambda e: e.dma_start(out=a2s[:, :], in_=a2_d[:, :]), writes=["lora"])
    S.dma("sp", lambda e: e.dma_start(out=g2s[:, :, :], in_=g2_d.rearrange("(k p) c -> p k c", p=128)), writes=["lora"])
    g1col = sb("g1col", [128, 16])
    g2bc = sb("g2bc", [128, D])
    S.dma("sp", lambda e: e.dma_start(out=g1col[:, :], in_=norm1_w.rearrange("o (k p) -> p (o k)", p=128)), writes=["gbc"])
    S.dma("sp", lambda e: e.dma_start(out=g2bc[:, :], in_=norm2_w.partition_broadcast(128)), writes=["gbc"])
    gnw = sb("gnw", [64, RW])
    gnb = sb("gnb", [64, RW])
    S.dma("sp", lambda e: e.dma_start(out=gnw[:, :], in_=gnw_d.partition_broadcast(64)), writes=["gbc"])
    S.dma("sp", lambda e: e.dma_start(out=gnb[:, :], in_=gnb_d.partition_broadcast(64)), writes=["gbc"])
    if cfg.get("sstop", 99) <= 4:
        S.barrier(); S.stack.close(); return nc
    qkg = sb("qkg", [128, 2])
    for hp in range(2):
        S.dma("sp", lambda e, hp=hp: e.dma_start(out=qkg[hp * 64:(hp + 1) * 64, 0:1], in_=qn_d[:, :]), writes=["gbc"])
        S.dma("sp", lambda e, hp=hp: e.dma_start(out=qkg[hp * 64:(hp + 1) * 64, 1:2], in_=kn_d[:, :]), writes=["gbc"])
    esink = sb("esink", [128, 8])
    for b in range(8):
        for hp in range(2):
            h = 2 * b + hp
            S.dma("sp", lambda e, b=b, hp=hp, h=h: e.dma_start(out=esink[hp * 64:(hp + 1) * 64, b:b + 1], in_=sink_d[h:h + 1, :].partition_broadcast(64)), writes=["esink"])
    S.op("act", lambda e: e.activation(out=esink[:, :], in_=esink[:, :], func=AF.Exp), reads=["esink"], writes=["esink"])
    if cfg.get("sstop", 99) <= 5:
        S.barrier(); S.stack.close(); return nc
    wr = sb("wr", [128, 16, NRL])
    S.op("dve", lambda e: e.memset(wr[:, :, :], 0.0), writes=["wr"])
    S.dma("sp", lambda e: e.dma_start(out=wr[:, :, 0:NG], in_=rcw_d.rearrange("(k p) c -> p k c", p=128)), writes=["wr"])
    S.dma("sp", lambda e: e.dma_start(out=wr[:, :, 8:NRL], in_=rfw_d.rearrange("(k p) c -> p k c", p=128)), writes=["wr"])
    rbias = sb("rbias", [128, NRL])
    S.op("dve", lambda e: e.memset(rbias[:, :], 0.0), writes=["wr"])
    S.dma("sp", lambda e: e.dma_start(out=rbias[:, 0:NG], in_=rcb_d.partition_broadcast(128)), writes=["wr"])
    S.dma("sp", lambda e: e.dma_start(out=rbias[:, 8:NRL], in_=rfb_d.partition_broadcast(128)), writes=["wr"])
    if cfg.get("sstop", 99) <= 6:
        S.barrier(); S.stack.close(); return nc
    mcur = sb("mcur", [128, 4, 128], BF16)
    mprev = sb("mprev", [128, 4, 128], BF16)
    onesL = sb("onesL", [128, 128], BF16)
    onesR = sb("onesR", [128, 128], BF16)
    maskA4 = sb("maskA4", [128, 4, 128])
    for i in range(4):
        S.op("dve", lambda e, i=i: e.tensor_copy(mcur[:, i, :], mcur_f), reads=["cst"], writes=["m1"])
        S.op("dve", lambda e, i=i: e.tensor_copy(mprev[:, i, :], mprev_f), reads=["cst"], writes=["m1"])
        S.op("dve", lambda e, i=i: e.tensor_copy(maskA4[:, i, :], maskA), reads=["cst"], writes=["m1"])
    S.op("dve", lambda e: e.tensor_copy(onesL[:, :], onesL_f), reads=["cst"], writes=["m1"])
    S.op("dve", lambda e: e.tensor_copy(onesR[:, :], onesR_f), reads=["cst"], writes=["m1"])
    ones64 = sb("ones64", [128, 64])
    S.op("dve", lambda e: e.memset(ones64[:, :], 1.0), writes=["m1"])

    if cfg.get("sstop", 99) <= 7:
        S.barrier(); S.stack.close(); return nc
    zt = sb("zt", [128, D], BF16)
    S.op("dve", lambda e: e.memset(zt[:, :], 0.0), writes=["zt"])
    for ex in range(NE):
        S.dma("sp", lambda e, ex=ex: e.dma_start(out=Xd[ex * CAP:(ex + 1) * CAP, :], in_=zt[:, :]), reads=["zt"], writes=[("xdz", ex)])
    xdz_keys = [("xdz", ex) for ex in range(NE)]
    bc_reg = nc.gpsimd.alloc_register("bcreg")
    nc.gpsimd.reg_mov(bc_reg, NE * CAP - 1)
    carry = sb("carry", [128, NPB])
    ST = sb("ST", [128, 8, 64])
    S.op("dve", lambda e: e.memset(carry[:, :], 0.0), writes=["carry"])
    S.op("dve", lambda e: e.memset(ST[:, :, :], 0.0), writes=["ST"])
    base_bc = sb("base_bc", [128, NE])
    S.op("dve", lambda e: e.memset(base_bc[:, :], 0.0), writes=["base"])
    kTs = sb("kTs", [128, 3, 4, 128], BF16)
    Vpad = sb("Vpad", [128, 3, 4, 2, 128], BF16)
    S.op("dve", lambda e: e.memset(kTs[:, :, :, :], 0.0), writes=["kTs0", "kTs1", "kTs2"])
    S.op("dve", lambda e: e.memset(Vpad[:, :, :, :, :], 0.0), writes=["Vp0", "Vp1", "Vp2"])

    st1 = sb("st1", [128, 8])
    tmp_sh = sb("tmp_sh", [128, TT])
    hT = sb("hT", [128, 16, TT], BF16)
    NWB = 3
    wbuf = [sb("wb%d" % i, [128, 16, 128], BF16) for i in range(NWB)]
    wb_ctr = [0]
    a_outT = sb("a_outT", [128, 8, TT], BF16)
    b_outT = sb("b_outT", [128, 8, TT], BF16)

    WC = nc.dram_tensor("wcache", [160, 128, 2048], BF16).ap()
    wcache = {}
    cq = [0]

    def load_wblock(key, i, K16, fill_fn):
        wb = wbuf[i]
        flat = wb[:, :, :].rearrange("p a b -> p (a b)")[:, 0:K16 * 128]
        if key in wcache:
            idx = wcache[key]
            q = ("sp", "pool")[cq[0] % 2]
            cq[0] += 1
            S.dma(q, lambda e: e.dma_start(out=flat, in_=WC[idx, :, 0:K16 * 128]), reads=[("wc", idx)], writes=[("wb", i)])
        else:
            fill_fn()
            idx = len(wcache)
            wcache[key] = idx
            S.dma("sp", lambda e: e.dma_start(out=WC[idx, :, 0:K16 * 128], in_=flat), reads=[("wb", i)], writes=[("wc", idx)])

    def mm_block(dram_w, col_specs, K16, M, rhs_fn, N, rkeys, wname):
        i = wb_ctr[0]
        wb_ctr[0] = (i + 1) % NWB
        wb = wbuf[i]

        def fill():
            for (c0, n, d0) in col_specs:
                S.dma("pool", lambda e, c0=c0, n=n, d0=d0: e.dma_start(
                    out=wb[:, 0:K16, d0:d0 + n], in_=dram_w[:, c0:c0 + n].rearrange("(kc p) c -> p kc c", p=128)),
                    writes=[("wb", i)])
        load_wblock((wname, tuple(col_specs)), i, K16, fill)
        b = nb()

        def f(e):
            ins = None
            for kc in range(K16):
                ins = e.matmul(PS[0:M, b, 0:N], lhsT=wb[:, kc, 0:M], rhs=rhs_fn(kc), start=(kc == 0), stop=(kc == K16 - 1))
            return ins
        S.op("pe", f, reads=[("wb", i)] + rkeys, writes=[pk(b)])
        return b

    def shift_evac(b, M, pblk, out_ap, okey):
        mu = mu_t[0:M, pblk:pblk + 1]
        om = omm_t[0:M, pblk:pblk + 1]
        S.op("dve", lambda e: e.tensor_scalar(tmp_sh[0:M, :], PS[0:M, b, 0:TT], mu, None, ALU.mult), reads=[pk(b)], writes=["tmp_sh"])
        S.op("dve", lambda e: e.scalar_tensor_tensor(out_ap[:, 1:TT], PS[0:M, b, 1:TT], om, tmp_sh[0:M, 0:TT - 1], ALU.mult, ALU.add),
             reads=[pk(b), "tmp_sh"], writes=[okey])
        S.op("dve", lambda e: e.scalar_tensor_tensor(out_ap[:, 0:1], PS[0:M, b, 0:1], om, carry[0:M, pblk:pblk + 1], ALU.mult, ALU.add),
             reads=[pk(b), "carry"], writes=[okey])
        S.op("dve", lambda e: e.tensor_copy(carry[0:M, pblk:pblk + 1], tmp_sh[0:M, TT - 1:TT]), reads=["tmp_sh"], writes=["carry"])

    hT_rhs = lambda kc: hT[:, kc, :]

    def c3(ap, c):
        return ap.rearrange("p (c t) -> p c t", c=c)

    class Stop(Exception):
        pass
    stop = cfg.get("stop", 99)

    def mixer_tile(ti):
        own = ti >= NPREV
        oi = ti - NPREV
        tok0 = ti * TT
        need_kv = own or (ti == NPREV - 1)
        ph = phase_begin()
        xin = sb("xin", [128, D])
        xn = xin
        junk = sb("junk", [128, D])
        for s in range(NS):
            S.dma("sp", lambda e: e.dma_start(out=xin[:, :], in_=xs[tok0 + s * 128:tok0 + (s + 1) * 128, :]), writes=["xin"])
            S.op("act", lambda e: e.activation(out=junk[:, :], in_=xin[:, :], func=AF.Square, accum_out=st1[:, 0:1]), reads=["xin"], writes=["junk", "st1"])
            S.op("dve", lambda e: e.tensor_scalar(st1[:, 1:2], st1[:, 0:1], 1.0 / D, 1e-6, ALU.mult, ALU.add), reads=["st1"], writes=["st1b"])
            S.op("act", lambda e: e.activation(out=st1[:, 2:3], in_=st1[:, 1:2], func=AF.Sqrt), reads=["st1b"], writes=["st1c"])
            S.op("dve", lambda e: e.reciprocal(st1[:, 2:3], st1[:, 2:3]), reads=["st1c"], writes=["st1c"])
            S.op("dve", lambda e: e.tensor_scalar(xn[:, :], xin[:, :], st1[:, 2:3], None, ALU.mult), reads=["xin", "st1c"], writes=["xin"])
            for g in range(4):
                b = nb()

                def f(e, g=g, b=b):
                    ins = None
                    for j in range(4):
                        kc = g * 4 + j
                        ins = e.transpose(PS[:, b, j * 128:(j + 1) * 128], xn[:, kc * 128:(kc + 1) * 128], ident)
                    return ins
                S.op("pe", f, reads=["xin"], writes=[pk(b)])
                for j in range(4):
                    kc = g * 4 + j
                    S.op("act", lambda e, kc=kc, j=j, b=b: e.activation(out=hT[:, kc, s * 128:(s + 1) * 128], in_=PS[:, b, j * 128:(j + 1) * 128],
                                                                    func=AF.Copy, scale=g1col[:, kc:kc + 1]),
                         reads=[pk(b)], writes=["hT"])
        phase_end(ph)
        if stop <= 1:
            raise Stop()
        ph = phase_begin()
        xwT = sb("xwT", [128, TT])
        xaT = sb("xaT", [128, TT])
        sgT = sb("sgT", [128, 2, TT])
        r_b = sb("r_b", [128, TT])
        k_b = sb("k_b", [128, TT])
        v_b = sb("v_b", [128, TT])
        a_b = sb("a_b", [128, TT])
        sw_b = sb("sw_b", [128, TT])
        r_bs = [r_b, sb("r_b2", [128, TT])]
        k_bs = [k_b, sb("k_b2", [128, TT])]
        v_bs = [v_b, sb("v_b2", [128, TT])]
        a_bs = [a_b, sb("a_b2", [128, TT])]
        sw_bs = [sw_b, sb("sw_b2", [128, TT])]
        cs_b = sb("cs_b", [128, TT])
        csx_b = sb("csx_b", [128, TT])
        e1 = sb("e1", [128, TT])
        e2 = sb("e2", [128, TT])
        e3 = sb("e3", [128, TT])
        e4 = sb("e4", [128, TT])
        nbias = sb("nbias", [128, NCH])
        kq = sb("kq", [128, TT])
        t1 = sb("t1", [128, TT])
        kkn = sb("kkn", [128, TT])
        kmod = sb("kmod", [128, TT])
        bb = sb("bb", [128, TT])
        rkk = sb("rkk", [128, TT])
        AR = sb("AR", [128, 8, NCH, 128])
        ARm = [sb("ARm%d" % i_, [128, 8, NCH, 128]) for i_ in range(2)]
        BK = sb("BK", [128, 8, NCH, 128])
        BKp = sb("BKp", [128, NCH, 128])
        gamC = sb("gamC", [128, 8, NCH])
        VU = sb("VU", [128, NCH, 16, 64])
        bsum = sb("bsum", [64, NCH, 16])
        AT = sb("AT", [128, 16, 128])
        ATK = [("AT", g_) for g_ in range(4)]
        PQ = [sb("PQ%d" % i, [64, 16, 128], BF16) for i in range(2)]
        Tt = [sb("Tt%d" % i, [64, 16, 64], BF16) for i in range(2)]
        Wsb = sb("Wsb", [64, 16, 64])
        ysq = sb("ysq", [64, RW])
        yn = sb("yn", [64, RW])
        gst = sb("gst", [64, 4, 16])
        BKtok = sb("BKtok", [128, NCH, 16, 128])
        TtF = sb("TtF", [64, 16, 128])
        S.op("dve", lambda e: e.memset(BKtok[:, :, :, :], 0.0), writes=["BKtok"])
        S.op("dve", lambda e: e.memset(TtF[:, :, :], 0.0), writes=[("TtF", 0), ("TtF", 1)])
        b = mm_block(w_in, [(C_XW, 96, 0)], 16, 96, hT_rhs, TT, ["hT"], "xw")
        shift_evac(b, 96, 24, xwT[0:96, :], "xwT")
        S.op("act", lambda e: e.activation(out=xwT[0:96, :], in_=xwT[0:96, :], func=AF.Tanh), reads=["xwT"], writes=["xwT"])
        b = mm_block(w_in, [(C_XA, 96, 0)], 16, 96, hT_rhs, TT, ["hT"], "xa")
        shift_evac(b, 96, 25, xaT[0:96, :], "xaT")
        for j in range(2):
            b = mm_block(w_in, [(C_XG + j * 128, 128, 0)], 16, 128, hT_rhs, TT, ["hT"], "xg")
            shift_evac(b, 128, 26 + j, sgT[:, j, :], "sgT")
        S.op("act", lambda e: e.activation(out=sgT[:, :, :], in_=sgT[:, :, :], func=AF.Sigmoid), reads=["sgT"], writes=["sgT"])
        if stop <= 1.2:
            raise Stop()
        def stageA(blk):
            par = blk % 2
            r_b, k_b, v_b, a_b, sw_b = r_bs[par], k_bs[par], v_bs[par], a_bs[par], sw_bs[par]
            b = mm_block(w_in, [(C_R + blk * 128, 128, 0)], 16, 128, hT_rhs, TT, ["hT"], "r")
            shift_evac(b, 128, blk, r_b[:, :], ("r_b", par))
            b = mm_block(w_in, [(C_K + blk * 128, 128, 0)], 16, 128, hT_rhs, TT, ["hT"], "k")
            shift_evac(b, 128, 8 + blk, k_b[:, :], ("k_b", par))
            b = mm_block(w_in, [(C_V + blk * 128, 128, 0)], 16, 128, hT_rhs, TT, ["hT"], "v")
            shift_evac(b, 128, 16 + blk, v_b[:, :], ("v_b", par))
            b = nb()
            S.op("pe", lambda e, b=b: e.matmul(PS[:, b, 0:TT], lhsT=a2s[:, blk * 128:(blk + 1) * 128], rhs=xaT[0:96, :], start=True, stop=True),
                 reads=["xaT"], writes=[pk(b)])
            S.op("act", lambda e, b=b: e.activation(out=a_b[:, :], in_=PS[:, b, 0:TT], func=AF.Sigmoid, bias=pch[:, 1, blk:blk + 1]),
                 reads=[pk(b)], writes=[("a_b", par)])
            b = nb()
            S.op("pe", lambda e, b=b: e.matmul(PS[:, b, 0:TT], lhsT=w2s[:, blk * 128:(blk + 1) * 128], rhs=xwT[0:96, :], start=True, stop=True),
                 reads=["xwT"], writes=[pk(b)])
            S.op("act", lambda e, b=b: e.activation(out=sw_b[:, :], in_=PS[:, b, 0:TT], func=AF.Sigmoid, bias=pch[:, 0, blk:blk + 1]),
                 reads=[pk(b)], writes=[("sw_b", par)])

        def stageB(blk):
            par = blk % 2
            r_b, k_b, v_b, a_b, sw_b = r_bs[par], k_bs[par], v_bs[par], a_bs[par], sw_bs[par]
            for ch in range(NCH):
                S.op("dve", lambda e, ch=ch: e.tensor_tensor_scan(cs_b[:, ch * CH:(ch + 1) * CH], ones64[:, :], sw_b[:, ch * CH:(ch + 1) * CH], 0.0, ALU.mult, ALU.add),
                     reads=[("sw_b", par)], writes=["cs_b"])
            S.op("dve", lambda e: e.tensor_sub(csx_b[:, :], cs_b[:, :], sw_b[:, :]), reads=["cs_b", ("sw_b", par)], writes=["csx_b"])
            S.op("act", lambda e: e.activation(out=e1[:, :], in_=cs_b[:, :], func=AF.Exp, scale=-DECAY_C), reads=["cs_b"], writes=["e1"])
            S.op("act", lambda e: e.activation(out=e2[:, :], in_=csx_b[:, :], func=AF.Exp, scale=-DECAY_C), reads=["csx_b"], writes=["e2"])
            S.op("act", lambda e: e.activation(out=e3[:, :], in_=cs_b[:, :], func=AF.Exp, scale=DECAY_C), reads=["cs_b"], writes=["e3"])
            S.op("dve", lambda e: e.tensor_scalar(nbias[:, :], c3(cs_b[:, :], NCH)[:, :, CH - 1], -DECAY_C, None, ALU.mult), reads=["cs_b"], writes=["nbias"])
            for ch in range(NCH):
                S.op("act", lambda e, ch=ch: e.activation(out=e4[:, ch * CH:(ch + 1) * CH], in_=cs_b[:, ch * CH:(ch + 1) * CH], func=AF.Exp,
                                                          scale=DECAY_C, bias=nbias[:, ch:ch + 1]), reads=["cs_b", "nbias"], writes=["e4"])
            S.op("dve", lambda e: e.tensor_copy(gamC[:, blk, :], c3(e1[:, :], NCH)[:, :, CH - 1]), reads=["e1"], writes=["gamC"])
            S.op("dve", lambda e: e.tensor_scalar(kq[:, :], k_b[:, :], pch[:, 2, blk:blk + 1], None, ALU.mult), reads=[("k_b", par)], writes=["kq"])
            S.op("act", lambda e: e.activation(out=t1[:, :], in_=kq[:, :], func=AF.Square), reads=["kq"], writes=["t1"])
            b = nb()
            S.op("pe", lambda e, b=b: e.matmul(PS[:, b, 0:TT], lhsT=bones, rhs=t1[:, :], start=True, stop=True), reads=["t1"], writes=[pk(b)])
            S.op("dve", lambda e, b=b: e.tensor_scalar(t1[:, :], PS[:, b, 0:TT], 1e-24, None, ALU.max), reads=[pk(b)], writes=["t1"])
            S.op("act", lambda e: e.activation(out=t1[:, :], in_=t1[:, :], func=AF.Sqrt), reads=["t1"], writes=["t1"])
            S.op("dve", lambda e: e.reciprocal(t1[:, :], t1[:, :]), reads=["t1"], writes=["t1"])
            S.op("dve", lambda e: e.tensor_mul(kkn[:, :], kq[:, :], t1[:, :]), reads=["kq", "t1"], writes=["kkn"])
            S.op("dve", lambda e: e.tensor_scalar(t1[:, :], a_b[:, :], pch[:, 3, blk:blk + 1], ka1[:, blk:blk + 1], ALU.mult, ALU.add),
                 reads=[("a_b", par), "kkn"], writes=["t1"])
            S.op("dve", lambda e: e.tensor_mul(kmod[:, :], k_b[:, :], t1[:, :]), reads=[("k_b", par), "t1"], writes=["kmod"])
            S.op("dve", lambda e: e.tensor_mul(bb[:, :], kkn[:, :], a_b[:, :]), reads=["kkn", ("a_b", par)], writes=["bb"])
            S.op("dve", lambda e: e.scalar_tensor_tensor(AR[:, blk, :, 0:64], c3(kkn[:, :], NCH), -1.0, c3(e2[:, :], NCH), ALU.mult, ALU.mult),
                 reads=["kkn", "e2"], writes=["AR"])
            S.op("dve", lambda e: e.tensor_mul(AR[:, blk, :, 64:128], c3(r_b[:, :], NCH), c3(e1[:, :], NCH)), reads=[("r_b", par), "e1"], writes=["AR"])
            for par in range(2):
                S.op("dve", lambda e, par=par: e.tensor_scalar(ARm[par][:, blk, :, :], AR[:, blk, :, :], headsel[:, par:par + 1], None, ALU.mult), reads=["AR"], writes=["ARm"])
            S.op("dve", lambda e: e.tensor_mul(BK[:, blk, :, 0:64], c3(kmod[:, :], NCH), c3(e3[:, :], NCH)), reads=["kmod", "e3"], writes=["BK"])
            S.op("dve", lambda e: e.tensor_mul(BK[:, blk, :, 64:128], c3(bb[:, :], NCH), c3(e3[:, :], NCH)), reads=["bb", "e3"], writes=["BK"])
            S.op("dve", lambda e: e.tensor_mul(BKp[:, :, 0:64], c3(kmod[:, :], NCH), c3(e4[:, :], NCH)), reads=["kmod", "e4"], writes=["BKp"])
            S.op("dve", lambda e: e.tensor_mul(BKp[:, :, 64:128], c3(bb[:, :], NCH), c3(e4[:, :], NCH)), reads=["bb", "e4"], writes=["BKp"])
            b = nb()

            def f(e, b=b):
                ins = None
                for ch in range(NCH):
                    ins = e.transpose(PS[:, b, ch * 128:(ch + 1) * 128], BKp[:, ch, :], ident)
                return ins
            S.op("pe", f, reads=["BKp"], writes=[pk(b)])
            for hp in range(2):
                S.op("act", lambda e, b=b, hp=hp: e.activation(
                    out=BKtok[:, :, 2 * blk + hp, hp * 64:(hp + 1) * 64],
                    in_=PS[:, b, 0:NCH * 128].rearrange("p (c h j) -> p c h j", c=NCH, h=2)[:, :, hp, :], func=AF.Copy),
                    reads=[pk(b)], writes=["BKtok"])
            b = nb()

            def f(e, b=b):
                ins = None
                for ch in range(NCH):
                    ins = e.transpose(PS[0:64, b, ch * 128:(ch + 1) * 128], v_b[:, ch * CH:(ch + 1) * CH], ident)
                return ins
            S.op("pe", f, reads=[("v_b", par)], writes=[pk(b)])
            S.op("act", lambda e, b=b: e.activation(out=VU[0:64, :, 2 * blk:2 * blk + 2, :],
                                                    in_=PS[0:64, b, 0:NCH * 128].rearrange("p (c h i) -> p c h i", c=NCH, h=2), func=AF.Copy),
                 reads=[pk(b)], writes=["VUv"])
            if own:
                S.op("dve", lambda e: e.scalar_tensor_tensor(rkk[:, :], r_b[:, :], pch[:, 4, blk:blk + 1], kmod[:, :], ALU.mult, ALU.mult),
                     reads=[("r_b", par), "kmod"], writes=["rkk"])

                b = nb()

                def f(e, b=b):
                    ins = None
                    for ch in range(NCH):
                        ins = e.matmul(PS[0:64, b, ch * 128:(ch + 1) * 128], lhsT=rkk[:, ch * CH:(ch + 1) * CH], rhs=bones, start=True, stop=True)
                    return ins
                S.op("pe", f, reads=["rkk"], writes=[pk(b)])
                S.op("dve", lambda e, b=b: e.tensor_copy(bsum[:, :, 2 * blk:2 * blk + 2], PS[0:64, b, 0:NCH * 128].rearrange("p (c h x) -> p c h x", c=NCH, h=2)[:, :, :, 0]),
                     reads=[pk(b)], writes=["bsum"])

        stageA(0)
        for blk in range(8):
            if blk + 1 < 8:
                stageA(blk + 1)
            stageB(blk)
        if stop <= 1.5:
            raise Stop()
        for ch in range(NCH):
            for g4 in range(4):
                b = nb()

                def f(e, b=b, g4=g4):
                    ins = None
                    for j in range(4):
                        h = g4 * 4 + j
                        blk, P0 = h // 2, (h % 2) * 64
                        ins = e.matmul(PS[:, b, j * 128:(j + 1) * 128], lhsT=BK[:, blk, ch, :], rhs=ARm[h % 2][:, blk, ch, :], start=True, stop=True)
                    return ins
                S.op("pe", f, reads=["ARm", "BK"], writes=[pk(b)])
                S.op("dve", lambda e, b=b, g4=g4: e.tensor_mul(AT[:, g4 * 4:(g4 + 1) * 4, :], PS[:, b, :].rearrange("p (h t) -> p h t", h=4), maskA4[:, :, :]),
                     reads=[pk(b)], writes=[("AT", g4)])
            if stop <= 1.6:
                raise Stop()
            for g8 in range(2):
                b = nb2()

                def f(e, b=b, g8=g8):
                    ins = None
                    for j in range(8):
                        h = g8 * 8 + j
                        blk, P0 = h // 2, (h % 2) * 64
                        bb_, off = b + j // 4, (j % 4) * 128
                        e.matmul(PS[0:64, bb_, off:off + 64], lhsT=ARm[h % 2][:, blk, ch, 0:64], rhs=BK[:, blk, ch, 64:128], start=True, stop=True)
                        ins = e.matmul(PS[0:64, bb_, off + 64:off + 128], lhsT=BK[:, blk, ch, 64:128], rhs=ARm[h % 2][:, blk, ch, 0:64], start=True, stop=True)
                    return ins
                S.op("pe", f, reads=["ARm", "BK"], writes=[pk(b), pk(b + 1)])
                for q in range(2):
                    hs = g8 * 8 + q * 4
                    S.op("dve", lambda e, b=b, q=q, hs=hs: e.tensor_mul(PQ[0][:, hs:hs + 4, 0:64], PS[0:64, b + q, :].rearrange("p (h x) -> p h x", h=4)[:, :, 0:64],
                                                                        maskL.unsqueeze(1).to_broadcast([64, 4, 64])), reads=[pk(b + q)], writes=[("PQ0", g8)])
                    S.op("dve", lambda e, b=b, q=q, hs=hs: e.tensor_mul(PQ[0][:, hs:hs + 4, 64:128], PS[0:64, b + q, :].rearrange("p (h x) -> p h x", h=4)[:, :, 64:128],
                                                                        maskU.unsqueeze(1).to_broadcast([64, 4, 64])), reads=[pk(b + q)], writes=[("PQ0", g8)])
                S.op("dve", lambda e, g8=g8: e.tensor_add(Tt[1][:, g8 * 8:(g8 + 1) * 8, :], PQ[0][:, g8 * 8:(g8 + 1) * 8, 64:128],
                                                          ident64.unsqueeze(1).to_broadcast([64, 8, 64])), reads=[("PQ0", g8)], writes=[("Tt1", g8)])
            if stop <= 1.7:
                raise Stop()
            tcur = 1
            for k in range(1, 7):
                src = PQ[(k - 1) % 2]
                dst = PQ[k % 2]
                sk, dk = "PQ%d" % ((k - 1) % 2), "PQ%d" % (k % 2)
                for g8 in range(2):
                    do_pq = k <= 5
                    do_t = k >= 2
                    b = nb2()
                    tsrc, tdst = Tt[tcur], Tt[1 - tcur]

                    def f(e, b=b, g8=g8, src=src, do_pq=do_pq, do_t=do_t, tsrc=tsrc):
                        ins = None
                        for j in range(8):
                            h = g8 * 8 + j
                            bb_, off = b + j // 4, (j % 4) * 128
                            if do_pq:
                                e.matmul(PS[0:64, bb_, off:off + 64], lhsT=src[:, h, 64:128], rhs=src[:, h, 0:64], start=True, stop=True)
                                ins = e.matmul(PS[0:64, bb_, off + 64:off + 128], lhsT=src[:, h, 0:64], rhs=src[:, h, 64:128], start=True, stop=True)
                        return ins
                    if do_pq:
                        S.op("pe", f, reads=[(sk, g8)], writes=[pk(b), pk(b + 1)])
                        for q in range(2):
                            hs = g8 * 8 + q * 4
                            S.op("act", lambda e, b=b, q=q, hs=hs, dst=dst: e.activation(out=dst[:, hs:hs + 4, :], in_=PS[0:64, b + q, :].rearrange("p (h x) -> p h x", h=4), func=AF.Copy),
                                 reads=[pk(b + q)], writes=[(dk, g8)])
                    if do_t:
                        b2 = nb()
                        def f2(e, b2=b2, g8=g8, src=src, tsrc=tsrc):
                            ins = None
                            for j in range(8):
                                h = g8 * 8 + j
                                ins = e.matmul(PS[0:64, b2, j * 64:(j + 1) * 64], lhsT=src[:, h, 0:64], rhs=tsrc[:, h, :], start=True, stop=True)
                            return ins
                        S.op("pe", f2, reads=[(sk, g8), ("Tt%d" % tcur, g8)], writes=[pk(b2)])
                        if k < 6:
                            S.op("dve", lambda e, b2=b2, g8=g8, tsrc=tsrc, tdst=tdst: e.tensor_add(
                                tdst[:, g8 * 8:(g8 + 1) * 8, :], tsrc[:, g8 * 8:(g8 + 1) * 8, :], PS[0:64, b2, :].rearrange("p (h x) -> p h x", h=8)),
                                reads=[pk(b2), ("Tt%d" % tcur, g8)], writes=[("Tt%d" % (1 - tcur), g8)])
                        else:
                            S.op("dve", lambda e, b2=b2, g8=g8, tsrc=tsrc: e.tensor_add(
                                TtF[:, g8 * 8:(g8 + 1) * 8, 64:128], tsrc[:, g8 * 8:(g8 + 1) * 8, :], PS[0:64, b2, :].rearrange("p (h x) -> p h x", h=8)),
                                reads=[pk(b2), ("Tt%d" % tcur, g8)], writes=[("TtF", g8)])
                if k >= 2:
                    tcur = 1 - tcur
            if stop <= 1.8:
                raise Stop()
            bw = nb2()

            def f(e, bw=bw):
                ins = None
                for h in range(16):
                    blk, P0 = h // 2, (h % 2) * 64
                    bb_, off = bw + h // 8, (h % 8) * 64
                    e.matmul(PS[0:64, bb_, off:off + 64], lhsT=ARm[h % 2][:, blk, ch, 0:64], rhs=ST[:, blk, :], start=True, stop=False)
                    ins = e.matmul(PS[0:64, bb_, off:off + 64], lhsT=AT[0:64, h, 0:64], rhs=VU[0:64, ch, h, :], start=False, stop=True)
                return ins
            S.op("pe", f, reads=["ARm", "ST", "VUv"] + [("AT", g) for g in range(4)], writes=[pk(bw), pk(bw + 1)])
            for q in range(2):
                S.op("dve", lambda e, q=q, bw=bw: e.tensor_copy(Wsb[:, q * 8:(q + 1) * 8, :], PS[0:64, bw + q, :].rearrange("p (h x) -> p h x", h=8)),
                     reads=[pk(bw + q)], writes=[("Wsb", q)])
            bu = nb2()

            def f(e, bu=bu):
                ins = None
                for h in range(16):
                    bb_, off = bu + h // 8, (h % 8) * 64
                    ins = e.matmul(PS[:, bb_, off:off + 64], lhsT=TtF[:, h, :], rhs=Wsb[:, h, :], start=True, stop=True)
                return ins
            S.op("pe", f, reads=[("Wsb", 0), ("Wsb", 1), ("TtF", 0), ("TtF", 1)], writes=[pk(bu), pk(bu + 1)])
            for q in range(2):
                S.op("dve", lambda e, q=q, bu=bu: e.tensor_copy(VU[64:128, ch, q * 8:(q + 1) * 8, :], PS[64:128, bu + q, :].rearrange("p (h x) -> p h x", h=8)),
                     reads=[pk(bu + q)], writes=[("VUu", q)])
            vu_keys = ["VUv", ("VUu", 0), ("VUu", 1)]
            if own:
                by = nb2()

                def f(e, by=by):
                    ins = None
                    for h in range(16):
                        blk, P0 = h // 2, (h % 2) * 64
                        bb_, off = by + h // 8, (h % 8) * 64
                        e.matmul(PS[0:64, bb_, off:off + 64], lhsT=AT[:, h, 64:128], rhs=VU[:, ch, h, :], start=True, stop=False)
                        ins = e.matmul(PS[0:64, bb_, off:off + 64], lhsT=ARm[h % 2][:, blk, ch, 64:128], rhs=ST[:, blk, :], start=False, stop=True)
                    return ins
                S.op("pe", f, reads=["ARm", "ST"] + vu_keys + [("AT", g) for g in range(4)], writes=[pk(by), pk(by + 1)])
            if stop <= 1.9:
                raise Stop()
            bs = nb()

            def f(e, bs=bs):
                ins = None
                for blk in range(8):
                    e.matmul(PS[:, bs, blk * 64:(blk + 1) * 64], lhsT=BKtok[:, ch, 2 * blk, :], rhs=VU[:, ch, 2 * blk, :], start=True, stop=False)
                    ins = e.matmul(PS[:, bs, blk * 64:(blk + 1) * 64], lhsT=BKtok[:, ch, 2 * blk + 1, :], rhs=VU[:, ch, 2 * blk + 1, :], start=False, stop=True)
                return ins
            S.op("pe", f, reads=["BKtok"] + vu_keys, writes=[pk(bs)])
            for blk in range(8):
                S.op("dve", lambda e, blk=blk, bs=bs: e.scalar_tensor_tensor(ST[:, blk, :], ST[:, blk, :], gamC[:, blk, ch:ch + 1], PS[:, bs, blk * 64:(blk + 1) * 64], ALU.mult, ALU.add),
                     reads=[pk(bs), "gamC", "ST"], writes=["ST"])
            if own:
                for q in range(2):
                    ysrc = PS[0:64, by + q, :].rearrange("p (h x) -> p h x", h=8)
                    hs = slice(q * 8, (q + 1) * 8)
                    S.op("dve", lambda e, ysrc=ysrc, hs=hs: e.tensor_reduce(gst[:, 0, hs], ysrc, AX.X, ALU.add), reads=[pk(by + q)], writes=[("gst0", q)])
                    S.op("act", lambda e, q=q: e.activation(out=ysq[:, q * 512:(q + 1) * 512], in_=PS[0:64, by + q, :], func=AF.Square), reads=[pk(by + q)], writes=[("ysq", q)])
                    S.op("dve", lambda e, q=q, hs=hs: e.tensor_reduce(gst[:, 1, hs], ysq[:, q * 512:(q + 1) * 512].rearrange("p (h x) -> p h x", h=8), AX.X, ALU.add),
                         reads=[("ysq", q)], writes=[("gst1", q)])
                gk = [("gst0", 0), ("gst0", 1), ("gst1", 0), ("gst1", 1)]
                S.op("dve", lambda e: e.tensor_scalar(gst[:, 0, :], gst[:, 0, :], 1.0 / 64, None, ALU.mult), reads=gk, writes=["gstm"])
                S.op("dve", lambda e: e.tensor_mul(gst[:, 2, :], gst[:, 0, :], gst[:, 0, :]), reads=["gstm"], writes=["gst2"])
                S.op("dve", lambda e: e.scalar_tensor_tensor(gst[:, 3, :], gst[:, 1, :], 1.0 / 64, gst[:, 2, :], ALU.mult, ALU.subtract), reads=gk + ["gst2"], writes=["gst3"])
                S.op("dve", lambda e: e.tensor_scalar(gst[:, 3, :], gst[:, 3, :], 64e-5, None, ALU.add), reads=["gst3"], writes=["gst3"])
                S.op("act", lambda e: e.activation(out=gst[:, 3, :], in_=gst[:, 3, :], func=AF.Sqrt), reads=["gst3"], writes=["gst3"])
                S.op("dve", lambda e: e.reciprocal(gst[:, 3, :], gst[:, 3, :]), reads=["gst3"], writes=["gst3"])
                for q in range(2):
                    ysrc = PS[0:64, by + q, :].rearrange("p (h x) -> p h x", h=8)
                    hs = slice(q * 8, (q + 1) * 8)
                    yd = yn[:, q * 512:(q + 1) * 512].rearrange("p (h x) -> p h x", h=8)
                    S.op("dve", lambda e, ysrc=ysrc, hs=hs, yd=yd: e.tensor_sub(yd, ysrc, gst[:, 0, hs].unsqueeze(2).to_broadcast([64, 8, 64])),
                         reads=[pk(by + q), "gstm"], writes=[("yn", q)])
                    S.op("dve", lambda e, hs=hs, yd=yd: e.tensor_mul(yd, yd, gst[:, 3, hs].unsqueeze(2).to_broadcast([64, 8, 64])),
                         reads=["gst3", ("yn", q)], writes=[("yn", q)])
                ynk = [("yn", 0), ("yn", 1)]
                S.op("dve", lambda e: e.tensor_mul(yn[:, :], yn[:, :], gnw[:, :]), reads=ynk, writes=ynk)
                S.op("dve", lambda e: e.tensor_add(yn[:, :], yn[:, :], gnb[:, :]), reads=ynk, writes=ynk)
                S.op("dve", lambda e: e.tensor_mul(ysq[:, :].rearrange("p (h x) -> p h x", h=16), VU[0:64, ch, :, :], bsum[:, ch, :].unsqueeze(2).to_broadcast([64, 16, 64])),
                     reads=["VUv", "bsum", ("ysq", 0), ("ysq", 1)], writes=[("ysq", 0), ("ysq", 1)])
                S.op("dve", lambda e: e.tensor_add(yn[:, :], yn[:, :], ysq[:, :]), reads=ynk + [("ysq", 0), ("ysq", 1)], writes=ynk)
                bg = nb2()

                def f(e, bg=bg):
                    ins = None
                    for half in range(2):
                        for kk_ in range(2):
                            ins = e.matmul(PS[0:64, bg + half, :], lhsT=sgT[:, kk_, ch * CH:(ch + 1) * CH], rhs=g2s[:, kk_, half * 512:(half + 1) * 512], start=(kk_ == 0), stop=(kk_ == 1))
                    return ins
                S.op("pe", f, reads=["sgT"], writes=[pk(bg), pk(bg + 1)])
                for half in range(2):
                    S.op("dve", lambda e, half=half, bg=bg: e.tensor_mul(yn[:, half * 512:(half + 1) * 512], yn[:, half * 512:(half + 1) * 512], PS[0:64, bg + half, :]),
                         reads=[pk(bg + half)] + ynk, writes=ynk)
                bt = nb()

                def f(e, bt=bt):
                    ins = None
                    for blk in range(8):
                        ins = e.transpose(PS[:, bt, blk * 64:(blk + 1) * 64], yn[:, blk * 128:(blk + 1) * 128], ident64)
                    return ins
                S.op("pe", f, reads=ynk, writes=[pk(bt)])
                S.op("act", lambda e, bt=bt: e.activation(out=a_outT[:, :, ch * CH:(ch + 1) * CH], in_=PS[:, bt, :].rearrange("p (b t) -> p b t", b=8), func=AF.Copy),
                     reads=[pk(bt)], writes=["a_outT"])
                if dbg:
                    S.op("dve", lambda e, bt=bt: e.tensor_copy(junk[:, 0:512], PS[:, bt, :]), reads=[pk(bt)], writes=["junk"])
                    for blk in range(8):
                        S.dma("sp", lambda e, blk=blk: e.dma_start(out=dbg_t["aout"][blk * 128:(blk + 1) * 128, oi * TT + ch * CH:oi * TT + (ch + 1) * CH],
                                                                    in_=junk[:, blk * 64:(blk + 1) * 64]), reads=["junk"])
        phase_end(ph)
        if stop <= 2 or (own and stop <= 2.5):
            raise Stop()
        ph = phase_begin()
        qsq = sb("qsq", [128, TT])
        qrs = sb("qrs", [128, TT])
        qT = sb("qT", [128, 8, TT], BF16)
        qTm = [sb("qTm%d" % i_, [128, 8, TT], BF16) for i_ in range(2)]
        Ecur = sb("Ecur", [128, 16, 128], BF16)
        Eprev = sb("Eprev", [128, 16, 128], BF16)
        den = sb("den", [128, 4, 128])
        if need_kv:
            for s in range(NS):
                kb = ti * NS + s
                sl = kb % 3
                for g in range(4):
                    b = mm_block(w_in, [(C_KA + g * 64, 64, 0), (C_KA + g * 64, 64, 64)], 16, 128, lambda kc: hT[:, kc, s * 128:(s + 1) * 128], 128, ["hT"], "ka")
                    S.op("act", lambda e, b=b: e.activation(out=qsq[:, 0:128], in_=PS[:, b, 0:128], func=AF.Square), reads=[pk(b)], writes=["qsq"])
                    b2 = nb()
                    S.op("pe", lambda e, b2=b2: e.matmul(PS[:, b2, 0:128], lhsT=bones, rhs=qsq[:, 0:128], start=True, stop=True), reads=["qsq"], writes=[pk(b2)])
                    S.op("dve", lambda e, b2=b2: e.tensor_scalar(qrs[:, 0:128], PS[:, b2, 0:128], 1.0 / 64, 1e-6, ALU.mult, ALU.add), reads=[pk(b2)], writes=["qrs"])
                    S.op("act", lambda e: e.activation(out=qrs[:, 0:128], in_=qrs[:, 0:128], func=AF.Sqrt), reads=["qrs"], writes=["qrs"])
                    S.op("dve", lambda e: e.reciprocal(qrs[:, 0:128], qrs[:, 0:128]), reads=["qrs"], writes=["qrs"])
                    S.op("dve", lambda e, b=b, g=g, sl=sl: e.scalar_tensor_tensor(kTs[:, sl, g, :], PS[:, b, 0:128], qkg[:, 1:2], qrs[:, 0:128], ALU.mult, ALU.mult),
                         reads=[pk(b), "qrs"], writes=["kTs%d" % sl])
                wi = wb_ctr[0]
                wb_ctr[0] = (wi + 1) % NWB
                wb = wbuf[wi]
                for hf in range(2):
                    load_wblock(("va", hf), wi, 16, lambda hf=hf, wb=wb, wi=wi: S.dma("pool", lambda e: e.dma_start(
                        out=wb[:, :, 0:128], in_=w_in[:, C_VA + hf * 128:C_VA + (hf + 1) * 128].rearrange("(kc p) c -> p kc c", p=128)), writes=[("wb", wi)]))
                    b = nb()

                    def f(e, b=b, wb=wb):
                        ins = None
                        for kc in range(16):
                            ins = e.matmul(PS[:, b, 0:128], lhsT=hT[:, kc, s * 128:(s + 1) * 128], rhs=wb[:, kc, 0:128], start=(kc == 0), stop=(kc == 15))
                        return ins
                    S.op("pe", f, reads=[("wb", wi), "hT"], writes=[pk(b)])
                    for gg in range(2):
                        g = hf * 2 + gg
                        S.op("act", lambda e, b=b, g=g, gg=gg, sl=sl: e.activation(out=Vpad[:, sl, g, 0, 0:64], in_=PS[:, b, gg * 64:(gg + 1) * 64], func=AF.Copy),
                             reads=[pk(b)], writes=["Vp%d" % sl])
                        S.op("act", lambda e, b=b, g=g, gg=gg, sl=sl: e.activation(out=Vpad[:, sl, g, 1, 64:128], in_=PS[:, b, gg * 64:(gg + 1) * 64], func=AF.Copy),
                             reads=[pk(b)], writes=["Vp%d" % sl])
        if own and stop <= 2.7:
            raise Stop()
        if own:
            for qb in range(8):
                b = mm_block(w_in, [(C_Q + qb * 128, 128, 0)], 16, 128, hT_rhs, TT, ["hT"], "q")
                S.op("act", lambda e, b=b: e.activation(out=qsq[:, :], in_=PS[:, b, 0:TT], func=AF.Square), reads=[pk(b)], writes=["qsq"])
                b2 = nb()
                S.op("pe", lambda e, b2=b2: e.matmul(PS[:, b2, 0:TT], lhsT=bones, rhs=qsq[:, :], start=True, stop=True), reads=["qsq"], writes=[pk(b2)])
                S.op("dve", lambda e, b2=b2: e.tensor_scalar(qrs[:, :], PS[:, b2, 0:TT], 1.0 / 64, 1e-6, ALU.mult, ALU.add), reads=[pk(b2)], writes=["qrs"])
                S.op("act", lambda e: e.activation(out=qrs[:, :], in_=qrs[:, :], func=AF.Sqrt), reads=["qrs"], writes=["qrs"])
                S.op("dve", lambda e: e.reciprocal(qrs[:, :], qrs[:, :]), reads=["qrs"], writes=["qrs"])
                S.op("dve", lambda e, b=b, qb=qb: e.scalar_tensor_tensor(qT[:, qb, :], PS[:, b, 0:TT], qkg[:, 0:1], qrs[:, :], ALU.mult, ALU.mult),
                     reads=[pk(b), "qrs"], writes=["qT"])
                for par in range(2):
                    S.op("dve", lambda e, par=par, qb=qb: e.tensor_scalar(qTm[par][:, qb, :], qT[:, qb, :], headsel[:, par:par + 1], None, ALU.mult), reads=["qT"], writes=["qTm"])
            for s in range(NS):
                kb = ti * NS + s
                slc, slp = kb % 3, (kb - 1) % 3
                first = (oi == 0 and s == 0)
                for g4 in range(4):
                    for (sl, Ed, msk, nm) in ((slp, Eprev, mprev, "Ep"), (slc, Ecur, mcur, "Ec")):
                        b = nb()

                        def f(e, b=b, g4=g4, sl=sl):
                            ins = None
                            for j in range(4):
                                h = g4 * 4 + j
                                P0 = (h % 2) * 64
                                ins = e.matmul(PS[:, b, j * 128:(j + 1) * 128], lhsT=kTs[:, sl, h // 4, :], rhs=qTm[h % 2][:, h // 2, s * 128:(s + 1) * 128], start=True, stop=True)
                            return ins
                        S.op("pe", f, reads=["kTs%d" % sl, "qTm"], writes=[pk(b)])
                        S.op("act", lambda e, b=b, g4=g4, Ed=Ed: e.activation(out=Ed[:, g4 * 4:(g4 + 1) * 4, :], in_=PS[:, b, :].rearrange("p (h q) -> p h q", h=4), func=AF.Exp, scale=0.125),
                             reads=[pk(b)], writes=[(nm, g4)])
                        S.op("dve", lambda e, g4=g4, Ed=Ed, msk=msk: e.tensor_mul(Ed[:, g4 * 4:(g4 + 1) * 4, :], Ed[:, g4 * 4:(g4 + 1) * 4, :], msk[:, :, :]),
                             reads=[(nm, g4)], writes=[(nm, g4)])
                        if first and nm == "Ep":
                            S.op("dve", lambda e, g4=g4, Ed=Ed: e.tensor_scalar(Ed[:, g4 * 4:(g4 + 1) * 4, :], Ed[:, g4 * 4:(g4 + 1) * 4, :], flag[:, 0:1], None, ALU.mult),
                                 reads=[(nm, g4)], writes=[(nm, g4)])
                ek = [("Ep", g) for g in range(4)] + [("Ec", g) for g in range(4)]
                for half in range(2):
                    bo = nb()
                    bd = nb()

                    def f(e, bo=bo, bd=bd, half=half):
                        ins = None
                        for jb in range(4):
                            pb = half * 4 + jb
                            g = pb // 2
                            o = PS[:, bo, jb * 128:(jb + 1) * 128]
                            d = PS[:, bd, jb * 128:(jb + 1) * 128]
                            e.matmul(o, lhsT=Vpad[:, slp, g, 0, :], rhs=Eprev[:, 2 * pb, :], start=True, stop=False)
                            e.matmul(o, lhsT=Vpad[:, slp, g, 1, :], rhs=Eprev[:, 2 * pb + 1, :], start=False, stop=False)
                            e.matmul(o, lhsT=Vpad[:, slc, g, 0, :], rhs=Ecur[:, 2 * pb, :], start=False, stop=False)
                            e.matmul(o, lhsT=Vpad[:, slc, g, 1, :], rhs=Ecur[:, 2 * pb + 1, :], start=False, stop=True)
                            e.matmul(d, lhsT=onesL[:, :], rhs=Eprev[:, 2 * pb, :], start=True, stop=False)
                            e.matmul(d, lhsT=onesR[:, :], rhs=Eprev[:, 2 * pb + 1, :], start=False, stop=False)
                            e.matmul(d, lhsT=onesL[:, :], rhs=Ecur[:, 2 * pb, :], start=False, stop=False)
                            ins = e.matmul(d, lhsT=onesR[:, :], rhs=Ecur[:, 2 * pb + 1, :], start=False, stop=True)
                        return ins
                    S.op("pe", f, reads=ek + ["Vp%d" % slp, "Vp%d" % slc], writes=[pk(bo), pk(bd)])
                    S.op("dve", lambda e, bd=bd, half=half: e.tensor_add(den[:, :, :], PS[:, bd, :].rearrange("p (b q) -> p b q", b=4),
                                                                         esink[:, half * 4:(half + 1) * 4].unsqueeze(2).to_broadcast([128, 4, 128])),
                         reads=[pk(bd), "esink"], writes=["den"])
                    S.op("dve", lambda e: e.reciprocal(den[:, :, :], den[:, :, :]), reads=["den"], writes=["den"])
                    S.op("dve", lambda e, bo=bo, half=half: e.tensor_mul(b_outT[:, half * 4:(half + 1) * 4, s * 128:(s + 1) * 128], PS[:, bo, :].rearrange("p (b q) -> p b q", b=4), den[:, :, :]),
                         reads=[pk(bo), "den"], writes=["b_outT"])
            if dbg:
                for blk in range(8):
                    S.op("dve", lambda e, blk=blk: e.tensor_copy(junk[:, 0:TT], b_outT[:, blk, :]), reads=["b_outT"], writes=["junk"])
                    S.dma("sp", lambda e, blk=blk: e.dma_start(out=dbg_t["bout"][blk * 128:(blk + 1) * 128, oi * TT:(oi + 1) * TT], in_=junk[:, 0:TT]), reads=["junk"])
        phase_end(ph)
        if not own:
            return
        if stop <= 3:
            raise Stop()
        ph = phase_begin()
        xin = sb("xin", [128, D])
        xn = xin
        junk = sb("junk", [128, D])
        sga = sb("sga", [128, TT])
        sgb = sb("sgb", [128, TT])
        mt = sb("mt", [128, TT])
        mergedT = sb("mergedT", [128, 16, TT], BF16)
        x1 = xn
        h2 = junk
        h2bf = sb("h2bf", [128, D], BF16)
        h2T = sb("h2T", [128, 16, 128])
        lg = sb("lg", [128, NRL])
        rsm = sb("rsm", [128, 16])
        gmask = sb("gmask", [128, 8])
        fsel3 = sb("fsel3", [128, NG, 8])
        fsel = sb("fsel", [128, 8])
        fm1 = sb("fm1", [128, 8])
        fm2 = sb("fm2", [128, 8])
        fmk = sb("fmk", [128, 8])
        M1 = sb("M1", [128, NG, 8])
        M2 = sb("M2", [128, NG, 8])
        Mt = sb("Mt", [128, NG, 8])
        posT = sb("posT", [128, NE])
        ptmp = sb("ptmp", [128, NE])
        dstf = sb("dstf", [128, 4])
        for c in range(16):
            b = mm_block(w_in, [(C_GA + c * 128, 128, 0)], 16, 128, hT_rhs, TT, ["hT"], "ga")
            S.op("act", lambda e, b=b: e.activation(out=sga[:, :], in_=PS[:, b, 0:TT], func=AF.Sigmoid), reads=[pk(b)], writes=["sga"])
            b = mm_block(w_in, [(C_GB + c * 128, 128, 0)], 16, 128, hT_rhs, TT, ["hT"], "gb")
            S.op("act", lambda e, b=b: e.activation(out=sgb[:, :], in_=PS[:, b, 0:TT], func=AF.Sigmoid), reads=[pk(b)], writes=["sgb"])
            b = mm_block(proj_r, [(c * 128, 128, 0)], 8, 128, lambda kc: a_outT[:, kc, :], TT, ["a_outT"], "pr")
            S.op("dve", lambda e, b=b: e.tensor_mul(mt[:, :], sga[:, :], PS[:, b, 0:TT]), reads=[pk(b), "sga"], writes=["mt"])
            b = mm_block(proj_a, [(c * 128, 128, 0)], 8, 128, lambda kc: b_outT[:, kc, :], TT, ["b_outT"], "pa")
            S.op("dve", lambda e, b=b: e.tensor_mul(sgb[:, :], sgb[:, :], PS[:, b, 0:TT]), reads=[pk(b), "sgb"], writes=["sgb"])
            S.op("dve", lambda e, c=c: e.tensor_add(mergedT[:, c, :], mt[:, :], sgb[:, :]), reads=["mt", "sgb"], writes=["mergedT"])
        for s in range(NS):
            st_i = oi * NS + s
            row0 = oi * TT + s * 128
            S.dma("sp", lambda e: e.dma_start(out=xin[:, :], in_=xs[tok0 + s * 128:tok0 + (s + 1) * 128, :]), writes=["xin"])
            for n in range(4):
                bo = nb()
                for j in range(4):
                    c = n * 4 + j
                    wi = wb_ctr[0]
                    wb_ctr[0] = (wi + 1) % NWB
                    wb = wbuf[wi]
                    load_wblock(("wo", c), wi, 16, lambda c=c, wb=wb, wi=wi: S.dma("pool", lambda e: e.dma_start(
                        out=wb[:, :, :], in_=w_out[:, c * 128:(c + 1) * 128].rearrange("(kc p) c -> p kc c", p=128)), writes=[("wb", wi)]))

                    def f(e, wb=wb, j=j, bo=bo):
                        ins = None
                        for kc in range(16):
                            ins = e.matmul(PS[:, bo, j * 128:(j + 1) * 128], lhsT=mergedT[:, kc, s * 128:(s + 1) * 128], rhs=wb[:, kc, :], start=(kc == 0), stop=(kc == 15))
                        return ins
                    S.op("pe", f, reads=[("wb", wi), "mergedT"], writes=[pk(bo)])
                S.op("dve", lambda e, bo=bo, n=n: e.tensor_add(x1[:, n * 512:(n + 1) * 512], PS[:, bo, :], xin[:, n * 512:(n + 1) * 512]),
                     reads=[pk(bo), "xin"], writes=["xin"])
            S.dma("sp", lambda e: e.dma_start(out=y_out[row0:row0 + 128, :], in_=x1[:, :]), reads=["xin"], writes=[("yrow", st_i)])
            if dbg:
                S.dma("sp", lambda e: e.dma_start(out=dbg_t["xin"][row0:row0 + 128, :], in_=x1[:, :]), reads=["xin"])
            S.op("act", lambda e: e.activation(out=junk[:, :], in_=x1[:, :], func=AF.Square, accum_out=st1[:, 4:5]), reads=["xin"], writes=["junk", "st1d"])
            S.op("dve", lambda e: e.tensor_scalar(st1[:, 5:6], st1[:, 4:5], 1.0 / D, 1e-6, ALU.mult, ALU.add), reads=["st1d"], writes=["st1e"])
            S.op("act", lambda e: e.activation(out=st1[:, 6:7], in_=st1[:, 5:6], func=AF.Sqrt), reads=["st1e"], writes=["st1f"])
            S.op("dve", lambda e: e.reciprocal(st1[:, 6:7], st1[:, 6:7]), reads=["st1f"], writes=["st1f"])
            S.op("dve", lambda e: e.scalar_tensor_tensor(h2[:, :], x1[:, :], st1[:, 6:7], g2bc[:, :], ALU.mult, ALU.mult), reads=["xin", "st1f"], writes=["junk"])
            S.op("act", lambda e: e.activation(out=h2bf[:, :], in_=h2[:, :], func=AF.Copy), reads=["junk"], writes=["h2bf"])
            for g in range(4):
                b = nb()

                def f(e, g=g, b=b):
                    ins = None
                    for j in range(4):
                        kc = g * 4 + j
                        ins = e.transpose(PS[:, b, j * 128:(j + 1) * 128], h2[:, kc * 128:(kc + 1) * 128], ident)
                    return ins
                S.op("pe", f, reads=["junk"], writes=[pk(b)])
                S.op("act", lambda e, g=g, b=b: e.activation(out=h2T[:, g * 4:(g + 1) * 4, :], in_=PS[:, b, :].rearrange("p (j t) -> p j t", j=4), func=AF.Copy),
                     reads=[pk(b)], writes=ATK)
            b = nb()

            def f(e, b=b):
                ins = None
                for kc in range(16):
                    ins = e.matmul(PS[:, b, 0:NRL], lhsT=h2T[:, kc, :], rhs=wr[:, kc, :], start=(kc == 0), stop=(kc == 15))
                return ins
            S.op("pe", f, reads=ATK, writes=[pk(b)])
            S.op("dve", lambda e, b=b: e.tensor_add(lg[:, :], PS[:, b, 0:NRL], rbias[:, :]), reads=[pk(b)], writes=["lg"])
            R = "rt"
            S.op("dve", lambda e: e.tensor_reduce(rsm[:, 0:1], lg[:, 0:NG], AX.X, ALU.max), reads=["lg"], writes=[R])
            S.op("dve", lambda e: e.tensor_scalar(gmask[:, 0:NG], lg[:, 0:NG], rsm[:, 0:1], None, ALU.is_equal), reads=["lg", R], writes=[R])
            S.op("dve", lambda e: e.tensor_scalar(rsm[:, 1:2], rsm[:, 0:1], -1.0, None, ALU.mult), reads=[R], writes=[R])
            S.op("act", lambda e: e.activation(out=fm1[:, 0:NG], in_=lg[:, 0:NG], func=AF.Exp, bias=rsm[:, 1:2], accum_out=rsm[:, 2:3]), reads=["lg", R], writes=[R])
            S.op("dve", lambda e: e.reciprocal(rsm[:, 3:4], rsm[:, 2:3]), reads=[R], writes=[R])
            S.op("dve", lambda e: e.tensor_mul(fsel3[:, :, :], lg[:, 8:NRL].rearrange("p (g x) -> p g x", g=NG), gmask[:, 0:NG].unsqueeze(2).to_broadcast([128, NG, 8])),
                 reads=["lg", R], writes=[R])
            S.op("dve", lambda e: e.tensor_reduce(fsel[:, :], fsel3[:, :, :].rearrange("p g x -> p x g"), AX.X, ALU.add), reads=[R], writes=[R])
            S.op("dve", lambda e: e.tensor_reduce(rsm[:, 4:5], fsel[:, :], AX.X, ALU.max), reads=[R], writes=[R])
            S.op("dve", lambda e: e.tensor_scalar(fm1[:, :], fsel[:, :], rsm[:, 4:5], None, ALU.is_equal), reads=[R], writes=[R])
            S.op("dve", lambda e: e.scalar_tensor_tensor(fmk[:, :], fm1[:, :], -1e30, fsel[:, :], ALU.mult, ALU.add), reads=[R], writes=[R])
            S.op("dve", lambda e: e.tensor_reduce(rsm[:, 5:6], fmk[:, :], AX.X, ALU.max), reads=[R], writes=[R])
            S.op("dve", lambda e: e.tensor_scalar(fm2[:, :], fmk[:, :], rsm[:, 5:6], None, ALU.is_equal), reads=[R], writes=[R])
            S.op("dve", lambda e: e.tensor_sub(rsm[:, 6:7], rsm[:, 5:6], rsm[:, 4:5]), reads=[R], writes=[R])
            S.op("act", lambda e: e.activation(out=rsm[:, 7:8], in_=rsm[:, 6:7], func=AF.Exp), reads=[R], writes=[R])
            S.op("dve", lambda e: e.tensor_scalar(rsm[:, 8:9], rsm[:, 7:8], 1.0, None, ALU.add), reads=[R], writes=[R])
            S.op("dve", lambda e: e.reciprocal(rsm[:, 8:9], rsm[:, 8:9]), reads=[R], writes=[R])
            S.op("dve", lambda e: e.tensor_mul(rsm[:, 9:10], rsm[:, 8:9], rsm[:, 3:4]), reads=[R], writes=[R])
            S.op("dve", lambda e: e.tensor_mul(rsm[:, 10:11], rsm[:, 9:10], rsm[:, 7:8]), reads=[R], writes=[R])
            S.op("dve", lambda e: e.tensor_mul(M1[:, :, :], gmask[:, 0:NG].unsqueeze(2).to_broadcast([128, NG, 8]), fm1[:, :].unsqueeze(1).to_broadcast([128, NG, 8])), reads=[R], writes=[R])
            S.op("dve", lambda e: e.tensor_mul(M2[:, :, :], gmask[:, 0:NG].unsqueeze(2).to_broadcast([128, NG, 8]), fm2[:, :].unsqueeze(1).to_broadcast([128, NG, 8])), reads=[R], writes=[R])
            S.op("dve", lambda e: e.tensor_add(Mt[:, :, :], M1[:, :, :], M2[:, :, :]), reads=[R], writes=["Mt"])
            Mt2 = Mt[:, :, :].rearrange("p g x -> p (g x)")
            b = nb()
            S.op("pe", lambda e, b=b: e.matmul(PS[:, b, 0:NE], lhsT=tri_strict, rhs=Mt2, start=True, stop=True), reads=["Mt"], writes=[pk(b)])
            S.op("dve", lambda e, b=b: e.tensor_add(posT[:, :], PS[:, b, 0:NE], base_bc[:, :]), reads=[pk(b), "base"], writes=["posT"])
            b = nb()
            S.op("pe", lambda e, b=b: e.matmul(PS[:, b, 0:NE], lhsT=ones_m, rhs=Mt2, start=True, stop=True), reads=["Mt"], writes=[pk(b)])
            S.op("dve", lambda e, b=b: e.tensor_add(base_bc[:, :], base_bc[:, :], PS[:, b, 0:NE]), reads=[pk(b), "posT"], writes=["base"])
            S.op("dve", lambda e: e.tensor_scalar(ptmp[:, :], posT[:, :], float(CAP), None, ALU.is_ge), reads=["posT"], writes=["ptmp"])
            S.op("dve", lambda e: e.tensor_add(posT[:, :], posT[:, :], ecap[:, :]), reads=["posT"], writes=["posT"])
            S.op("dve", lambda e: e.scalar_tensor_tensor(posT[:, :], ptmp[:, :], 1e7, posT[:, :], ALU.mult, ALU.add), reads=["posT", "ptmp"], writes=["posT"])
            for kx, Mx in enumerate((M1, M2)):
                S.op("dve", lambda e, Mx=Mx: e.tensor_mul(ptmp[:, :], posT[:, :], Mx[:, :, :].rearrange("p g x -> p (g x)")), reads=["posT", R], writes=["ptmp"])
                S.op("dve", lambda e, kx=kx: e.tensor_reduce(dstf[:, kx:kx + 1], ptmp[:, :], AX.X, ALU.add), reads=["ptmp"], writes=["dstf"])
            S.op("dve", lambda e: e.tensor_scalar(dstf[:, 2:4], dstf[:, 0:2], 1e6, None, ALU.is_lt), reads=["dstf"], writes=["dstf2"])
            S.op("dve", lambda e: e.tensor_mul(rt_w[:, st_i, :], rsm[:, 9:11], dstf[:, 2:4]), reads=[R, "dstf2"], writes=[("rtw", st_i)])
            S.op("dve", lambda e: e.tensor_copy(rt_d[:, st_i, :], dstf[:, 0:2]), reads=["dstf"], writes=[("rtd", st_i)])
            if dbg:
                S.op("dve", lambda e: e.tensor_copy(junk[:, 0:2], dstf[:, 0:2]), reads=["dstf"], writes=["junk"])
                S.op("dve", lambda e: e.tensor_copy(junk[:, 2:4], rt_w[:, st_i, :]), reads=[("rtw", st_i)], writes=["junk"])
                S.op("dve", lambda e: e.tensor_copy(junk[:, 4:8], rsm[:, 0:4]), reads=[R], writes=["junk"])
                S.dma("sp", lambda e: e.dma_start(out=dbg_t["rt"][row0:row0 + 128, :], in_=junk[:, 0:8]), reads=["junk"])
            for kx in range(2):
                S.dma("pool", lambda e, kx=kx: e.indirect_dma_start(
                    out=Xd[:, :], out_offset=bass.IndirectOffsetOnAxis(ap=rt_d[:, st_i, kx:kx + 1], axis=0),
                    in_=h2bf[:, :], in_offset=None, bounds_check=bc_reg, oob_is_err=False),
                    reads=["h2bf", ("rtd", st_i)] + xdz_keys, writes=[("xd", st_i, kx)])
        phase_end(ph)

    S.barrier()
    print("sbuf remaining after mixer alloc:", nc.sbuf_bytes_remaining)
    if stop <= 0:
        S.stack.close()
        return nc
    try:
        for ti in range(NT):
            mixer_tile(ti)
    except Stop:
        S.barrier()
        S.stack.close()
        return nc
    S.barrier()
    if stop <= 4:
        S.stack.close()
        return nc
    scope[0].close()
    scope[0] = ExitStack()

    xd_keys = [("xd", i, k) for i in range(NSUB) for k in range(2)]
    bank_lo[0] = 4
    bank_ctr[0] = 4
    Xe = sb("Xe", [128, D], BF16)
    XeT = sb("XeT", [128, 16, 128], BF16)
    identb = sb("identb", [128, 128], BF16)
    S.op("dve", lambda e: e.tensor_copy(identb[:, :], ident), writes=["identb"])
    NPB_ = 6
    pbuf = [sb("pb%d" % i, [128, 16, 256], BF16) for i in range(NPB_)]
    pb_ctr = [0]
    actT = sb("actT", [128, 8, 128], BF16)
    sil = sb("sil", [128, 128])
    Ye = sb("Ye", [128, D])
    x1 = sb("x1m", [128, D])

    def nxt_pb():
        i = pb_ctr[0]
        pb_ctr[0] = (i + 1) % NPB_
        return i

    NSTG = 4
    stg = [sb("stg%d" % i, [128, 16, 256]) for i in range(NSTG)]
    stg_ctr = [0]

    def load_piece(ip, src_ap, view=None):
        k = stg_ctr[0] % NSTG
        eng = ("dve", "act")[stg_ctr[0] % 2]
        q = ("sp", "pool")[stg_ctr[0] % 2]
        stg_ctr[0] += 1
        sv = stg[k][:, :, :] if view is None else view(stg[k])
        S.dma(q, lambda e: e.dma_start(out=sv, in_=src_ap), writes=[("stg", k)])
        if eng == "act":
            S.op("act", lambda e: e.activation(out=pbuf[ip][:, :, :], in_=stg[k][:, :, :], func=AF.Copy), reads=[("stg", k)], writes=[("pb", ip)])
        else:
            S.op(eng, lambda e: e.tensor_copy(pbuf[ip][:, :, :], stg[k][:, :, :]), reads=[("stg", k)], writes=[("pb", ip)])

    for ex in range(NE):
        S.dma("sp", lambda e, ex=ex: e.dma_start(out=Xe[:, :], in_=Xd[ex * CAP:(ex + 1) * CAP, :]), reads=xd_keys, writes=["Xe"])
        for g in range(4):
            b = nb()

            def f(e, g=g, b=b):
                ins = None
                for j in range(4):
                    kc = g * 4 + j
                    ins = e.matmul(PS[:, b, j * 128:(j + 1) * 128], lhsT=Xe[:, kc * 128:(kc + 1) * 128], rhs=identb[:, :], start=True, stop=True)
                return ins
            S.op("pe", f, reads=["Xe", "identb"], writes=[pk(b)])
            S.op("act", lambda e, g=g, b=b: e.activation(out=XeT[:, g * 4:(g + 1) * 4, :], in_=PS[:, b, :].rearrange("p (j t) -> p j t", j=4), func=AF.Copy),
                 reads=[pk(b)], writes=["XeT"])
        for fblk in range(4):
            ig = nxt_pb()
            load_piece(ig, ewg[ex, :, fblk * 256:(fblk + 1) * 256].rearrange("(kc p) c -> p kc c", p=128))
            iu = nxt_pb()
            load_piece(iu, ewu[ex, :, fblk * 256:(fblk + 1) * 256].rearrange("(kc p) c -> p kc c", p=128))
            for m in range(2):
                b = nb()

                def f(e, b=b, m=m, ig=ig, iu=iu):
                    ins = None
                    for kc in range(16):
                        e.matmul(PS[:, b, 0:128], lhsT=pbuf[ig][:, kc, m * 128:(m + 1) * 128], rhs=XeT[:, kc, :], start=(kc == 0), stop=(kc == 15))
                    for kc in range(16):
                        ins = e.matmul(PS[:, b, 128:256], lhsT=pbuf[iu][:, kc, m * 128:(m + 1) * 128], rhs=XeT[:, kc, :], start=(kc == 0), stop=(kc == 15))
                    return ins
                S.op("pe", f, reads=[("pb", ig), ("pb", iu), "XeT"], writes=[pk(b)])
                S.op("act", lambda e, b=b: e.activation(out=sil[:, :], in_=PS[:, b, 0:128], func=AF.Silu), reads=[pk(b)], writes=["sil"])
                S.op("dve", lambda e, b=b, fblk=fblk, m=m: e.tensor_mul(actT[:, fblk * 2 + m, :], sil[:, :], PS[:, b, 128:256]), reads=[pk(b), "sil"], writes=["actT"])
        for pc in range(4):
            ip = nxt_pb()
            load_piece(ip, ewd[ex, pc * 256:(pc + 1) * 256, :].rearrange("(k p) c -> p k c", p=128),
                       view=lambda t: t[:, :, :].rearrange("p a b -> p (a b)").rearrange("p (k c) -> p k c", k=2))
            wdv = pbuf[ip][:, :, :].rearrange("p a b -> p (a b)").rearrange("p (k c) -> p k c", k=2)

            def f(e, pc=pc, wdv=wdv):
                ins = None
                for k2 in range(2):
                    kc = pc * 2 + k2
                    for n in range(4):
                        ins = e.matmul(PS[:, n, :], lhsT=actT[:, kc, :], rhs=wdv[:, k2, n * 512:(n + 1) * 512], start=(kc == 0), stop=(kc == 7))
                return ins
            S.op("pe", f, reads=[("pb", ip), "actT"], writes=[pk(0), pk(1), pk(2), pk(3)])
        for n in range(4):
            eng = "act" if n % 2 else "dve"
            if eng == "act":
                S.op("act", lambda e, n=n: e.activation(out=Ye[:, n * 512:(n + 1) * 512], in_=PS[:, n, :], func=AF.Copy), reads=[pk(n)], writes=[("Ye", n)])
            else:
                S.op("dve", lambda e, n=n: e.tensor_copy(Ye[:, n * 512:(n + 1) * 512], PS[:, n, :]), reads=[pk(n)], writes=[("Ye", n)])
        S.dma("sp", lambda e, ex=ex: e.dma_start(out=Yd[ex * CAP:(ex + 1) * CAP, :], in_=Ye[:, :]), reads=[("Ye", n) for n in range(4)], writes=[("yd", ex)])
    yd_keys = [("yd", ex) for ex in range(NE)]
    r1 = sb("r1", [128, D])
    r2 = sb("r2", [128, D])
    S.op("dve", lambda e: e.memset(r1[:, :], 0.0), writes=["r1"])
    S.op("dve", lambda e: e.memset(r2[:, :], 0.0), writes=["r2"])
    fin = []
    for st_i in range(NSUB):
        row0 = st_i * 128
        S.dma("sp", lambda e, row0=row0: e.dma_start(out=x1[:, :], in_=y_out[row0:row0 + 128, :]), reads=[("yrow", st_i)], writes=["xin"])
        for kx, rr in enumerate((r1, r2)):
            S.dma("pool", lambda e, kx=kx, rr=rr, st_i=st_i: e.indirect_dma_start(
                out=rr[:, :], out_offset=None, in_=Yd[:, :],
                in_offset=bass.IndirectOffsetOnAxis(ap=rt_d[:, st_i, kx:kx + 1], axis=0), bounds_check=bc_reg, oob_is_err=False),
                reads=yd_keys + [("rtd", st_i)], writes=["r%d" % (kx + 1)])
        S.op("dve", lambda e, st_i=st_i: e.scalar_tensor_tensor(x1[:, :], r1[:, :], rt_w[:, st_i, 0:1], x1[:, :], ALU.mult, ALU.add), reads=["r1", "xin", ("rtw", st_i)], writes=["xin"])
        S.op("dve", lambda e, st_i=st_i: e.scalar_tensor_tensor(x1[:, :], r2[:, :], rt_w[:, st_i, 1:2], x1[:, :], ALU.mult, ALU.add), reads=["r2", "xin", ("rtw", st_i)], writes=["xin"])
        S.dma("sp", lambda e, row0=row0: e.dma_start(out=y_out[row0:row0 + 128, :], in_=x1[:, :]), reads=["xin"], writes=[("yfin", st_i)])
        fin.append(("yfin", st_i))
    S.wait_all("sp", fin)
    if dbg:
        S.wait_all("sp", ["junk"])
    S.barrier()
    S.stack.close()
    return nc


def make_consts(n_exp, cap):
    c = np.zeros((128, 1408), np.float32)
    p = np.arange(128)
    c[:, 0:128] = np.eye(128)
    s = (p % 64)[:, None]
    t = np.arange(64)[None, :]
    c[:, 128:192] = (s < t)
    c[:, 192:256] = (s <= t)
    c[0:64, 256:320] = (np.arange(64)[:, None] < t)
    c[0:64, 320:384] = (np.arange(64)[:, None] > t)
    c[:, 384:512] = (p[:, None] // 64 == p[None, :] // 64)
    c[:, 512] = (p < 64)
    c[:, 513] = (p >= 64)
    c[:, 640:768] = (p[:, None] < p[None, :])
    c[:, 768:896] = 1.0
    c[:, 896:1024] = (p[:, None] <= p[None, :])
    c[:, 1024:1152] = (p[:, None] > p[None, :])
    c[:, 1152:1216] = 1.0
    c[:, 1344:1408] = 1.0
    ecap = np.tile((np.arange(n_exp) * cap).astype(np.float32)[None, :], (128, 1))
    return c, ecap


def core_inputs(inp, b, hh, n_prev, n_own, n_groups, cap):
    sq = lambda a: np.ascontiguousarray(np.asarray(a)[0])
    x = np.asarray(inp["x"])
    own = x[b, hh * n_own * TT:(hh + 1) * n_own * TT]
    if hh == 0:
        prev = np.zeros((n_prev * TT, D), np.float32)
    else:
        prev = x[b, hh * n_own * TT - n_prev * TT:hh * n_own * TT]
    ne = n_groups * 8
    c, ecap = make_consts(ne, cap)
    m = {
        "xs": np.ascontiguousarray(np.concatenate([prev, own], 0)),
        "flag": np.full((128, 1), float(hh), np.float32),
        "w_in": sq(inp["w_in"]),
        "norm1_w": sq(inp["norm1_w"]).reshape(1, D),
        "norm2_w": sq(inp["norm2_w"]).reshape(1, D),
        "rwkv_mu": sq(inp["rwkv_mu"]).reshape(-1, 1),
        "rwkv_w0": sq(inp["rwkv_w0"]).reshape(-1, 1),
        "rwkv_a0": sq(inp["rwkv_a0"]).reshape(-1, 1),
        "rwkv_k_k": sq(inp["rwkv_k_k"]).reshape(-1, 1),
        "rwkv_k_a": sq(inp["rwkv_k_a"]).reshape(-1, 1),
        "rwkv_r_k": sq(inp["rwkv_r_k"]).reshape(-1, 1),
        "rwkv_w2": sq(inp["rwkv_w2"]),
        "rwkv_a2": sq(inp["rwkv_a2"]),
        "rwkv_g2": sq(inp["rwkv_g2"]),
        "rwkv_gn_w": sq(inp["rwkv_gn_w"]).reshape(1, -1),
        "rwkv_gn_b": sq(inp["rwkv_gn_b"]).reshape(1, -1),
        "q_norm_w": sq(inp["q_norm_w"]).reshape(-1, 1),
        "k_norm_w": sq(inp["k_norm_w"]).reshape(-1, 1),
        "attn_sinks": sq(inp["attn_sinks"]).reshape(-1, 1),
        "proj_rwkv": sq(inp["proj_rwkv"]),
        "proj_attn": sq(inp["proj_attn"]),
        "w_out": sq(inp["w_out"]),
        "router_coarse_w": sq(inp["router_coarse_w"]),
        "router_coarse_b": sq(inp["router_coarse_b"]).reshape(1, -1),
        "router_fine_w": sq(inp["router_fine_w"]),
        "router_fine_b": sq(inp["router_fine_b"]).reshape(1, -1),
        "expert_w_gate": sq(inp["expert_w_gate"]),
        "expert_w_up": sq(inp["expert_w_up"]),
        "expert_w_down": sq(inp["expert_w_down"]),
        "consts": c,
        "ecap": ecap,
    }
    return m


def kernel(**inputs):
    cfg = dict(n_prev=16, n_own=16, n_groups=8, cap=128)
    nc = build_program(cfg)
    in_maps = []
    for c in range(8):
        in_maps.append(core_inputs(inputs, c // 2, c % 2, 16, 16, 8, 128))
    res = run_bass_kernel_spmd(nc, in_maps, core_ids=list(range(8)))
    out = np.zeros((4, 4096, D), np.float32)
    for c in range(8):
        b, hh = c // 2, c % 2
        out[b, hh * 2048:(hh + 1) * 2048] = res.results[c]["y"]
    return out
```
